# Optimizing a Trainium2 kernel written in Bass

```python
import jax
import jax.numpy as jnp
from jax import lax
import numpy as np


D_MODEL = 1024
BATCH = 8
SEQ = 4096
DEPTH = 4

GRID_W = 64
CTX_LEN = 256
NORM_EPS = 1e-6
ROPE_BASE = 10000.0
NEG_INF = -1e30

A_HEADS = 8
A_KV_HEADS = 2
A_HEAD_DIM = 64
WINDOW = 128
A_BLOCK = 128
B_WIDTH = 512
B_GROUPS = 4
B_CHUNK = 128
C_HEADS = 8
C_Q_LORA = 384
C_KV_LORA = 256
C_NOPE = 64
C_ROPE = 32
C_V = 64
C_BLOCK = 128
D_WIDTH = 512
D_CONV = 3
N_EXPERTS = 16
EXPERT_FF = 1024
EC_FACTOR = 2

A_Q_W = A_HEADS * A_HEAD_DIM
A_KV_W = A_KV_HEADS * A_HEAD_DIM
EVEN_CTX_COLS = 2 * A_KV_W
EVEN_IN = EVEN_CTX_COLS + A_Q_W + 2 * B_WIDTH
EVEN_OUT = A_Q_W + B_WIDTH
ODD_CTX_COLS = C_KV_LORA + C_ROPE
ODD_IN = ODD_CTX_COLS + C_Q_LORA + 3 * D_WIDTH
ODD_OUT = C_HEADS * C_V + D_WIDTH
N_EVEN = (DEPTH + 1) // 2
N_ODD = DEPTH // 2

kernel_name = 'hybrid_diffusion_backbone'


def rms_norm(x, g):
    xf = x.astype(jnp.float32)
    y = xf * lax.rsqrt(jnp.mean(xf * xf, axis=-1, keepdims=True) + NORM_EPS)
    return (y * g.astype(jnp.float32)).astype(x.dtype)


def layer_norm(x, g):
    xf = x.astype(jnp.float32)
    mu = jnp.mean(xf, axis=-1, keepdims=True)
    var = jnp.mean(jnp.square(xf - mu), axis=-1, keepdims=True)
    return ((xf - mu) * lax.rsqrt(var + NORM_EPS) * g.astype(jnp.float32)).astype(x.dtype)


def axial_rope_tables(rows, rot_dim):
    row = jnp.repeat(jnp.arange(rows, dtype=jnp.float32), GRID_W)
    col = jnp.tile(jnp.arange(GRID_W, dtype=jnp.float32), rows)
    n_freq = rot_dim // 4
    inv_freq = ROPE_BASE ** (-jnp.arange(n_freq, dtype=jnp.float32) / n_freq)
    ang = jnp.concatenate([row[:, None] * inv_freq[None, :], col[:, None] * inv_freq[None, :]], axis=-1)
    return jnp.cos(ang), jnp.sin(ang)


def apply_rope(x, cos, sin):
    x1, x2 = jnp.split(x, 2, axis=-1)
    cs = cos[None, :, None, :].astype(x.dtype)
    sn = sin[None, :, None, :].astype(x.dtype)
    return jnp.concatenate([x1 * cs - x2 * sn, x2 * cs + x1 * sn], axis=-1)


def gqa_attend(q, k, v, mask, sink, scale):
    s = jnp.einsum('bqhgd,bkhd->bhgqk', q, k).astype(jnp.float32) * scale
    if mask is not None:
        s = jnp.where(mask, s, NEG_INF)
    if sink is not None:
        sk = jnp.broadcast_to(sink.astype(jnp.float32)[None, :, :, None, None], s.shape[:-1] + (1,))
        s = jnp.concatenate([s, sk], axis=-1)
    p = jax.nn.softmax(s, axis=-1)
    if sink is not None:
        p = p[..., :-1]
    return jnp.einsum('bhgqk,bkhd->bqhgd', p.astype(v.dtype), v)


def window_attention(q, k, v, k_ctx, v_ctx, sink_g, scale):
    bsz, n = q.shape[0], q.shape[1]
    g = A_HEADS // A_KV_HEADS
    nb = n // A_BLOCK
    span = A_BLOCK + 2 * WINDOW
    pad = ((0, 0), (WINDOW, WINDOW), (0, 0), (0, 0))
    kp = jnp.pad(k, pad)
    vp = jnp.pad(v, pad)
    qi = jnp.arange(A_BLOCK)
    kj = jnp.arange(span)
    band = jnp.abs(kj[None, :] - WINDOW - qi[:, None]) <= WINDOW
    ctx_ok = jnp.ones((A_BLOCK, k_ctx.shape[1]), dtype=jnp.bool_)

    def block(nidx):
        start = nidx * A_BLOCK
        qb = lax.dynamic_slice_in_dim(q, start, A_BLOCK, axis=1).reshape(bsz, A_BLOCK, A_KV_HEADS, g, A_HEAD_DIM)
        kb = lax.dynamic_slice_in_dim(kp, start, span, axis=1)
        vb = lax.dynamic_slice_in_dim(vp, start, span, axis=1)
        kpos = start - WINDOW + kj
        mask = band & ((kpos >= 0) & (kpos < n))[None, :]
        mask = jnp.concatenate([mask, ctx_ok], axis=1)
        o = gqa_attend(qb, jnp.concatenate([kb, k_ctx], axis=1), jnp.concatenate([vb, v_ctx], axis=1), mask, sink_g, scale)
        return o.reshape(bsz, A_BLOCK, A_Q_W)

    out = lax.map(block, jnp.arange(nb))
    return jnp.moveaxis(out, 0, 1).reshape(bsz, n, A_Q_W)


def dense_block_attention(q, k_all, v_all, scale):
    bsz, n, h, _ = q.shape
    dv = v_all.shape[-1]
    nb = n // C_BLOCK

    def block(nidx):
        qb = lax.dynamic_slice_in_dim(q, nidx * C_BLOCK, C_BLOCK, axis=1)
        return gqa_attend(qb[:, :, :, None], k_all, v_all, None, None, scale)[:, :, :, 0]

    out = lax.map(block, jnp.arange(nb))
    return jnp.moveaxis(out, 0, 1).reshape(bsz, n, h * dv)


def spatial_gating(z, norm_g, w_s, b_s):
    bsz, n, _ = z.shape
    u, v = jnp.split(jax.nn.gelu(z), 2, axis=-1)
    v = layer_norm(v, norm_g)
    vc = v.reshape(bsz, n // B_CHUNK, B_CHUNK, B_GROUPS, B_WIDTH // B_GROUPS)
    mixed = jnp.einsum('gpq,bcqgd->bcpgd', w_s, vc) + b_s.T[None, None, :, :, None]
    return u * mixed.reshape(bsz, n, B_WIDTH)


def gated_short_conv(z, conv_w):
    gb, gc, hh = jnp.split(z, 3, axis=-1)
    u = gc * hh
    n = u.shape[1]
    half = D_CONV // 2
    up = jnp.pad(u, ((0, 0), (half, half), (0, 0)))
    conv = sum(up[:, j:j + n] * conv_w[j] for j in range(D_CONV))
    return gb * conv


def mla_q(cq, q_norm_g, w_uq, cos, sin):
    bsz, n, _ = cq.shape
    q = (rms_norm(cq, q_norm_g) @ w_uq).reshape(bsz, n, C_HEADS, C_NOPE + C_ROPE)
    q_nope, q_rope = q[..., :C_NOPE], q[..., C_NOPE:]
    if cos is not None:
        q_rope = apply_rope(q_rope, cos, sin)
    return jnp.concatenate([q_nope, q_rope], axis=-1)


def mla_kv(ckv, kr, kv_norm_g, w_ukv, cos, sin):
    bsz, n, _ = ckv.shape
    kv = (rms_norm(ckv, kv_norm_g) @ w_ukv).reshape(bsz, n, C_HEADS, C_NOPE + C_V)
    k_nope, v = kv[..., :C_NOPE], kv[..., C_NOPE:]
    kr = kr[:, :, None, :]
    if cos is not None:
        kr = apply_rope(kr, cos, sin)
    k = jnp.concatenate([k_nope, jnp.broadcast_to(kr, (bsz, n, C_HEADS, C_ROPE))], axis=-1)
    return k, v


def even_mixer(h_lat, h_ctx, w_in, sink, sgu_norm_g, sgu_w, sgu_b, w_out, cos, sin, need_ctx):
    bsz, n_lat, _ = h_lat.shape
    n_ctx = h_ctx.shape[1]
    g = A_HEADS // A_KV_HEADS
    scale = A_HEAD_DIM ** -0.5
    sink_g = sink.reshape(A_KV_HEADS, g)
    z_lat = h_lat @ w_in
    z_ctx = h_ctx @ (w_in if need_ctx else w_in[:, :EVEN_CTX_COLS])
    k_ctx = z_ctx[..., :A_KV_W].reshape(bsz, n_ctx, A_KV_HEADS, A_HEAD_DIM)
    v_ctx = z_ctx[..., A_KV_W:EVEN_CTX_COLS].reshape(bsz, n_ctx, A_KV_HEADS, A_HEAD_DIM)
    k_lat = apply_rope(z_lat[..., :A_KV_W].reshape(bsz, n_lat, A_KV_HEADS, A_HEAD_DIM), cos, sin)
    v_lat = z_lat[..., A_KV_W:EVEN_CTX_COLS].reshape(bsz, n_lat, A_KV_HEADS, A_HEAD_DIM)
    q_lat = apply_rope(z_lat[..., EVEN_CTX_COLS:EVEN_CTX_COLS + A_Q_W].reshape(bsz, n_lat, A_HEADS, A_HEAD_DIM), cos, sin)
    o_lat = window_attention(q_lat, k_lat, v_lat, k_ctx, v_ctx, sink_g, scale)
    s_lat = spatial_gating(z_lat[..., EVEN_CTX_COLS + A_Q_W:], sgu_norm_g, sgu_w, sgu_b)
    y_lat = jnp.concatenate([o_lat, s_lat], axis=-1) @ w_out
    if not need_ctx:
        return y_lat, None
    q_ctx = z_ctx[..., EVEN_CTX_COLS:EVEN_CTX_COLS + A_Q_W].reshape(bsz, n_ctx, A_KV_HEADS, g, A_HEAD_DIM)
    o_ctx = gqa_attend(q_ctx, k_ctx, v_ctx, None, sink_g, scale).reshape(bsz, n_ctx, A_Q_W)
    s_ctx = spatial_gating(z_ctx[..., EVEN_CTX_COLS + A_Q_W:], sgu_norm_g, sgu_w, sgu_b)
    y_ctx = jnp.concatenate([o_ctx, s_ctx], axis=-1) @ w_out
    return y_lat, y_ctx


def odd_mixer(h_lat, h_ctx, w_in, q_norm_g, w_uq, kv_norm_g, w_ukv, conv_w, w_out, cos, sin, need_ctx):
    bsz, n_ctx = h_ctx.shape[0], h_ctx.shape[1]
    scale = (C_NOPE + C_ROPE) ** -0.5
    z_lat = h_lat @ w_in
    z_ctx = h_ctx @ (w_in if need_ctx else w_in[:, :ODD_CTX_COLS])
    k_ctx, v_ctx = mla_kv(z_ctx[..., :C_KV_LORA], z_ctx[..., C_KV_LORA:ODD_CTX_COLS], kv_norm_g, w_ukv, None, None)
    k_lat, v_lat = mla_kv(z_lat[..., :C_KV_LORA], z_lat[..., C_KV_LORA:ODD_CTX_COLS], kv_norm_g, w_ukv, cos, sin)
    q_lat = mla_q(z_lat[..., ODD_CTX_COLS:ODD_CTX_COLS + C_Q_LORA], q_norm_g, w_uq, cos, sin)
    k_all = jnp.concatenate([k_lat, k_ctx], axis=1)
    v_all = jnp.concatenate([v_lat, v_ctx], axis=1)
    o_lat = dense_block_attention(q_lat, k_all, v_all, scale)
    c_lat = gated_short_conv(z_lat[..., ODD_CTX_COLS + C_Q_LORA:], conv_w)
    y_lat = jnp.concatenate([o_lat, c_lat], axis=-1) @ w_out
    if not need_ctx:
        return y_lat, None
    q_ctx = mla_q(z_ctx[..., ODD_CTX_COLS:ODD_CTX_COLS + C_Q_LORA], q_norm_g, w_uq, None, None)
    o_ctx = gqa_attend(q_ctx[:, :, :, None], k_ctx, v_ctx, None, None, scale)[:, :, :, 0].reshape(bsz, n_ctx, C_HEADS * C_V)
    c_ctx_out = gated_short_conv(z_ctx[..., ODD_CTX_COLS + C_Q_LORA:], conv_w)
    y_ctx = jnp.concatenate([o_ctx, c_ctx_out], axis=-1) @ w_out
    return y_lat, y_ctx


def expert_choice_ffn(h, w_router, w1, w3, w2):
    n_tok, d = h.shape[1], h.shape[2]
    cap = EC_FACTOR * n_tok // N_EXPERTS
    aff = jax.nn.softmax(jnp.einsum('bnd,de->bne', h, w_router).astype(jnp.float32), axis=-1)
    gate, idx = lax.top_k(jnp.swapaxes(aff, 1, 2), cap)
    xs = jax.vmap(lambda hb, ib: hb[ib])(h, idx)
    a = jnp.einsum('becd,edf->becf', xs, w1)
    b = jnp.einsum('becd,edf->becf', xs, w3)
    y = jnp.einsum('becf,efd->becd', jax.nn.silu(a) * b, w2) * gate[..., None].astype(h.dtype)

    def combine(yb, ib):
        return jnp.zeros((n_tok, d), h.dtype).at[ib.reshape(-1)].add(yb.reshape(-1, d))

    return jax.vmap(combine)(y, idx)


def setup_inputs(seed: int = 0) -> dict:
    key = jax.random.key(seed)
    ks = iter(jax.random.split(key, 32))
    D = D_MODEL

    def nrm(shape, scale):
        return jax.random.normal(next(ks), shape, jnp.float32) * scale

    return {
        'x': nrm((BATCH, SEQ, D), 1.0),
        'c': nrm((BATCH, D), 1.0),
        'ctx': nrm((BATCH, CTX_LEN, D), 1.0),
        'c_ctx': nrm((D,), 1.0),
        'mod_w': nrm((DEPTH, D, 6 * D), 0.5 * D ** -0.5),
        'mod_b': nrm((DEPTH, 6 * D), 0.01),
        'norm1_g': 1.0 + nrm((DEPTH, D), 0.05),
        'norm2_g': 1.0 + nrm((DEPTH, D), 0.05),
        'ev_w_in': nrm((N_EVEN, D, EVEN_IN), D ** -0.5),
        'ev_sink': nrm((N_EVEN, A_HEADS), 0.5),
        'ev_sgu_norm_g': 1.0 + nrm((N_EVEN, B_WIDTH), 0.05),
        'ev_sgu_w': nrm((N_EVEN, B_GROUPS, B_CHUNK, B_CHUNK), B_CHUNK ** -0.5),
        'ev_sgu_b': nrm((N_EVEN, B_GROUPS, B_CHUNK), 0.1),
        'ev_w_out': nrm((N_EVEN, EVEN_OUT, D), EVEN_OUT ** -0.5),
        'od_w_in': nrm((N_ODD, D, ODD_IN), D ** -0.5),
        'od_q_norm_g': 1.0 + nrm((N_ODD, C_Q_LORA), 0.05),
        'od_w_uq': nrm((N_ODD, C_Q_LORA, C_HEADS * (C_NOPE + C_ROPE)), C_Q_LORA ** -0.5),
        'od_kv_norm_g': 1.0 + nrm((N_ODD, C_KV_LORA), 0.05),
        'od_w_ukv': nrm((N_ODD, C_KV_LORA, C_HEADS * (C_NOPE + C_V)), C_KV_LORA ** -0.5),
        'od_conv_w': nrm((N_ODD, D_CONV, D_WIDTH), D_CONV ** -0.5),
        'od_w_out': nrm((N_ODD, ODD_OUT, D), ODD_OUT ** -0.5),
        'router_w': nrm((DEPTH, D, N_EXPERTS), D ** -0.5),
        'exp_w1': nrm((DEPTH, N_EXPERTS, D, EXPERT_FF), D ** -0.5),
        'exp_w3': nrm((DEPTH, N_EXPERTS, D, EXPERT_FF), D ** -0.5),
        'exp_w2': nrm((DEPTH, N_EXPERTS, EXPERT_FF, D), EXPERT_FF ** -0.5),
        'final_g': 1.0 + nrm((D,), 0.05),
    }


def reference(x, c, ctx, c_ctx, mod_w, mod_b, norm1_g, norm2_g, ev_w_in, ev_sink, ev_sgu_norm_g, ev_sgu_w, ev_sgu_b, ev_w_out, od_w_in, od_q_norm_g, od_w_uq, od_kv_norm_g, od_w_ukv, od_conv_w, od_w_out, router_w, exp_w1, exp_w3, exp_w2, final_g):
    rows = x.shape[1] // GRID_W
    cos_a, sin_a = axial_rope_tables(rows, A_HEAD_DIM)
    cos_c, sin_c = axial_rope_tables(rows, C_ROPE)
    silu_c = jax.nn.silu(c)
    silu_cc = jax.nn.silu(c_ctx)[None, :]
    x_lat, x_ctx = x, ctx
    for layer in range(DEPTH):
        need_ctx = layer < DEPTH - 1
        m_lat = jnp.split((silu_c @ mod_w[layer] + mod_b[layer])[:, None, :], 6, axis=-1)
        m_ctx = jnp.split((silu_cc @ mod_w[layer] + mod_b[layer])[:, None, :], 6, axis=-1)
        h_lat = rms_norm(x_lat, norm1_g[layer]) * (1 + m_lat[1]) + m_lat[0]
        h_ctx = rms_norm(x_ctx, norm1_g[layer]) * (1 + m_ctx[1]) + m_ctx[0]
        i = layer // 2
        if layer % 2 == 0:
            y_lat, y_ctx = even_mixer(h_lat, h_ctx, ev_w_in[i], ev_sink[i], ev_sgu_norm_g[i], ev_sgu_w[i], ev_sgu_b[i], ev_w_out[i], cos_a, sin_a, need_ctx)
        else:
            y_lat, y_ctx = odd_mixer(h_lat, h_ctx, od_w_in[i], od_q_norm_g[i], od_w_uq[i], od_kv_norm_g[i], od_w_ukv[i], od_conv_w[i], od_w_out[i], cos_c, sin_c, need_ctx)
        x_lat = x_lat + m_lat[2] * y_lat
        h_lat = rms_norm(x_lat, norm2_g[layer]) * (1 + m_lat[4]) + m_lat[3]
        x_lat = x_lat + m_lat[5] * expert_choice_ffn(h_lat, router_w[layer], exp_w1[layer], exp_w3[layer], exp_w2[layer])
        if need_ctx:
            x_ctx = x_ctx + m_ctx[2] * y_ctx
            h_ctx = rms_norm(x_ctx, norm2_g[layer]) * (1 + m_ctx[4]) + m_ctx[3]
            x_ctx = x_ctx + m_ctx[5] * expert_choice_ffn(h_ctx, router_w[layer], exp_w1[layer], exp_w3[layer], exp_w2[layer])
    return rms_norm(x_lat, final_g)
```

```python
import os
import numpy as np
from contextlib import ExitStack
import concourse.bass as bass
import concourse.mybir as mybir
from concourse.bass_utils import run_bass_kernel_spmd

F32 = mybir.dt.float32
BF16 = mybir.dt.bfloat16
I32 = mybir.dt.int32
AF = mybir.ActivationFunctionType
ALU = mybir.AluOpType
AX = mybir.AxisListType

D = 1024
NLAT = 4096
NCTX = 256
NTOK = NLAT + NCTX
NT = NTOK // 128
NTL = NLAT // 128
DEPTH = 4
NE = 16
EPS = 1e-6
EV_IN = 1792
OD_IN = 2208


class Buf:
    __slots__ = ("name", "w", "r")

    def __init__(self, name=""):
        self.name = name
        self.w = None
        self.r = []


class TB:
    def __init__(self, t, name):
        self.t = t
        self.b = Buf(name)


def _free(ap):
    n = 1
    for d in tuple(ap.shape)[1:]:
        n *= int(d)
    return n


_DTB = {F32: 4, BF16: 2, I32: 4}


class Sched:
    ENGS = ("pe", "act", "dve", "pool", "sp")

    def __init__(self, nc, es, ndma=None):
        ndma = ndma or {"sp": 14, "pool": 14}
        self.nc = nc
        self.sems = []
        self.esem = {}
        for e in self.ENGS:
            self.esem[e] = len(self.sems)
            self.sems.append(es.enter_context(nc.semaphore("s_" + e)))
        self.dpool = {}
        for e, n in ndma.items():
            self.dpool[e] = []
            for i in range(n):
                self.dpool[e].append(len(self.sems))
                self.sems.append(es.enter_context(nc.semaphore("d_%s%d" % (e, i))))
        self.nodes = []
        self.tbl = {}
        self.segs = [0]
        self.nops = 0
        self.limit = None
        self.force = False
        self.ninst = 0
        self.reorder = True
        self.noreorder = set(int(x) for x in os.environ.get("KNOREORDER", "").split(",") if x)
        self.cur_seg = -1

    def _cost(self, eng, name, a, kw, dma):
        try:
            if dma:
                nb = None
                for ap in (kw["out"], kw["in_"]):
                    n_ = 1
                    for d in tuple(ap.shape):
                        n_ *= int(d)
                    n_ *= _DTB.get(ap.dtype, 4)
                    nb = n_ if nb is None else min(nb, n_)
                if "indirect" in name:
                    return 1.0, 3.0 + nb / 150e3
                occ = 0.6 if eng == "pool" else 0.08
                return occ, 2.2 + nb / 380e3
            if eng == "pe":
                if name == "matmul":
                    rhs = kw["rhs"]
                    n = max(_free(rhs), 64)
                    if rhs.dtype == F32:
                        n *= 4
                    t = 0.03 + n / 2400.0
                else:
                    t = 0.09
                return t, t + 0.05
            out = kw.get("out", a[0] if a else None)
            f = _free(out) if out is not None else 64
            if eng == "act":
                t = 0.22 + f / 1200.0
            elif eng == "dve":
                t = 0.08 + f / 960.0
            else:
                t = 0.12 + f / 450.0
            return t, t + 0.05
        except Exception:
            return 0.3, 0.4

    def op(self, eng, name, reads, writes, *a, dma=False, **kw):
        self.nops += 1
        if self.limit is not None and self.nops > self.limit and not self.force:
            return None
        nid = len(self.nodes)
        deps = set()
        for b in reads:
            if b.w is not None:
                deps.add(b.w)
        for b in writes:
            if b.w is not None:
                deps.add(b.w)
            deps.update(b.r)
        occ, lat = self._cost(eng, name, a, kw, dma)
        tbl = None
        if eng == "act" and name == "activation":
            f = kw.get("func")
            if f == AF.Sqrt:
                tbl = "sqrt"
            elif f in (AF.Exp, AF.Tanh):
                tbl = "exp"
            elif f == AF.Silu:
                tbl = "silu"
            elif f == AF.Sigmoid:
                tbl = "sig"
        self.tbl[nid] = tbl
        self.nodes.append((eng, (name, a, kw), dma, deps, occ, lat))
        for b in reads:
            b.r.append(nid)
        for b in writes:
            b.w = nid
            b.r = []
        return nid

    def barrier(self):
        if self.segs[-1] != len(self.nodes):
            self.segs.append(len(self.nodes))

    def _schedule(self, lo, hi):
        import heapq
        nodes = self.nodes
        if not self.reorder or self.cur_seg in self.noreorder:
            return list(range(lo, hi))
        indeg = {}
        succ = {}
        ready_t = {}
        for n in range(lo, hi):
            c = 0
            for d in nodes[n][3]:
                if d >= lo:
                    c += 1
                    succ.setdefault(d, []).append(n)
            indeg[n] = c
            ready_t[n] = 0.0
        pend = {e: [] for e in self.ENGS}
        avail = {e: [] for e in self.ENGS}
        for n in range(lo, hi):
            if indeg[n] == 0:
                heapq.heappush(pend[nodes[n][0]], (0.0, n))
        t_eng = {e: 0.0 for e in self.ENGS}
        dma_free = 0.0
        order = []
        cur_tbl = None
        tblmap = self.tbl
        table_aware = not os.environ.get("KNOTBL")
        total = hi - lo
        while len(order) < total:
            best = None
            for e in self.ENGS:
                pe_, av = pend[e], avail[e]
                te = t_eng[e]
                while pe_ and pe_[0][0] <= te:
                    heapq.heappush(av, heapq.heappop(pe_)[1])
                if av:
                    pick = av[0]
                    if e == "act" and table_aware and len(av) > 1:
                        t0_ = tblmap.get(pick)
                        if t0_ is not None and t0_ != cur_tbl:
                            best_alt = None
                            for x_ in av:
                                tx = tblmap.get(x_)
                                if (tx is None or tx == cur_tbl) and x_ < pick + 400 and (best_alt is None or x_ < best_alt):
                                    best_alt = x_
                            if best_alt is not None:
                                pick = best_alt
                    cand = (te, pick, e, True)
                elif pe_:
                    cand = (pe_[0][0], pe_[0][1], e, False)
                else:
                    continue
                if best is None or cand[:2] < best[:2]:
                    best = cand
            st, n, e, from_av = best
            if from_av:
                if avail[e][0] == n:
                    heapq.heappop(avail[e])
                else:
                    avail[e].remove(n)
                    heapq.heapify(avail[e])
            else:
                heapq.heappop(pend[e])
            eng, fn, dma, deps, occ, lat = nodes[n]
            if e == "act":
                tn = tblmap.get(n)
                if tn is not None and tn != cur_tbl:
                    cur_tbl = tn
                    st += 1.3
            if dma:
                s2 = max(st, dma_free)
                fin = s2 + lat
                dma_free = s2 + max(lat - (3.0 if "indirect" in fn[0] else 2.2), 0.0)
            else:
                fin = st + lat
            t_eng[e] = st + occ
            order.append(n)
            if os.environ.get("KTRACE") and self.cur_seg == int(os.environ["KTRACE"]) and len(order) % 150 == 0:
                print("SIM %5d %-5s %-22s st=%8.2f fin=%8.2f" % (n, e, fn[0], st, fin))
            for m in succ.get(n, ()):
                hop = 0.05 if nodes[m][0] == e and not dma else 0.25
                rt = fin + hop
                if rt > ready_t[m]:
                    ready_t[m] = rt
                indeg[m] -= 1
                if indeg[m] == 0:
                    heapq.heappush(pend[nodes[m][0]], (ready_t[m], m))
        self.est_time = getattr(self, "est_time", 0.0) + max(t_eng.values())
        if os.environ.get("KVERBOSE"):
            print("[kernel] seg", self.cur_seg, lo, hi, "est", round(max(t_eng.values()), 1), {e: round(v, 1) for e, v in t_eng.items()})
        return order

    def emit(self, block):
        nodes = self.nodes
        if self.segs[-1] != len(nodes):
            self.segs.append(len(nodes))
        prog = {e: [] for e in self.ENGS}
        cnt = {e: 0 for e in self.ENGS}
        known = {e: {} for e in self.ENGS}
        dval = {k: 0 for e in self.dpool for k in self.dpool[e]}
        dnext = {e: 0 for e in self.dpool}
        ev = {}
        pe_own = self.esem["pe"]
        for si in range(len(self.segs) - 1):
            lo, hi = self.segs[si], self.segs[si + 1]
            self.cur_seg = si
            if os.environ.get("KVERBOSE"):
                print("[kernel] segment", si, lo, hi)
            for n in self._schedule(lo, hi):
                eng, fn, dma, deps, occ, lat = nodes[n]
                kn = known[eng]
                waits = {}
                for d in deps:
                    k, v = ev[d]
                    if eng == "pe" and k == pe_own:
                        continue
                    if kn.get(k, 0) >= v:
                        continue
                    if waits.get(k, 0) < v:
                        waits[k] = v
                if dma:
                    pool = self.dpool[eng]
                    k = pool[dnext[eng] % len(pool)]
                    dnext[eng] += 1
                    if dval[k] > 0 and kn.get(k, 0) < dval[k] and waits.get(k, 0) < dval[k]:
                        waits[k] = dval[k]
                    dval[k] += 16
                    ev[n] = (k, dval[k])
                    inc = 16
                else:
                    k = self.esem[eng]
                    cnt[eng] += 1
                    ev[n] = (k, cnt[eng])
                    inc = 1
                for k2, v in waits.items():
                    kn[k2] = v
                prog[eng].append((list(waits.items()), fn, k, inc))
                self.ninst += 1 + len(waits)
            targets = [(self.esem[e], cnt[e]) for e in self.ENGS if cnt[e] > 0]
            targets += [(k, v) for k, v in dval.items() if v > 0]
            for e in self.ENGS:
                waits = []
                for k, v in targets:
                    if known[e].get(k, 0) >= v:
                        continue
                    known[e][k] = v
                    waits.append((k, v))
                if waits:
                    prog[e].append((waits, None, None, 0))
                    self.ninst += len(waits)
        sems = self.sems

        def mk(p):
            def body(engine):
                regcache = {}
                for waits, fn, k, inc in p:
                    for k2, v in waits:
                        engine.wait_ge(sems[k2], v)
                    if fn is not None:
                        kw = fn[2]
                        if "bounds_check" in kw:
                            bv = kw["bounds_check"]
                            if bv not in regcache:
                                regcache[bv] = engine.to_reg(bv)
                            kw = dict(kw)
                            kw["bounds_check"] = regcache[bv]
                        fn = (fn[0], fn[1], kw)
                        try:
                            ins = getattr(engine, fn[0])(*fn[1], **fn[2])
                        except Exception:
                            print("[kernel] failed to emit", fn[0], fn[1], fn[2])
                            raise
                        ins.then_inc(sems[k], inc)
            return body

        block.tensor(mk(prog["pe"]))
        block.scalar(mk(prog["act"]))
        block.vector(mk(prog["dve"]))
        block.gpsimd(mk(prog["pool"]))
        block.sync(mk(prog["sp"]))
        print("[kernel] instructions incl. waits:", self.ninst, "est us:", round(getattr(self, "est_time", 0.0), 1))


class KB:
    def __init__(self, nc, S, es):
        self.nc = nc
        self.S = S
        self.es = es
        self.n = 0

    def sb(self, es, shape, dt, name=None):
        self.n += 1
        name = (name or "t") + "_%d" % self.n
        return TB(es.enter_context(self.nc.sbuf_tensor(name, list(shape), dt)), name)

    def ring(self, es, n, shape, dt, name):
        return [self.sb(es, shape, dt, name + str(i)) for i in range(n)]


import os
VERBOSE = bool(os.environ.get("KVERBOSE"))


def build_program(depth_run=DEPTH, dbg=False, stop_after=None, limit=None):
    nc = bass.Bass("TRN2", target_bir_lowering=False)
    es = ExitStack()
    with es:
        _build(nc, es, depth_run, dbg, stop_after, limit)
    return nc


def _dram_in(nc, name, shape, dt=F32):
    return nc.dram_tensor(name, list(shape), dt, kind="ExternalInput").ap()


def _build(nc, es, depth_run, dbg, stop_after, limit=None):
    x_in = _dram_in(nc, "x", [NLAT, D])
    ctx_in = _dram_in(nc, "ctx", [NCTX, D])
    c_in = _dram_in(nc, "c", [D])
    cctx_in = _dram_in(nc, "c_ctx", [D])
    mod_w = _dram_in(nc, "mod_w", [DEPTH, D, 6 * D])
    mod_b = _dram_in(nc, "mod_b", [DEPTH, 6 * D])
    norm1_g = _dram_in(nc, "norm1_g", [DEPTH, D])
    norm2_g = _dram_in(nc, "norm2_g", [DEPTH, D])
    ev_w_in = _dram_in(nc, "ev_w_in", [2, D, EV_IN])
    ev_sink = _dram_in(nc, "ev_sink", [2, 8])
    ev_sgu_norm_g = _dram_in(nc, "ev_sgu_norm_g", [2, 512])
    ev_sgu_w = _dram_in(nc, "ev_sgu_w", [2, 4, 128, 128])
    ev_sgu_b = _dram_in(nc, "ev_sgu_b", [2, 4, 128])
    ev_w_out = _dram_in(nc, "ev_w_out", [2, D, D])
    od_w_in = _dram_in(nc, "od_w_in", [2, D, OD_IN])
    od_q_norm_g = _dram_in(nc, "od_q_norm_g", [2, 384])
    od_w_uq = _dram_in(nc, "od_w_uq", [2, 384, 768])
    od_kv_norm_g = _dram_in(nc, "od_kv_norm_g", [2, 256])
    od_w_ukv = _dram_in(nc, "od_w_ukv", [2, 256, 1024])
    od_conv_w = _dram_in(nc, "od_conv_w", [2, 3, 512])
    od_w_out = _dram_in(nc, "od_w_out", [2, D, D])
    router_w = _dram_in(nc, "router_w", [DEPTH, D, NE])
    exp_w1 = _dram_in(nc, "exp_w1", [DEPTH, NE, D, D])
    exp_w3 = _dram_in(nc, "exp_w3", [DEPTH, NE, D, D])
    exp_w2 = _dram_in(nc, "exp_w2", [DEPTH, NE, D, D])
    final_g = _dram_in(nc, "final_g", [D])
    ropeA = _dram_in(nc, "ropeA", [NTOK, 64])
    ropeC = _dram_in(nc, "ropeC", [NTOK, 32])
    out = nc.dram_tensor("out", [NLAT, D], F32, kind="ExternalOutput").ap()

    X = nc.dram_tensor("Xres", [NTOK, D], F32, kind="Internal").ap()
    MROW = nc.dram_tensor("mrow", [DEPTH, 2, 6 * D], F32, kind="Internal").ap()
    H2 = nc.dram_tensor("h2", [NTOK, D], BF16, kind="Internal").ap()
    AFF = nc.dram_tensor("aff", [NTOK, NE], F32, kind="Internal").ap()
    CS = nc.dram_tensor("cs", [NE * NTL, 128], F32, kind="Internal").ap()
    CT = nc.dram_tensor("ct", [NE * NTL], F32, kind="Internal").ap()
    QTD = nc.dram_tensor("qtd", [8, 96, NTOK], BF16, kind="Internal").ap()
    OTD = nc.dram_tensor("otd", [8, 64, NTOK], BF16, kind="Internal").ap()

    S = Sched(nc, es)
    S.limit = limit
    kb = KB(nc, S, es)
    Xb = [Buf("X%d" % t) for t in range(NT)]

    def I(eng, name, reads, writes, *a, **kw):
        S.op(eng, name, reads, writes, *a, **kw)

    def DMA(eng, reads, writes, out, in_):
        S.op(eng, "dma_start", reads, writes, out=out, in_=in_, dma=True)

    def MM(reads, writes, out, lhsT, rhs, start, stop):
        S.op("pe", "matmul", reads, writes, out, lhsT=lhsT, rhs=rhs, start=start, stop=stop)

    def TR(reads, writes, out, in_, ident):
        S.op("pe", "transpose", reads, writes, out=out, in_=in_, identity=ident)

    def ACT(reads, writes, out, in_, func, **kw):
        S.op("act", "activation", reads, writes, out=out, in_=in_, func=func, **kw)

    def TT(eng, reads, writes, out, in0, in1, op):
        S.op(eng, "tensor_tensor", reads, writes, out=out, in0=in0, in1=in1, op=op)

    def TS(eng, reads, writes, out, in0, s1, s2, op0, op1=None, **kw):
        if op1 is None:
            S.op(eng, "tensor_scalar", reads, writes, out=out, in0=in0, scalar1=s1, scalar2=None, op0=op0, **kw)
        else:
            S.op(eng, "tensor_scalar", reads, writes, out=out, in0=in0, scalar1=s1, scalar2=s2, op0=op0, op1=op1, **kw)

    def STT(eng, reads, writes, out, in0, scalar, in1, op0, op1):
        S.op(eng, "scalar_tensor_tensor", reads, writes, out=out, in0=in0, scalar=scalar, in1=in1, op0=op0, op1=op1)

    def RSQ(dst, dst_ap, src, src_ap):
        ACT([src.b], [dst.b], dst_ap, src_ap, AF.Sqrt, bias=epsb.t[0:dst_ap.shape[0], 0:1])
        I("dve", "reciprocal", [dst.b], [dst.b], out=dst_ap, in_=dst_ap)

    def IDMA(reads, writes, out, out_off, in_, in_off, bound, add=False):
        kw = dict(out=out, out_offset=out_off, in_=in_, in_offset=in_off, bounds_check=bound, oob_is_err=False)
        if add:
            kw["compute_op"] = ALU.add
        S.op("pool", "indirect_dma_start", reads, writes, dma=True, **kw)

    PB = [TB(es.enter_context(nc.psum_tensor("pb%d" % i, [128, 512], F32)), "pb%d" % i) for i in range(8)]

    def pbf(i):
        return PB[i].t[:].bitcast(BF16)

    identb = kb.sb(es, [128, 128], BF16, "identb")
    identf = kb.sb(es, [128, 128], F32, "identf")
    onesf = kb.sb(es, [128, 128], F32, "onesf")
    maskneg = kb.sb(es, [128, 2, 512], BF16, "maskneg")
    e65 = kb.sb(es, [1, 65], BF16, "e65")
    epsb = kb.sb(es, [128, 1], F32, "epsb")
    ropeA_sb = kb.sb(es, [128, NT, 64], F32, "ropeA")
    ropeC_sb = kb.sb(es, [128, NT, 32], F32, "ropeC")

    for idt in (identb, identf):
        I("pool", "memset", [], [idt.b], idt.t[:], 0.0)
        I("pool", "affine_select", [idt.b], [idt.b], out=idt.t[:], in_=idt.t[:], pattern=[[-1, 128]],
          compare_op=ALU.not_equal, fill=1.0, base=0, channel_multiplier=1)
    I("pool", "memset", [], [onesf.b], onesf.t[:], 1.0)
    I("pool", "memset", [], [maskneg.b], maskneg.t[:], 0.0)
    I("pool", "affine_select", [maskneg.b], [maskneg.b], out=maskneg.t[:, 0, :], in_=maskneg.t[:, 0, :],
      pattern=[[0, 4], [-1, 128]], compare_op=ALU.is_ge, fill=-30000.0, base=0, channel_multiplier=1)
    I("pool", "affine_select", [maskneg.b], [maskneg.b], out=maskneg.t[:, 1, :], in_=maskneg.t[:, 1, :],
      pattern=[[0, 4], [1, 128]], compare_op=ALU.is_ge, fill=-30000.0, base=0, channel_multiplier=-1)
    I("pool", "memset", [], [epsb.b], epsb.t[:], EPS)
    I("pool", "memset", [], [e65.b], e65.t[:], 0.0)
    I("pool", "memset", [e65.b], [e65.b], e65.t[0:1, 64:65], 1.0)
    DMA("sp", [], [ropeA_sb.b], ropeA_sb.t[:], ropeA.rearrange("(t p) f -> p t f", p=128))
    DMA("sp", [], [ropeC_sb.b], ropeC_sb.t[:], ropeC.rearrange("(t p) f -> p t f", p=128))
    for t in range(NT):
        src = x_in[t * 128:(t + 1) * 128, :] if t < NTL else ctx_in[(t - NTL) * 128:(t - NTL + 1) * 128, :]
        DMA("sp", [], [Xb[t]], X[t * 128:(t + 1) * 128, :], src)

    with ExitStack() as ms:
        sc = kb.sb(ms, [128, 8, 2], F32, "sc")
        modb2 = kb.sb(ms, [2, 6 * D], F32, "modb2")
        mrow_sb = kb.sb(ms, [2, 6 * D], F32, "mrow_sb")
        mw = kb.ring(ms, 2, [128, 8, 512], F32, "mw")
        DMA("sp", [], [sc.b], sc.t[:, :, 0:1], c_in.rearrange("(k p o) -> p k o", p=128, o=1))
        DMA("sp", [], [sc.b], sc.t[:, :, 1:2], cctx_in.rearrange("(k p o) -> p k o", p=128, o=1))
        ACT([sc.b], [sc.b], sc.t[:], sc.t[:], AF.Silu)
        i = 0
        for l in range(depth_run):
            for r in range(2):
                DMA("sp", [], [modb2.b], modb2.t[r:r + 1, :], mod_b[l:l + 1, :])
            for n in range(12):
                m = mw[i % 2]
                i += 1
                DMA("sp", [], [m.b], m.t[:], mod_w[l].rearrange("(k p) c -> p k c", p=128)[:, :, n * 512:(n + 1) * 512])
                for k in range(8):
                    MM([sc.b, m.b], [PB[0].b], PB[0].t[0:2, :], sc.t[:, k, :], m.t[:, k, :], k == 0, k == 7)
                TT("dve", [PB[0].b, modb2.b], [mrow_sb.b], mrow_sb.t[:, n * 512:(n + 1) * 512], PB[0].t[0:2, :],
                   modb2.t[:, n * 512:(n + 1) * 512], ALU.add)
            DMA("sp", [mrow_sb.b], [], MROW[l], mrow_sb.t[:])
        S.barrier()

    def bcast_row(dst, row_ap, bufs_w):
        DMA("sp", [], bufs_w, dst, row_ap.partition_broadcast(128))

    def load_mod(ms, l, j0, gvec, want_g=True, want_as=True):
        res = []
        for r in range(2):
            A = Sh = G = None
            if want_as:
                A = kb.sb(ms, [128, D], F32, "A")
                Sh = kb.sb(ms, [128, D], F32, "Sh")
            if want_g:
                G = kb.sb(ms, [128, D], F32, "G")
            res.append((A, Sh, G))
        with ExitStack() as tmpst:
            gb = None
            if want_as:
                gb = kb.sb(tmpst, [128, D], F32, "gb")
                bcast_row(gb.t[:], gvec, [gb.b])
            for r in range(2):
                A, Sh, G = res[r]
                if want_as:
                    bcast_row(Sh.t[:], MROW[l, r, j0 * D:(j0 + 1) * D], [Sh.b])
                    bcast_row(A.t[:], MROW[l, r, (j0 + 1) * D:(j0 + 2) * D], [A.b])
                    STT("dve", [A.b, gb.b], [A.b], A.t[:], A.t[:], 1.0, gb.t[:], ALU.add, ALU.mult)
                if want_g:
                    bcast_row(G.t[:], MROW[l, r, (j0 + 2) * D:(j0 + 3) * D], [G.b])
            if want_as:
                S.barrier()
        return res

    def norm_mod(xt, A, Sh, hout, ms_t, rstd_t, junk, tmp):
        ACT([xt.b], [ms_t.b, hout.b], hout.t[:], xt.t[:], AF.Square, scale=1.0 / 32.0, accum_out=ms_t.t[:, 0:1])
        RSQ(rstd_t, rstd_t.t[:, 0:1], ms_t, ms_t.t[:, 0:1])
        STT("dve", [xt.b, rstd_t.b, A.b], [tmp.b], tmp.t[:], xt.t[:], rstd_t.t[:, 0:1], A.t[:], ALU.mult, ALU.mult)
        TT("dve", [tmp.b, Sh.b], [hout.b], hout.t[:], tmp.t[:], Sh.t[:], ALU.add)

    def transpose8(h, hT, bank):
        pv = pbf(bank)
        for k in range(8):
            TR([h.b, identb.b], [PB[bank].b], pv[:, k * 128:(k + 1) * 128], h.t[:, k * 128:(k + 1) * 128], identb.t[:])
        ACT([PB[bank].b], [hT.b], hT.t[:].rearrange("p k t -> p (k t)"), pv[:, :], AF.Copy)

    def wout_residual(t, ON, cT, Wo_o, Wo_c, G, x, yn, o, ybanks=(0, 1)):
        for half in range(2):
            cs_ = slice(half * 512, (half + 1) * 512)
            yb = ybanks[half]
            for h in range(8):
                MM([ON.b, Wo_o.b], [PB[yb].b], PB[yb].t[:, :], ON.t[:, h, :], Wo_o.t[:, h, cs_], h == 0, False)
            for k in range(4):
                MM([cT.b, Wo_c.b], [PB[yb].b], PB[yb].t[:, :], cT.t[:, k, :], Wo_c.t[:, k, cs_], False, k == 3)
            TT("dve", [PB[yb].b, G.b], [o.b], o.t[:, cs_], PB[yb].t[:, :], G.t[:, cs_], ALU.mult)
        TT("dve", [o.b, x.b], [o.b], o.t[:], o.t[:], x.t[:], ALU.add)
        DMA("sp", [o.b], [Xb[t]], X[t * 128:(t + 1) * 128, :], o.t[:])

    def attn_norm(pbank, bbank, rs, Osb, on_out, nq, on_buf):
        I("dve", "reciprocal", [PB[pbank].b], [rs.b], out=rs.t[64:65, 0:nq], in_=PB[pbank].t[64:65, 0:nq])
        ACT([PB[pbank].b], [Osb.b], Osb.t[:, 0:nq], PB[pbank].t[0:64, 0:nq], AF.Copy)
        MM([onesf.b, rs.b], [PB[bbank].b], PB[bbank].t[0:64, 0:nq], onesf.t[64:65, 0:64], rs.t[64:65, 0:nq], True, True)
        TT("dve", [Osb.b, PB[bbank].b], [on_buf], on_out, Osb.t[:, 0:nq], PB[bbank].t[0:64, 0:nq], ALU.mult)

    def even_mixer(l, need_ctx):
        i = l // 2
        with ExitStack() as ms:
            Win = kb.sb(ms, [128, 8, EV_IN], BF16, "Win")
            Wo_o = kb.sb(ms, [64, 8, D], BF16, "Wo_o")
            Wo_s = kb.sb(ms, [128, 4, D], BF16, "Wo_s")
            Wsn = kb.sb(ms, [128, 4, 128], BF16, "Wsn")
            WsT = kb.sb(ms, [128, 4, 128], BF16, "WsT")
            bsT = kb.sb(ms, [128, 4], F32, "bsT")
            sgug = kb.sb(ms, [128, 512], F32, "sgug")
            snk = kb.sb(ms, [1, 8], F32, "snk")
            esink = kb.sb(ms, [1, 8, 128], BF16, "esink")
            DMA("pool", [], [Win.b], Win.t[:], ev_w_in[i].rearrange("(k p) c -> p k c", p=128))
            DMA("pool", [], [Wo_o.b], Wo_o.t[:], ev_w_out[i, 0:512, :].rearrange("(h d) c -> d h c", d=64))
            DMA("pool", [], [Wo_s.b], Wo_s.t[:], ev_w_out[i, 512:1024, :].rearrange("(k p) c -> p k c", p=128))
            DMA("pool", [], [Wsn.b], Wsn.t[:], ev_sgu_w[i].rearrange("g p q -> p g q"))
            DMA("sp", [], [bsT.b], bsT.t[:].rearrange("p (g o) -> p g o", o=1), ev_sgu_b[i].rearrange("g (p o) -> p g o", o=1))
            bcast_row(sgug.t[:], ev_sgu_norm_g[i], [sgug.b])
            DMA("sp", [], [snk.b], snk.t[:], ev_sink[i:i + 1, :])
            ACT([snk.b], [snk.b], snk.t[:], snk.t[:], AF.Exp)
            I("dve", "tensor_copy", [snk.b], [esink.b], out=esink.t[:],
              in_=snk.t[:].rearrange("o (h u) -> o h u", u=1).to_broadcast([1, 8, 128]))
            pv = pbf(4)
            for g in range(4):
                TR([Wsn.b, identb.b], [PB[4].b], pv[:, g * 128:(g + 1) * 128], Wsn.t[:, g, :], identb.t[:])
            ACT([PB[4].b], [WsT.b], WsT.t[:].rearrange("p g q -> p (g q)"), pv[:, 0:512], AF.Copy)
            mods = load_mod(ms, l, 0, norm1_g[l])

            xt = kb.ring(ms, 4, [128, D], F32, "xt")
            junk = None
            tmp_r = kb.ring(ms, 2, [128, D], F32, "tmp")
            hb_r = kb.ring(ms, 2, [128, D], BF16, "hb")
            hT_r = kb.ring(ms, 2, [128, 8, 128], BF16, "hT")
            msr_r = kb.ring(ms, 2, [128, 1], F32, "msr")
            rstd_r = kb.ring(ms, 2, [128, 1], F32, "rstd")
            zs_r = kb.ring(ms, 2, [128, 12, 64], F32, "zs")
            zr_r = kb.ring(ms, 2, [128, 12, 64], BF16, "zr")
            r1_r = kb.ring(ms, 1, [128, 12, 32], F32, "r1")
            r2_r = kb.ring(ms, 1, [128, 12, 32], F32, "r2")
            r3_r = kb.ring(ms, 1, [128, 12, 32], F32, "r3")
            r4_r = kb.ring(ms, 1, [128, 12, 32], F32, "r4")
            KT = kb.ring(ms, 5, [64, 2, 128], BF16, "KT")
            VA = kb.ring(ms, 5, [128, 2, 65], BF16, "VA")
            KTc = kb.ring(ms, 2, [64, 2, 128], BF16, "KTc")
            VAc = kb.ring(ms, 2, [128, 2, 65], BF16, "VAc")
            QT = kb.ring(ms, 3, [64, 8, 128], BF16, "QT")
            sT = kb.ring(ms, 3, [128, 4, 128], BF16, "sT")
            gu_r = kb.ring(ms, 2, [128, 2, 512], F32, "gu")
            g1_r = kb.ring(ms, 2, [128, 2, 512], F32, "g1")
            bst_r = kb.ring(ms, 2, [128, 6], F32, "bst")
            mv_r = kb.ring(ms, 2, [128, 2], F32, "mv")
            lrs_r = kb.ring(ms, 2, [128, 1], F32, "lrs")
            vn_r = kb.ring(ms, 2, [128, 512], F32, "vn")
            vnb_r = kb.ring(ms, 2, [128, 512], BF16, "vnb")
            sb_r = kb.ring(ms, 2, [128, 512], BF16, "s")
            PT = kb.ring(ms, 3, [128, 512], BF16, "PT")
            rs_r = kb.ring(ms, 2, [65, 512], F32, "rs")
            Osb_r = kb.ring(ms, 2, [64, 512], F32, "Osb")
            ON_r = kb.ring(ms, 2, [64, 8, 128], BF16, "ON")
            xo_r = kb.ring(ms, 2, [128, D], F32, "xo")
            for v in VA + VAc:
                I("pool", "memset", [], [v.b], v.t[:], 1.0)

            def kv_of(t):
                return (KTc[t - NTL], VAc[t - NTL]) if t >= NTL else (KT[t % 5], VA[t % 5])

            def phaseA(t):
                if VERBOSE:
                    print("even phaseA", t, "op", S.nops)
                r = 1 if t >= NTL else 0
                A, Sh, G = mods[r]
                x = xt[t % 4]
                i2 = t % 2
                tmp, hb, hT, msr, rstd, zs, zr = tmp_r[i2], hb_r[i2], hT_r[i2], msr_r[i2], rstd_r[i2], zs_r[i2], zr_r[i2]
                r1, r2, r3, r4 = r1_r[0], r2_r[0], r3_r[0], r4_r[0]
                gu, g1, bst, mv, lrs, vn, vnb, sb_ = gu_r[i2], g1_r[i2], bst_r[i2], mv_r[i2], lrs_r[i2], vn_r[i2], vnb_r[i2], sb_r[i2]
                DMA("sp", [Xb[t]], [x.b], x.t[:], X[t * 128:(t + 1) * 128, :])
                norm_mod(x, A, Sh, hb, msr, rstd, junk, tmp)
                transpose8(hb, hT, 4)
                for (bank, c0, c1) in ((0, 0, 512), (1, 512, 768)):
                    for k in range(8):
                        MM([hT.b, Win.b], [PB[bank].b], PB[bank].t[:, 0:c1 - c0], hT.t[:, k, :], Win.t[:, k, c0:c1], k == 0, k == 7)
                zsf = zs.t[:].rearrange("p h d -> p (h d)")
                ACT([PB[0].b], [zs.b], zsf[:, 0:512], PB[0].t[:, :], AF.Copy)
                ACT([PB[1].b], [zs.b], zsf[:, 512:768], PB[1].t[:, 0:256], AF.Copy)
                for (bank, c0) in ((2, 768), (3, 1280)):
                    for k in range(8):
                        MM([hT.b, Win.b], [PB[bank].b], PB[bank].t[:, :], hT.t[:, k, :], Win.t[:, k, c0:c0 + 512], k == 0, k == 7)
                cos = ropeA_sb.t[:, t, 0:32].rearrange("p (o f) -> p o f", o=1).to_broadcast([128, 12, 32])
                sin = ropeA_sb.t[:, t, 32:64].rearrange("p (o f) -> p o f", o=1).to_broadcast([128, 12, 32])
                x1 = zs.t[:, :, 0:32]
                x2 = zs.t[:, :, 32:64]
                TT("dve", [zs.b, ropeA_sb.b], [r1.b], r1.t[:], x1, cos, ALU.mult)
                TT("pool", [zs.b, ropeA_sb.b], [r2.b], r2.t[:], x2, sin, ALU.mult)
                TT("dve", [zs.b, ropeA_sb.b], [r3.b], r3.t[:], x2, cos, ALU.mult)
                TT("pool", [zs.b, ropeA_sb.b], [r4.b], r4.t[:], x1, sin, ALU.mult)
                TT("dve", [r1.b, r2.b], [zr.b], zr.t[:, :, 0:32], r1.t[:], r2.t[:], ALU.subtract)
                TT("pool", [r3.b, r4.b], [zr.b], zr.t[:, :, 32:64], r3.t[:], r4.t[:], ALU.add)
                kt, va = kv_of(t)
                ACT([zs.b], [va.b], va.t[:, :, 0:64], zs.t[:, 2:4, :], AF.Copy)
                pq = pbf(4)
                pk = pbf(5)
                for h in range(8):
                    TR([zr.b, identb.b], [PB[4].b], pq[0:64, h * 128:(h + 1) * 128], zr.t[:, 4 + h, :], identb.t[:])
                for g in range(2):
                    TR([zr.b, identb.b], [PB[5].b], pk[0:64, g * 128:(g + 1) * 128], zr.t[:, g, :], identb.t[:])
                q = QT[t % 3]
                ACT([PB[4].b], [q.b], q.t[:].rearrange("p h t -> p (h t)"), pq[0:64, :], AF.Copy)
                ACT([PB[5].b], [kt.b], kt.t[:].rearrange("p h t -> p (h t)"), pk[0:64, 0:256], AF.Copy)
                for hh, bank in ((0, 2), (1, 3)):
                    ACT([PB[bank].b], [gu.b], gu.t[:, hh, :], PB[bank].t[:, :], AF.Copy)
                guf = gu.t[:].rearrange("p a f -> p (a f)")
                g1f = g1.t[:].rearrange("p a f -> p (a f)")
                TT("pool", [gu.b], [g1.b], g1f, guf, guf, ALU.mult)
                TS("dve", [g1.b], [g1.b], g1f, g1f, 0.044715, 1.0, ALU.mult, ALU.add)
                TT("pool", [g1.b, gu.b], [g1.b], g1f, g1f, guf, ALU.mult)
                ACT([g1.b], [g1.b], g1f, g1f, AF.Tanh, scale=0.7978845608)
                TS("dve", [g1.b], [g1.b], g1f, g1f, 0.5, 0.5, ALU.mult, ALU.add)
                TT("dve", [g1.b, gu.b], [gu.b], guf, guf, g1f, ALU.mult)
                I("dve", "bn_stats", [gu.b], [bst.b], out=bst.t[:], in_=gu.t[:, 1, :])
                I("dve", "bn_aggr", [bst.b], [mv.b], out=mv.t[:], in_=bst.t[:])
                RSQ(lrs, lrs.t[:], mv, mv.t[:, 1:2])
                TS("dve", [gu.b, mv.b, lrs.b], [vn.b], vn.t[:], gu.t[:, 1, :], mv.t[:, 0:1], lrs.t[:, 0:1], ALU.subtract, ALU.mult)
                TT("pool", [vn.b, sgug.b], [vnb.b], vnb.t[:], vn.t[:], sgug.t[:], ALU.mult)
                for g in range(4):
                    MM([WsT.b, vnb.b], [PB[3].b], PB[3].t[:, g * 128:(g + 1) * 128], WsT.t[:, g, :], vnb.t[:, g * 128:(g + 1) * 128], True, True)
                for g in range(4):
                    STT("dve", [PB[3].b, bsT.b, gu.b], [sb_.b], sb_.t[:, g * 128:(g + 1) * 128], PB[3].t[:, g * 128:(g + 1) * 128],
                        bsT.t[:, g:g + 1], gu.t[:, 0, g * 128:(g + 1) * 128], ALU.add, ALU.mult)
                pv2 = pbf(4)
                for k in range(4):
                    TR([sb_.b, identb.b], [PB[4].b], pv2[:, k * 128:(k + 1) * 128], sb_.t[:, k * 128:(k + 1) * 128], identb.t[:])
                st_ = sT[t % 3]
                ACT([PB[4].b], [st_.b], st_.t[:].rearrange("p k t -> p (k t)"), pv2[:, 0:512], AF.Copy)

            def phaseB(t):
                if VERBOSE:
                    print("even phaseB", t, "op", S.nops)
                r = 1 if t >= NTL else 0
                A, Sh, G = mods[r]
                x = xt[t % 4]
                q = QT[t % 3]
                i2 = t % 2
                tmp, ON, xo = tmp_r[i2], ON_r[i2], xo_r[i2]
                if t >= NTL:
                    keys = [(NTL, None), (NTL + 1, None)]
                else:
                    keys = []
                    if t > 0:
                        keys.append((t - 1, 0))
                    keys.append((t, None))
                    if t < NTL - 1:
                        keys.append((t + 1, 1))
                    keys += [(NTL, None), (NTL + 1, None)]
                npt = 0
                for g in range(2):
                    for j, (kt_i, mk) in enumerate(keys):
                        kt, va = kv_of(kt_i)
                        bank = 5 + (npt % 2)
                        pt_ = PT[npt % 3]
                        npt += 1
                        MM([kt.b, q.b], [PB[bank].b], PB[bank].t[:, :], kt.t[:, g, :],
                           q.t[:, 4 * g:4 * g + 4, :].rearrange("p h t -> p (h t)"), True, mk is None)
                        if mk is not None:
                            MM([identb.b, maskneg.b], [PB[bank].b], PB[bank].t[:, :], identb.t[:], maskneg.t[:, mk, :], False, True)
                        ACT([PB[bank].b], [pt_.b], pt_.t[:], PB[bank].t[:, :], AF.Exp, scale=0.125)
                        MM([va.b, pt_.b], [PB[7].b], PB[7].t[0:65, :], va.t[:, g, :], pt_.t[:], j == 0, False)
                    MM([e65.b, esink.b], [PB[7].b], PB[7].t[0:65, :], e65.t[0:1, :],
                       esink.t[0:1, 4 * g:4 * g + 4, :].rearrange("p h t -> p (h t)"), False, True)
                    attn_norm(7, 2, rs_r[g], Osb_r[g], ON.t[:, 4 * g:4 * g + 4, :].rearrange("p h t -> p (h t)"), 512, ON.b)
                wout_residual(t, ON, sT[t % 3], Wo_o, Wo_s, G, x, tmp, xo)

            phaseA(NTL)
            phaseA(NTL + 1)
            if need_ctx:
                phaseB(NTL)
                phaseB(NTL + 1)
            phaseA(0)
            for t in range(NTL):
                if t + 1 < NTL:
                    phaseA(t + 1)
                phaseB(t)
            S.barrier()

    def odd_mixer(l, need_ctx):
        i = l // 2
        SCALE = 96.0 ** -0.5
        with ExitStack() as ks:
            KT = kb.sb(ks, [96, 8, NTOK], BF16, "KTm")
            VA = kb.sb(ks, [128, NT, 8, 65], BF16, "VAm")
            I("pool", "memset", [], [VA.b], VA.t[:].rearrange("p t h d -> p (t h d)"), 1.0)
            with ExitStack() as ms:
                Wa = kb.sb(ms, [128, 8, 672], BF16, "Wa")
                Wukv = kb.sb(ms, [128, 2, 1024], BF16, "Wukv")
                Wuq = kb.sb(ms, [128, 3, 768], BF16, "Wuq")
                gkv = kb.sb(ms, [128, 256], F32, "gkv")
                gq = kb.sb(ms, [128, 384], F32, "gq")
                DMA("pool", [], [Wa.b], Wa.t[:], od_w_in[i].rearrange("(k p) c -> p k c", p=128)[:, :, 0:672])
                DMA("pool", [], [Wukv.b], Wukv.t[:], od_w_ukv[i].rearrange("(k p) c -> p k c", p=128))
                DMA("pool", [], [Wuq.b], Wuq.t[:], od_w_uq[i].rearrange("(k p) c -> p k c", p=128))
                bcast_row(gkv.t[:], od_kv_norm_g[i], [gkv.b])
                bcast_row(gq.t[:], od_q_norm_g[i], [gq.b])
                mods = load_mod(ms, l, 0, norm1_g[l], want_g=False)
                xt = kb.ring(ms, 2, [128, D], F32, "oxt")
                junk = kb.sb(ms, [128, D], BF16, "ojunk")
                tmp = kb.sb(ms, [128, D], F32, "otmp")
                hb = kb.sb(ms, [128, D], BF16, "ohb")
                hT = kb.sb(ms, [128, 8, 128], BF16, "ohT")
                msr = kb.sb(ms, [128, 1], F32, "omsr")
                rstd = kb.sb(ms, [128, 1], F32, "orstd")
                zc = kb.sb(ms, [128, 672], F32, "zc")
                ms2 = kb.sb(ms, [128, 2], F32, "ms2")
                rs2 = kb.sb(ms, [128, 2], F32, "rs2")
                cn = kb.sb(ms, [128, 640], BF16, "cn")
                cT = kb.sb(ms, [128, 5, 128], BF16, "cT")
                qs = kb.sb(ms, [128, 9, 96], F32, "qs")
                qr = kb.sb(ms, [128, 9, 96], BF16, "qr")
                kf = kb.sb(ms, [128, 8, 96], BF16, "kf")
                p1 = kb.sb(ms, [128, 9, 16], F32, "p1")
                p2 = kb.sb(ms, [128, 9, 16], F32, "p2")
                p3 = kb.sb(ms, [128, 9, 16], F32, "p3")
                p4 = kb.sb(ms, [128, 9, 16], F32, "p4")
                qT = kb.ring(ms, 2, [96, 8, 128], BF16, "qT")
                I("pool", "memset", [], [qs.b], qs.t[:].rearrange("p h d -> p (h d)"), 0.0)
                for t in range(NT):
                    r = 1 if t >= NTL else 0
                    A, Sh, _ = mods[r]
                    x = xt[t % 2]
                    DMA("sp", [Xb[t]], [x.b], x.t[:], X[t * 128:(t + 1) * 128, :])
                    norm_mod(x, A, Sh, hb, msr, rstd, junk, tmp)
                    transpose8(hb, hT, 4)
                    for (bank, c0, c1) in ((0, 0, 512), (1, 512, 672)):
                        for k in range(8):
                            MM([hT.b, Wa.b], [PB[bank].b], PB[bank].t[:, 0:c1 - c0], hT.t[:, k, :], Wa.t[:, k, c0:c1], k == 0, k == 7)
                    ACT([PB[0].b], [zc.b], zc.t[:, 0:512], PB[0].t[:, :], AF.Copy)
                    ACT([PB[1].b], [zc.b], zc.t[:, 512:672], PB[1].t[:, 0:160], AF.Copy)
                    ACT([zc.b], [junk.b, ms2.b], junk.t[:, 0:256], zc.t[:, 0:256], AF.Square, scale=1.0 / 16.0, accum_out=ms2.t[:, 0:1])
                    ACT([zc.b], [junk.b, ms2.b], junk.t[:, 256:640], zc.t[:, 288:672], AF.Square, scale=384.0 ** -0.5, accum_out=ms2.t[:, 1:2])
                    RSQ(rs2, rs2.t[:], ms2, ms2.t[:])
                    STT("dve", [zc.b, rs2.b, gkv.b], [cn.b], cn.t[:, 0:256], zc.t[:, 0:256], rs2.t[:, 0:1], gkv.t[:], ALU.mult, ALU.mult)
                    STT("dve", [zc.b, rs2.b, gq.b], [cn.b], cn.t[:, 256:640], zc.t[:, 288:672], rs2.t[:, 1:2], gq.t[:], ALU.mult, ALU.mult)
                    pv = pbf(4)
                    for k in range(5):
                        TR([cn.b, identb.b], [PB[4].b], pv[:, k * 128:(k + 1) * 128], cn.t[:, k * 128:(k + 1) * 128], identb.t[:])
                    ACT([PB[4].b], [cT.b], cT.t[:].rearrange("p k t -> p (k t)"), pv[:, 0:640], AF.Copy)
                    for half in range(2):
                        for kc in range(2):
                            MM([cT.b, Wukv.b], [PB[2 + half].b], PB[2 + half].t[:, :], cT.t[:, kc, :],
                               Wukv.t[:, kc, half * 512:(half + 1) * 512], kc == 0, kc == 1)
                    for (bank, c0, c1) in ((5, 0, 512), (6, 512, 768)):
                        for kc in range(3):
                            MM([cT.b, Wuq.b], [PB[bank].b], PB[bank].t[:, 0:c1 - c0], cT.t[:, 2 + kc, :], Wuq.t[:, kc, c0:c1], kc == 0, kc == 2)
                    for half in range(2):
                        pvv = PB[2 + half].t[:, :].rearrange("p (h d) -> p h d", d=128)
                        ACT([PB[2 + half].b], [kf.b], kf.t[:, 4 * half:4 * half + 4, 0:64], pvv[:, :, 0:64], AF.Copy)
                        ACT([PB[2 + half].b], [VA.b], VA.t[:, t, 4 * half:4 * half + 4, 0:64], pvv[:, :, 64:128], AF.Copy)
                    qsf = qs.t[:].rearrange("p h d -> p (h d)")
                    ACT([PB[5].b], [qs.b], qsf[:, 0:512], PB[5].t[:, :], AF.Copy)
                    ACT([PB[6].b], [qs.b], qsf[:, 512:768], PB[6].t[:, 0:256], AF.Copy)
                    I("dve", "tensor_copy", [zc.b], [qs.b], out=qs.t[:, 8, 64:96], in_=zc.t[:, 256:288])
                    cos = ropeC_sb.t[:, t, 0:16].rearrange("p (o f) -> p o f", o=1).to_broadcast([128, 9, 16])
                    sin = ropeC_sb.t[:, t, 16:32].rearrange("p (o f) -> p o f", o=1).to_broadcast([128, 9, 16])
                    x1 = qs.t[:, :, 64:80]
                    x2 = qs.t[:, :, 80:96]
                    TT("dve", [qs.b, ropeC_sb.b], [p1.b], p1.t[:], x1, cos, ALU.mult)
                    TT("pool", [qs.b, ropeC_sb.b], [p2.b], p2.t[:], x2, sin, ALU.mult)
                    TT("dve", [qs.b, ropeC_sb.b], [p3.b], p3.t[:], x2, cos, ALU.mult)
                    TT("pool", [qs.b, ropeC_sb.b], [p4.b], p4.t[:], x1, sin, ALU.mult)
                    TT("dve", [p1.b, p2.b], [qr.b], qr.t[:, :, 64:80], p1.t[:], p2.t[:], ALU.subtract)
                    TT("pool", [p3.b, p4.b], [qr.b], qr.t[:, :, 80:96], p3.t[:], p4.t[:], ALU.add)
                    ACT([qs.b], [qr.b], qr.t[:, 0:8, 0:64], qs.t[:, 0:8, 0:64], AF.Copy)
                    I("dve", "tensor_copy", [qr.b], [kf.b], out=kf.t[:, :, 64:96],
                      in_=qr.t[:, 8:9, 64:96].to_broadcast([128, 8, 32]))
                    pk = pbf(4)
                    pq = pbf(7)
                    for h in range(8):
                        TR([kf.b, identb.b], [PB[4].b], pk[0:96, h * 128:(h + 1) * 128], kf.t[:, h, :], identb.t[:])
                    for h in range(8):
                        TR([qr.b, identb.b], [PB[7].b], pq[0:96, h * 128:(h + 1) * 128], qr.t[:, h, :], identb.t[:])
                    ACT([PB[4].b], [KT.b], KT.t[:, :, t * 128:(t + 1) * 128], pk[0:96, :].rearrange("p (h t) -> p h t", t=128), AF.Copy)
                    q_ = qT[t % 2]
                    ACT([PB[7].b], [q_.b], q_.t[:].rearrange("p h t -> p (h t)"), pq[0:96, :], AF.Copy)
                    DMA("sp", [q_.b], [], QTD[:, :, t * 128:(t + 1) * 128].rearrange("h d t -> d h t"), q_.t[:])
                S.barrier()
            with ExitStack() as ms:
                qt = kb.ring(ms, 2, [96, 8, 512], BF16, "qt")
                PT = kb.ring(ms, 6, [128, 512], BF16, "PTm")
                rs = kb.sb(ms, [65, 512], F32, "rsm")
                Osb = kb.sb(ms, [64, 512], F32, "Osbm")
                on = kb.ring(ms, 2, [64, 512], BF16, "onm")
                blocks = [(b * 512, 512, list(range(NT))) for b in range(NLAT // 512)]
                if need_ctx:
                    blocks.append((NLAT, NCTX, [NTL, NTL + 1]))
                npt = 0
                nh = 0
                for bi, (q0, nq, keys) in enumerate(blocks):
                    q = qt[bi % 2]
                    DMA("sp", [], [q.b], q.t[:, :, 0:nq], QTD[:, :, q0:q0 + nq].rearrange("h d t -> d h t"))
                    for h in range(8):
                        pbank = 7 if nh % 2 == 0 else 3
                        bbank = 2 if nh % 2 == 0 else 1
                        o_ = on[nh % 2]
                        nh += 1
                        for j, kt_i in enumerate(keys):
                            bank = (5, 6, 0, 4)[npt % 4]
                            pt_ = PT[npt % 6]
                            npt += 1
                            MM([KT.b, q.b], [PB[bank].b], PB[bank].t[:, 0:nq], KT.t[:, h, kt_i * 128:(kt_i + 1) * 128], q.t[:, h, 0:nq], True, True)
                            ACT([PB[bank].b], [pt_.b], pt_.t[:, 0:nq], PB[bank].t[:, 0:nq], AF.Exp, scale=SCALE)
                            MM([VA.b, pt_.b], [PB[pbank].b], PB[pbank].t[0:65, 0:nq], VA.t[:, kt_i, h, :], pt_.t[:, 0:nq], j == 0, j == len(keys) - 1)
                        attn_norm(pbank, bbank, rs, Osb, o_.t[:, 0:nq], nq, o_.b)
                        DMA("sp", [o_.b], [], OTD[h, :, q0:q0 + nq], o_.t[:, 0:nq])
                S.barrier()
        with ExitStack() as ms:
            Wb = kb.sb(ms, [128, 8, 1536], BF16, "Wb")
            Wo_o = kb.sb(ms, [64, 8, D], BF16, "Wo_o2")
            Wo_c = kb.sb(ms, [128, 4, D], BF16, "Wo_c")
            cw = kb.sb(ms, [128, 3, 4], F32, "cw")
            cwb = kb.sb(ms, [128, 3, 4, 128], F32, "cwb")
            DMA("pool", [], [Wb.b], Wb.t[:], od_w_in[i].rearrange("(k p) c -> p k c", p=128)[:, :, 672:2208])
            DMA("pool", [], [Wo_o.b], Wo_o.t[:], od_w_out[i, 0:512, :].rearrange("(h d) c -> d h c", d=64))
            DMA("pool", [], [Wo_c.b], Wo_c.t[:], od_w_out[i, 512:1024, :].rearrange("(k p) c -> p k c", p=128))
            DMA("sp", [], [cw.b], cw.t[:].rearrange("p j (c o) -> p j c o", o=1), od_conv_w[i].rearrange("j (c p o) -> p j c o", p=128, o=1))
            I("dve", "tensor_copy", [cw.b], [cwb.b], out=cwb.t[:].rearrange("p j c t -> p (j c) t"),
              in_=cw.t[:].rearrange("p j (c o) -> p (j c) o", o=1).to_broadcast([128, 12, 128]))
            mods = load_mod(ms, l, 0, norm1_g[l])
            xt = kb.ring(ms, 3, [128, D], F32, "cxt")
            junk = kb.sb(ms, [128, D], BF16, "cjunk")
            tmp_r = kb.ring(ms, 2, [128, D], F32, "ctmp")
            hb_r = kb.ring(ms, 2, [128, D], BF16, "chb")
            hT_r = kb.ring(ms, 2, [128, 8, 128], BF16, "chT")
            msr_r = kb.ring(ms, 2, [128, 1], F32, "cmsr")
            rstd_r = kb.ring(ms, 2, [128, 1], F32, "crstd")
            UT = kb.ring(ms, 3, [128, 4, 130], F32, "UT")
            BT = kb.ring(ms, 3, [128, 4, 128], F32, "BT")
            Cs_r = kb.ring(ms, 2, [128, 4, 128], F32, "Cs")
            c1_r = kb.ring(ms, 2, [128, 4, 128], F32, "c1")
            c2_r = kb.ring(ms, 2, [128, 4, 128], F32, "c2")
            cTt_r = kb.ring(ms, 2, [128, 4, 128], BF16, "cTt")
            ot = kb.ring(ms, 3, [64, 8, 128], BF16, "ot")
            xo_r = kb.ring(ms, 2, [128, D], F32, "cxo")
            last = NT if need_ctx else NTL

            def seg_first(t):
                return t == 0 or t == NTL

            def seg_last(t):
                return t == NTL - 1 or t == NT - 1

            def phaseA(t):
                r = 1 if t >= NTL else 0
                A, Sh, G = mods[r]
                x = xt[t % 3]
                u = UT[t % 3]
                i2 = t % 2
                tmp, hb, hT, msr, rstd, Cs = tmp_r[i2], hb_r[i2], hT_r[i2], msr_r[i2], rstd_r[i2], Cs_r[i2]
                DMA("sp", [Xb[t]], [x.b], x.t[:], X[t * 128:(t + 1) * 128, :])
                norm_mod(x, A, Sh, hb, msr, rstd, junk, tmp)
                transpose8(hb, hT, 6)
                zb = 3 * i2
                for part in range(3):
                    for c in range(4):
                        c0 = part * 512 + c * 128
                        for k in range(8):
                            MM([hT.b, Wb.b], [PB[zb + part].b], PB[zb + part].t[:, c * 128:(c + 1) * 128], Wb.t[:, k, c0:c0 + 128], hT.t[:, k, :], k == 0, k == 7)
                b_ = BT[t % 3]
                ACT([PB[zb].b], [b_.b], b_.t[:].rearrange("p c t -> p (c t)"), PB[zb].t[:, :], AF.Copy)
                ACT([PB[zb + 1].b], [Cs.b], Cs.t[:].rearrange("p c t -> p (c t)"), PB[zb + 1].t[:, :], AF.Copy)
                TT("dve", [Cs.b, PB[zb + 2].b], [u.b], u.t[:, :, 1:129], Cs.t[:], PB[zb + 2].t[:, :].rearrange("p (c t) -> p c t", t=128), ALU.mult)
                if seg_first(t):
                    I("pool", "memset", [u.b], [u.b], u.t[:, :, 0:1], 0.0)
                else:
                    up = UT[(t - 1) % 3]
                    I("pool", "tensor_copy", [u.b, up.b], [u.b], out=u.t[:, :, 0:1], in_=up.t[:, :, 128:129])
                    I("pool", "tensor_copy", [u.b, up.b], [up.b], out=up.t[:, :, 129:130], in_=u.t[:, :, 1:2])
                if seg_last(t):
                    I("pool", "memset", [u.b], [u.b], u.t[:, :, 129:130], 0.0)

            def phaseB(t):
                r = 1 if t >= NTL else 0
                A, Sh, G = mods[r]
                x = xt[t % 3]
                u = UT[t % 3]
                b_ = BT[t % 3]
                i2 = t % 2
                tmp, c1, c2, cTt, xo = tmp_r[i2], c1_r[i2], c2_r[i2], cTt_r[i2], xo_r[i2]
                TT("dve", [u.b, cwb.b], [c1.b], c1.t[:], u.t[:, :, 0:128], cwb.t[:, 0], ALU.mult)
                TT("pool", [u.b, cwb.b], [c2.b], c2.t[:], u.t[:, :, 1:129], cwb.t[:, 1], ALU.mult)
                TT("dve", [c1.b, c2.b], [c1.b], c1.t[:], c1.t[:], c2.t[:], ALU.add)
                TT("pool", [u.b, cwb.b], [c2.b], c2.t[:], u.t[:, :, 2:130], cwb.t[:, 2], ALU.mult)
                TT("dve", [c1.b, c2.b], [c1.b], c1.t[:], c1.t[:], c2.t[:], ALU.add)
                TT("dve", [c1.b, b_.b], [cTt.b], cTt.t[:], c1.t[:], b_.t[:], ALU.mult)
                o_ = ot[t % 3]
                DMA("sp", [], [o_.b], o_.t[:], OTD[:, :, t * 128:(t + 1) * 128].rearrange("h d t -> d h t"))
                wout_residual(t, o_, cTt, Wo_o, Wo_c, G, x, tmp, xo, ybanks=(7, 7))

            order = list(range(NTL)) + ([NTL, NTL + 1] if need_ctx else [])
            for idx, t in enumerate(order):
                if idx == 0 or seg_first(t):
                    phaseA(t)
                if not seg_last(t):
                    phaseA(t + 1)
                phaseB(t)
            S.barrier()

    def ffn(l, need_ctx):
        with ExitStack() as ms:
            mods = load_mod(ms, l, 3, norm2_g[l], want_as=False)
            G2 = [mods[0][2], mods[1][2]]
            affT = kb.sb(ms, [128, NT, NE], F32, "affT")
            affE = kb.sb(ms, [NE, NTOK], F32, "affE")
            tok_i = kb.sb(ms, [128, 2, 4, NE], I32, "tok_i")
            ntiles = NT if need_ctx else NTL
            with ExitStack() as f1:
                modsA = load_mod(f1, l, 3, norm2_g[l], want_g=False)
                Wr = kb.sb(f1, [128, 8, NE], BF16, "Wr")
                DMA("pool", [], [Wr.b], Wr.t[:], router_w[l].rearrange("(k p) c -> p k c", p=128))
                xt = kb.ring(f1, 3, [128, D], F32, "fxt")
                junk = kb.sb(f1, [128, D], BF16, "fjunk")
                tmp_r = kb.ring(f1, 2, [128, D], F32, "ftmp")
                hb = kb.ring(f1, 3, [128, D], BF16, "fhb")
                hT_r = kb.ring(f1, 2, [128, 8, 128], BF16, "fhT")
                msr_r = kb.ring(f1, 2, [128, 1], F32, "fmsr")
                rstd_r = kb.ring(f1, 2, [128, 1], F32, "frstd")
                mx_r = kb.ring(f1, 2, [128, 1], F32, "mx")
                ssum_r = kb.ring(f1, 2, [128, 1], F32, "ssum")
                ee_r = kb.ring(f1, 2, [128, NE], F32, "ee")
                for t in range(ntiles):
                    r = 1 if t >= NTL else 0
                    A, Sh, _ = modsA[r]
                    x = xt[t % 3]
                    h = hb[t % 3]
                    i2 = t % 2
                    tmp, hT, msr, rstd, mx, ssum, ee = tmp_r[i2], hT_r[i2], msr_r[i2], rstd_r[i2], mx_r[i2], ssum_r[i2], ee_r[i2]
                    DMA("sp", [Xb[t]], [x.b], x.t[:], X[t * 128:(t + 1) * 128, :])
                    norm_mod(x, A, Sh, h, msr, rstd, junk, tmp)
                    DMA("sp", [h.b], [], H2[t * 128:(t + 1) * 128, :], h.t[:])
                    transpose8(h, hT, 4 + i2)
                    for k in range(8):
                        MM([hT.b, Wr.b], [PB[i2].b], PB[i2].t[:, 0:NE], hT.t[:, k, :], Wr.t[:, k, :], k == 0, k == 7)
                    I("dve", "tensor_reduce", [PB[i2].b], [mx.b], out=mx.t[:], in_=PB[i2].t[:, 0:NE], axis=AX.X, op=ALU.max)
                    TS("dve", [mx.b], [mx.b], mx.t[:], mx.t[:], -1.0, None, ALU.mult)
                    ACT([PB[i2].b, mx.b], [ee.b, ssum.b], ee.t[:], PB[i2].t[:, 0:NE], AF.Exp, bias=mx.t[:, 0:1], accum_out=ssum.t[:, 0:1])
                    I("dve", "reciprocal", [ssum.b], [ssum.b], out=ssum.t[:], in_=ssum.t[:])
                    TS("dve", [ee.b, ssum.b], [affT.b], affT.t[:, t, :], ee.t[:], ssum.t[:, 0:1], None, ALU.mult)
                    TR([affT.b, identf.b], [PB[2 + i2].b], PB[2 + i2].t[0:NE, 0:128], affT.t[:, t, :], identf.t[:])
                    ACT([PB[2 + i2].b], [affE.b], affE.t[:, t * 128:(t + 1) * 128], PB[2 + i2].t[0:NE, 0:128], AF.Copy)
                DMA("sp", [affT.b], [], AFF[0:ntiles * 128, :].rearrange("(t p) e -> p t e", p=128), affT.t[:, 0:ntiles, :])
                S.barrier()
            segs = [(0, NLAT, 512)]
            if need_ctx:
                segs.append((NLAT, NCTX, 32))
            with ExitStack() as ws:
                lo = kb.sb(ws, [NE, 1], F32, "lo")
                mid = kb.sb(ws, [NE, 1], F32, "mid")
                cnt = kb.sb(ws, [NE, 1], F32, "cnt")
                stp = kb.sb(ws, [NE, 1], F32, "stp")
                mask = kb.sb(ws, [NE, NLAT], F32, "mask")
                csum = kb.sb(ws, [NE, NLAT], F32, "csum")
                ctl = kb.sb(ws, [NE, NTL], F32, "ctl")
                ctb = kb.sb(ws, [128, NE * NTL], F32, "ctb")
                le = kb.sb(ws, [128, NE, 128], F32, "le")
                Tf = kb.sb(ws, [128, NE], F32, "Tf")
                ridx = kb.sb(ws, [128, NE], F32, "ridx")
                ridx_i = kb.sb(ws, [128, NE], I32, "ridx_i")
                Gall = kb.sb(ws, [128, NE, 128], F32, "Gall")
                loc = kb.sb(ws, [128, NE], F32, "loc")
                cslot = kb.sb(ws, [128, 4], F32, "cslot")
                e32 = kb.sb(ws, [128, NE], F32, "e32")
                I("pool", "iota", [], [cslot.b], cslot.t[:], pattern=[[128, 4]], base=0, channel_multiplier=1,
                  allow_small_or_imprecise_dtypes=True)
                csb = Buf("CS")
                ctbuf = Buf("CT")
                Gb = [Buf("G%d" % ex) for ex in range(NE)]
                for si, (n0, n, cap) in enumerate(segs):
                    ntl = n // 128
                    nst = (cap + 127) // 128
                    I("pool", "iota", [], [e32.b], e32.t[:], pattern=[[ntl, NE]], base=0, channel_multiplier=0,
                      allow_small_or_imprecise_dtypes=True)
                    av = affE.t[:, n0:n0 + n]
                    I("dve", "memset", [], [lo.b], lo.t[:], 0.0)
                    for it in range(23):
                        w = 2.0 ** (-(it + 1))
                        TS("dve", [lo.b], [mid.b], mid.t[:], lo.t[:], w, None, ALU.add)
                        TS("dve", [affE.b, mid.b], [mask.b, cnt.b], mask.t[:, 0:n], av, mid.t[:, 0:1], 0.0, ALU.is_ge, ALU.add,
                           accum_out=cnt.t[:, 0:1])
                        TS("dve", [cnt.b], [stp.b], stp.t[:], cnt.t[:], float(cap) - 0.5, w, ALU.is_ge, ALU.mult)
                        TT("dve", [lo.b, stp.b], [lo.b], lo.t[:], lo.t[:], stp.t[:], ALU.add)
                    TS("dve", [affE.b, lo.b], [mask.b], mask.t[:, 0:n], av, lo.t[:, 0:1], None, ALU.is_ge)
                    I("dve", "tensor_tensor_scan", [mask.b], [csum.b], out=csum.t[:, 0:n], data0=mask.t[:, 0:n], data1=mask.t[:, 0:n],
                      initial=0.0, op0=ALU.add, op1=ALU.max)
                    DMA("sp", [csum.b], [csb], CS[0:NE * ntl, :].rearrange("(e t) p -> e t p", t=ntl),
                        csum.t[:, 0:n].rearrange("e (t p) -> e t p", p=128))
                    I("dve", "tensor_copy", [csum.b], [ctl.b], out=ctl.t[:, 0:ntl],
                      in_=csum.t[:, 0:n].rearrange("e (t p) -> e t p", p=128)[:, :, 127])
                    DMA("sp", [ctl.b], [ctbuf], CT[0:NE * ntl].rearrange("(e t) -> e t", t=ntl), ctl.t[:, 0:ntl])
                    DMA("sp", [ctbuf], [ctb.b], ctb.t[:, 0:NE * ntl], CT[0:NE * ntl].partition_broadcast(128))
                    ctbv = ctb.t[:, 0:NE * ntl].rearrange("p (e t) -> p e t", t=ntl)
                    for j in range(nst):
                        TS("dve", [ctb.b, cslot.b], [le.b], le.t[:, :, 0:ntl], ctbv, cslot.t[:, j:j + 1], None, ALU.is_le)
                        I("dve", "tensor_reduce", [le.b], [Tf.b], out=Tf.t[:], in_=le.t[:, :, 0:ntl], axis=AX.X, op=ALU.add)
                        TT("dve", [Tf.b, e32.b], [ridx.b], ridx.t[:], Tf.t[:], e32.t[:], ALU.add)
                        I("dve", "tensor_copy", [ridx.b], [ridx_i.b], out=ridx_i.t[:], in_=ridx.t[:])
                        for ex in range(NE):
                            IDMA([ridx_i.b, csb], [Gb[ex]], Gall.t[:, ex, :], None, CS[:, :],
                                 bass.IndirectOffsetOnAxis(ap=ridx_i.t[:, ex:ex + 1], axis=0), NE * ntl - 1)
                        TS("dve", Gb + [cslot.b], [le.b], le.t[:], Gall.t[:], cslot.t[:, j:j + 1], None, ALU.is_le)
                        I("dve", "tensor_reduce", [le.b], [loc.b], out=loc.t[:], in_=le.t[:], axis=AX.X, op=ALU.add)
                        STT("dve", [Tf.b, loc.b], [loc.b], loc.t[:], Tf.t[:], 128.0, loc.t[:], ALU.mult, ALU.add)
                        if n0:
                            TS("dve", [loc.b], [loc.b], loc.t[:], loc.t[:], float(n0), None, ALU.add)
                        I("dve", "tensor_copy", [loc.b], [tok_i.b], out=tok_i.t[:, si, j, :], in_=loc.t[:])
                S.barrier()
            with ExitStack() as ws:
                W1 = kb.ring(ws, 2, [128, 8, D], BF16, "W1")
                W3 = kb.ring(ws, 2, [128, 8, D], BF16, "W3")
                W2 = kb.ring(ws, 2, [128, 8, D], BF16, "W2")
                xg = kb.ring(ws, 4, [128, D], BF16, "xg")
                ag = kb.ring(ws, 5, [128, NE], F32, "ag")
                xsT_r = kb.ring(ws, 2, [128, 8, 512], BF16, "xsT")
                gT_r = kb.ring(ws, 2, [128, 8, 512], BF16, "gT")
                sa = kb.ring(ws, 3, [128, 512], F32, "sa")
                yo = kb.ring(ws, 3, [128, D], F32, "yo")
                prev_marks = []
                nxg = 0
                nyo = 0
                nsa = 0
                nb = 0
                for ex in range(NE):
                    w1, w3, w2 = W1[ex % 2], W3[ex % 2], W2[ex % 2]
                    for (wt, src) in ((w1, exp_w1), (w3, exp_w3), (w2, exp_w2)):
                        DMA("pool", [], [wt.b], wt.t[:], src[l, ex].rearrange("(k p) c -> p k c", p=128))
                    for si, (n0, n, cap) in enumerate(segs):
                        nst = (cap + 127) // 128
                        sp_ = min(cap, 128)
                        ns = cap
                        xsT = xsT_r[nb % 2]
                        gT = gT_r[nb % 2]
                        nb += 1
                        ags = []
                        for j in range(nst):
                            g_ = xg[nxg % 4]
                            a_ = ag[nxg % 5]
                            nxg += 1
                            ags.append(a_)
                            off = bass.IndirectOffsetOnAxis(ap=tok_i.t[0:sp_, si, j, ex:ex + 1], axis=0)
                            IDMA([tok_i.b], [g_.b], g_.t[0:sp_, :], None, H2[:, :], off, NTOK - 1)
                            IDMA([tok_i.b], [a_.b], a_.t[0:sp_, :], None, AFF[:, :], off, NTOK - 1)
                            pv = pbf(4)
                            for k in range(8):
                                TR([g_.b, identb.b], [PB[4].b], pv[:, k * 128:k * 128 + sp_], g_.t[0:sp_, k * 128:(k + 1) * 128],
                                   identb.t[0:sp_, 0:sp_])
                            ACT([PB[4].b], [xsT.b], xsT.t[:, :, j * 128:j * 128 + sp_],
                                pv[:, :].rearrange("p (k t) -> p k t", t=128)[:, :, 0:sp_], AF.Copy)
                        for f in range(8):
                            fs = slice(f * 128, (f + 1) * 128)
                            ba, bb = (0, 1) if f % 2 == 0 else (2, 3)
                            for k in range(8):
                                MM([w1.b, xsT.b], [PB[ba].b], PB[ba].t[:, 0:ns], w1.t[:, k, fs], xsT.t[:, k, 0:ns], k == 0, k == 7)
                            for k in range(8):
                                MM([w3.b, xsT.b], [PB[bb].b], PB[bb].t[:, 0:ns], w3.t[:, k, fs], xsT.t[:, k, 0:ns], k == 0, k == 7)
                            s_ = sa[nsa % 3]
                            nsa += 1
                            ACT([PB[ba].b], [s_.b], s_.t[:, 0:ns], PB[ba].t[:, 0:ns], AF.Silu)
                            TT("dve", [s_.b, PB[bb].b], [gT.b], gT.t[:, f, 0:ns], s_.t[:, 0:ns], PB[bb].t[:, 0:ns], ALU.mult)
                        marks = []
                        for j in range(nst):
                            y_ = yo[nyo % 3]
                            nyo += 1
                            a_ = ags[j]
                            for half in range(2):
                                cs_ = slice(half * 512, (half + 1) * 512)
                                bank = 5 + half
                                for f in range(8):
                                    MM([gT.b, w2.b], [PB[bank].b], PB[bank].t[0:sp_, :], gT.t[:, f, j * 128:j * 128 + sp_], w2.t[:, f, cs_],
                                       f == 0, f == 7)
                                STT("dve", [PB[bank].b, a_.b, G2[si].b], [y_.b], y_.t[0:sp_, cs_], PB[bank].t[0:sp_, :],
                                    a_.t[0:sp_, ex:ex + 1], G2[si].t[0:sp_, cs_], ALU.mult, ALU.mult)
                            mk = Buf("mk")
                            marks.append(mk)
                            off = bass.IndirectOffsetOnAxis(ap=tok_i.t[0:sp_, si, j, ex:ex + 1], axis=0)
                            IDMA([y_.b, tok_i.b] + prev_marks, [mk], X[:, :], off, y_.t[0:sp_, :], None, NTOK - 1, add=True)
                        prev_marks = marks
                S.barrier()

    def final_norm():
        with ExitStack() as ms:
            fg = kb.sb(ms, [128, D], F32, "fg")
            bcast_row(fg.t[:], final_g, [fg.b])
            xt = kb.ring(ms, 2, [128, D], F32, "nxt")
            junk = kb.sb(ms, [128, D], BF16, "njunk")
            ot = kb.ring(ms, 2, [128, D], F32, "not")
            msr = kb.sb(ms, [128, 1], F32, "nmsr")
            rstd = kb.sb(ms, [128, 1], F32, "nrstd")
            for t in range(NTL):
                x = xt[t % 2]
                o = ot[t % 2]
                DMA("sp", [], [x.b], x.t[:], X[t * 128:(t + 1) * 128, :])
                ACT([x.b], [junk.b, msr.b], junk.t[:], x.t[:], AF.Square, scale=1.0 / 32.0, accum_out=msr.t[:, 0:1])
                RSQ(rstd, rstd.t[:, 0:1], msr, msr.t[:, 0:1])
                STT("dve", [x.b, rstd.b, fg.b], [o.b], o.t[:], x.t[:], rstd.t[:, 0:1], fg.t[:], ALU.mult, ALU.mult)
                DMA("sp", [o.b], [], out[t * 128:(t + 1) * 128, :], o.t[:])
            S.barrier()

    S.barrier()
    for l in range(depth_run):
        need_ctx = l < DEPTH - 1
        if l % 2 == 0:
            even_mixer(l, need_ctx)
        else:
            odd_mixer(l, need_ctx)
        if stop_after == (l, "mixer"):
            break
        ffn(l, need_ctx)
    print("[kernel] ops before final:", S.nops)
    S.force = True
    if dbg:
        xd = nc.dram_tensor("xdbg", [NTOK, D], F32, kind="ExternalOutput").ap()
        DMA("sp", [], [], xd[:, :], X[:, :])
    final_norm()
    S.barrier()
    with nc.allow_non_contiguous_dma(reason="tiny strided parameter vectors"):
        with nc.Block() as block:
            S.emit(block)


def _rope_tables():
    def tab(rot_dim):
        rows = NLAT // 64
        row = np.repeat(np.arange(rows, dtype=np.float32), 64)
        col = np.tile(np.arange(64, dtype=np.float32), rows)
        n_freq = rot_dim // 4
        inv = (np.float32(10000.0) ** (-np.arange(n_freq, dtype=np.float32) / np.float32(n_freq))).astype(np.float32)
        ang = np.concatenate([row[:, None] * inv[None, :], col[:, None] * inv[None, :]], axis=-1).astype(np.float32)
        t = np.concatenate([np.cos(ang), np.sin(ang)], axis=-1).astype(np.float32)
        c = np.concatenate([np.ones((NCTX, rot_dim // 2), np.float32), np.zeros((NCTX, rot_dim // 2), np.float32)], axis=-1)
        return np.ascontiguousarray(np.concatenate([t, c], axis=0))
    return tab(64), tab(32)


_SHARED = ["c_ctx", "mod_w", "mod_b", "norm1_g", "norm2_g", "ev_w_in", "ev_sink", "ev_sgu_norm_g", "ev_sgu_w", "ev_sgu_b",
           "ev_w_out", "od_w_in", "od_q_norm_g", "od_w_uq", "od_kv_norm_g", "od_w_ukv", "od_conv_w", "od_w_out", "router_w",
           "exp_w1", "exp_w3", "exp_w2", "final_g"]


def make_in_maps(inputs, cores):
    ropeA, ropeC = _rope_tables()
    shared = {k: np.ascontiguousarray(np.asarray(inputs[k], dtype=np.float32)) for k in _SHARED}
    maps = []
    for b in cores:
        m = dict(shared)
        m["x"] = np.ascontiguousarray(np.asarray(inputs["x"][b], dtype=np.float32))
        m["ctx"] = np.ascontiguousarray(np.asarray(inputs["ctx"][b], dtype=np.float32))
        m["c"] = np.ascontiguousarray(np.asarray(inputs["c"][b], dtype=np.float32))
        m["ropeA"] = ropeA
        m["ropeC"] = ropeC
        maps.append(m)
    return maps


def kernel(**inputs):
    nc = build_program()
    maps = make_in_maps(inputs, list(range(8)))
    res = run_bass_kernel_spmd(nc, maps, core_ids=list(range(8)))
    return np.stack([np.asarray(r["out"], dtype=np.float32) for r in res.results], axis=0)
```

```python
import os
import numpy as np
from contextlib import ExitStack
import concourse.bass as bass
import concourse.mybir as mybir
from concourse.bass_utils import run_bass_kernel_spmd

F32 = mybir.dt.float32
BF16 = mybir.dt.bfloat16
I32 = mybir.dt.int32
AF = mybir.ActivationFunctionType
ALU = mybir.AluOpType
AX = mybir.AxisListType

D = 1024
NLAT = 4096
NCTX = 256
NTOK = NLAT + NCTX
NT = NTOK // 128
NTL = NLAT // 128
DEPTH = 4
NE = 16
EPS = 1e-6
EV_IN = 1792
OD_IN = 2208


class Buf:
    __slots__ = ("name", "w", "r")

    def __init__(self, name=""):
        self.name = name
        self.w = None
        self.r = []


class TB:
    def __init__(self, t, name):
        self.t = t
        self.b = Buf(name)


def _free(ap):
    n = 1
    for d in tuple(ap.shape)[1:]:
        n *= int(d)
    return n


_DTB = {F32: 4, BF16: 2, I32: 4}


class Sched:
    ENGS = ("pe", "act", "dve", "pool", "sp")

    def __init__(self, nc, es, ndma=None):
        ndma = ndma or {"sp": 14, "pool": 14}
        self.nc = nc
        self.sems = []
        self.esem = {}
        for e in self.ENGS:
            self.esem[e] = len(self.sems)
            self.sems.append(es.enter_context(nc.semaphore("s_" + e)))
        self.dpool = {}
        for e, n in ndma.items():
            self.dpool[e] = []
            for i in range(n):
                self.dpool[e].append(len(self.sems))
                self.sems.append(es.enter_context(nc.semaphore("d_%s%d" % (e, i))))
        self.nodes = []
        self.tbl = {}
        self.segs = [0]
        self.nops = 0
        self.limit = None
        self.force = False
        self.ninst = 0
        self.reorder = True
        self.noreorder = set(int(x) for x in os.environ.get("KNOREORDER", "").split(",") if x)
        self.cur_seg = -1

    def _cost(self, eng, name, a, kw, dma):
        try:
            if dma:
                nb = None
                for ap in (kw["out"], kw["in_"]):
                    n_ = 1
                    for d in tuple(ap.shape):
                        n_ *= int(d)
                    n_ *= _DTB.get(ap.dtype, 4)
                    nb = n_ if nb is None else min(nb, n_)
                if "indirect" in name:
                    return 1.0, 3.0 + nb / 150e3
                occ = 0.6 if eng == "pool" else 0.08
                return occ, 2.2 + nb / 380e3
            if eng == "pe":
                if name == "matmul":
                    rhs = kw["rhs"]
                    n = max(_free(rhs), 64)
                    if rhs.dtype == F32:
                        n *= 4
                    t = 0.03 + n / 2400.0
                else:
                    t = 0.09
                return t, t + 0.05
            out = kw.get("out", a[0] if a else None)
            f = _free(out) if out is not None else 64
            if eng == "act":
                t = 0.22 + f / 1200.0
            elif eng == "dve":
                t = 0.08 + f / 960.0
            else:
                t = 0.12 + f / 450.0
            return t, t + 0.05
        except Exception:
            return 0.3, 0.4

    def op(self, eng, name, reads, writes, *a, dma=False, **kw):
        self.nops += 1
        if self.limit is not None and self.nops > self.limit and not self.force:
            return None
        nid = len(self.nodes)
        deps = set()
        for b in reads:
            if b.w is not None:
                deps.add(b.w)
        for b in writes:
            if b.w is not None:
                deps.add(b.w)
            deps.update(b.r)
        occ, lat = self._cost(eng, name, a, kw, dma)
        tbl = None
        if eng == "act" and name == "activation":
            f = kw.get("func")
            if f == AF.Sqrt:
                tbl = "sqrt"
            elif f in (AF.Exp, AF.Tanh):
                tbl = "exp"
            elif f == AF.Silu:
                tbl = "silu"
            elif f == AF.Sigmoid:
                tbl = "sig"
        self.tbl[nid] = tbl
        self.nodes.append((eng, (name, a, kw), dma, deps, occ, lat))
        for b in reads:
            b.r.append(nid)
        for b in writes:
            b.w = nid
            b.r = []
        return nid

    def barrier(self):
        if self.segs[-1] != len(self.nodes):
            self.segs.append(len(self.nodes))

    def _schedule(self, lo, hi):
        import heapq
        nodes = self.nodes
        if not self.reorder or self.cur_seg in self.noreorder:
            return list(range(lo, hi))
        indeg = {}
        succ = {}
        ready_t = {}
        for n in range(lo, hi):
            c = 0
            for d in nodes[n][3]:
                if d >= lo:
                    c += 1
                    succ.setdefault(d, []).append(n)
            indeg[n] = c
            ready_t[n] = 0.0
        pend = {e: [] for e in self.ENGS}
        avail = {e: [] for e in self.ENGS}
        for n in range(lo, hi):
            if indeg[n] == 0:
                heapq.heappush(pend[nodes[n][0]], (0.0, n))
        t_eng = {e: 0.0 for e in self.ENGS}
        dma_free = 0.0
        order = []
        cur_tbl = None
        tblmap = self.tbl
        table_aware = not os.environ.get("KNOTBL")
        total = hi - lo
        while len(order) < total:
            best = None
            for e in self.ENGS:
                pe_, av = pend[e], avail[e]
                te = t_eng[e]
                while pe_ and pe_[0][0] <= te:
                    heapq.heappush(av, heapq.heappop(pe_)[1])
                if av:
                    pick = av[0]
                    if e == "act" and table_aware and len(av) > 1:
                        t0_ = tblmap.get(pick)
                        if t0_ is not None and t0_ != cur_tbl:
                            best_alt = None
                            for x_ in av:
                                tx = tblmap.get(x_)
                                if (tx is None or tx == cur_tbl) and x_ < pick + 400 and (best_alt is None or x_ < best_alt):
                                    best_alt = x_
                            if best_alt is not None:
                                pick = best_alt
                    cand = (te, pick, e, True)
                elif pe_:
                    cand = (pe_[0][0], pe_[0][1], e, False)
                else:
                    continue
                if best is None or cand[:2] < best[:2]:
                    best = cand
            st, n, e, from_av = best
            if from_av:
                if avail[e][0] == n:
                    heapq.heappop(avail[e])
                else:
                    avail[e].remove(n)
                    heapq.heapify(avail[e])
            else:
                heapq.heappop(pend[e])
            eng, fn, dma, deps, occ, lat = nodes[n]
            if e == "act":
                tn = tblmap.get(n)
                if tn is not None and tn != cur_tbl:
                    cur_tbl = tn
                    st += 1.3
            if dma:
                s2 = max(st, dma_free)
                fin = s2 + lat
                dma_free = s2 + max(lat - (3.0 if "indirect" in fn[0] else 2.2), 0.0)
            else:
                fin = st + lat
            t_eng[e] = st + occ
            order.append(n)
            if os.environ.get("KTRACE") and self.cur_seg == int(os.environ["KTRACE"]) and len(order) % 150 == 0:
                print("SIM %5d %-5s %-22s st=%8.2f fin=%8.2f" % (n, e, fn[0], st, fin))
            for m in succ.get(n, ()):
                hop = 0.05 if nodes[m][0] == e and not dma else 0.25
                rt = fin + hop
                if rt > ready_t[m]:
                    ready_t[m] = rt
                indeg[m] -= 1
                if indeg[m] == 0:
                    heapq.heappush(pend[nodes[m][0]], (ready_t[m], m))
        self.est_time = getattr(self, "est_time", 0.0) + max(t_eng.values())
        if os.environ.get("KVERBOSE"):
            print("[kernel] seg", self.cur_seg, lo, hi, "est", round(max(t_eng.values()), 1), {e: round(v, 1) for e, v in t_eng.items()})
        return order

    def emit(self, block):
        nodes = self.nodes
        if self.segs[-1] != len(nodes):
            self.segs.append(len(nodes))
        prog = {e: [] for e in self.ENGS}
        cnt = {e: 0 for e in self.ENGS}
        known = {e: {} for e in self.ENGS}
        dval = {k: 0 for e in self.dpool for k in self.dpool[e]}
        dnext = {e: 0 for e in self.dpool}
        ev = {}
        pe_own = self.esem["pe"]
        for si in range(len(self.segs) - 1):
            lo, hi = self.segs[si], self.segs[si + 1]
            self.cur_seg = si
            if os.environ.get("KVERBOSE"):
                print("[kernel] segment", si, lo, hi)
            for n in self._schedule(lo, hi):
                eng, fn, dma, deps, occ, lat = nodes[n]
                kn = known[eng]
                waits = {}
                for d in deps:
                    k, v = ev[d]
                    if eng == "pe" and k == pe_own:
                        continue
                    if kn.get(k, 0) >= v:
                        continue
                    if waits.get(k, 0) < v:
                        waits[k] = v
                if dma:
                    pool = self.dpool[eng]
                    k = pool[dnext[eng] % len(pool)]
                    dnext[eng] += 1
                    if dval[k] > 0 and kn.get(k, 0) < dval[k] and waits.get(k, 0) < dval[k]:
                        waits[k] = dval[k]
                    dval[k] += 16
                    ev[n] = (k, dval[k])
                    inc = 16
                else:
                    k = self.esem[eng]
                    cnt[eng] += 1
                    ev[n] = (k, cnt[eng])
                    inc = 1
                for k2, v in waits.items():
                    kn[k2] = v
                prog[eng].append((list(waits.items()), fn, k, inc))
                self.ninst += 1 + len(waits)
            targets = [(self.esem[e], cnt[e]) for e in self.ENGS if cnt[e] > 0]
            targets += [(k, v) for k, v in dval.items() if v > 0]
            for e in self.ENGS:
                waits = []
                for k, v in targets:
                    if known[e].get(k, 0) >= v:
                        continue
                    known[e][k] = v
                    waits.append((k, v))
                if waits:
                    prog[e].append((waits, None, None, 0))
                    self.ninst += len(waits)
        sems = self.sems

        def mk(p):
            def body(engine):
                regcache = {}
                for waits, fn, k, inc in p:
                    for k2, v in waits:
                        engine.wait_ge(sems[k2], v)
                    if fn is not None:
                        kw = fn[2]
                        if "bounds_check" in kw:
                            bv = kw["bounds_check"]
                            if bv not in regcache:
                                regcache[bv] = engine.to_reg(bv)
                            kw = dict(kw)
                            kw["bounds_check"] = regcache[bv]
                        fn = (fn[0], fn[1], kw)
                        try:
                            ins = getattr(engine, fn[0])(*fn[1], **fn[2])
                        except Exception:
                            print("[kernel] failed to emit", fn[0], fn[1], fn[2])
                            raise
                        ins.then_inc(sems[k], inc)
            return body

        block.tensor(mk(prog["pe"]))
        block.scalar(mk(prog["act"]))
        block.vector(mk(prog["dve"]))
        block.gpsimd(mk(prog["pool"]))
        block.sync(mk(prog["sp"]))
        print("[kernel] instructions incl. waits:", self.ninst, "est us:", round(getattr(self, "est_time", 0.0), 1))


class KB:
    def __init__(self, nc, S, es):
        self.nc = nc
        self.S = S
        self.es = es
        self.n = 0

    def sb(self, es, shape, dt, name=None):
        self.n += 1
        name = (name or "t") + "_%d" % self.n
        return TB(es.enter_context(self.nc.sbuf_tensor(name, list(shape), dt)), name)

    def ring(self, es, n, shape, dt, name):
        return [self.sb(es, shape, dt, name + str(i)) for i in range(n)]


import os
VERBOSE = bool(os.environ.get("KVERBOSE"))


def build_program(depth_run=DEPTH, dbg=False, stop_after=None, limit=None):
    nc = bass.Bass("TRN2", target_bir_lowering=False)
    es = ExitStack()
    with es:
        _build(nc, es, depth_run, dbg, stop_after, limit)
    return nc


def _dram_in(nc, name, shape, dt=F32):
    return nc.dram_tensor(name, list(shape), dt, kind="ExternalInput").ap()


def _build(nc, es, depth_run, dbg, stop_after, limit=None):
    x_in = _dram_in(nc, "x", [NLAT, D])
    ctx_in = _dram_in(nc, "ctx", [NCTX, D])
    c_in = _dram_in(nc, "c", [D])
    cctx_in = _dram_in(nc, "c_ctx", [D])
    mod_w = _dram_in(nc, "mod_w", [DEPTH, D, 6 * D])
    mod_b = _dram_in(nc, "mod_b", [DEPTH, 6 * D])
    norm1_g = _dram_in(nc, "norm1_g", [DEPTH, D])
    norm2_g = _dram_in(nc, "norm2_g", [DEPTH, D])
    ev_w_in = _dram_in(nc, "ev_w_in", [2, D, EV_IN])
    ev_sink = _dram_in(nc, "ev_sink", [2, 8])
    ev_sgu_norm_g = _dram_in(nc, "ev_sgu_norm_g", [2, 512])
    ev_sgu_w = _dram_in(nc, "ev_sgu_w", [2, 4, 128, 128])
    ev_sgu_b = _dram_in(nc, "ev_sgu_b", [2, 4, 128])
    ev_w_out = _dram_in(nc, "ev_w_out", [2, D, D])
    od_w_in = _dram_in(nc, "od_w_in", [2, D, OD_IN])
    od_q_norm_g = _dram_in(nc, "od_q_norm_g", [2, 384])
    od_w_uq = _dram_in(nc, "od_w_uq", [2, 384, 768])
    od_kv_norm_g = _dram_in(nc, "od_kv_norm_g", [2, 256])
    od_w_ukv = _dram_in(nc, "od_w_ukv", [2, 256, 1024])
    od_conv_w = _dram_in(nc, "od_conv_w", [2, 3, 512])
    od_w_out = _dram_in(nc, "od_w_out", [2, D, D])
    router_w = _dram_in(nc, "router_w", [DEPTH, D, NE])
    exp_w1 = _dram_in(nc, "exp_w1", [DEPTH, NE, D, D])
    exp_w3 = _dram_in(nc, "exp_w3", [DEPTH, NE, D, D])
    exp_w2 = _dram_in(nc, "exp_w2", [DEPTH, NE, D, D])
    final_g = _dram_in(nc, "final_g", [D])
    ropeA = _dram_in(nc, "ropeA", [128, NT * 64])
    ropeC = _dram_in(nc, "ropeC", [128, NT * 32])
    out = nc.dram_tensor("out", [NLAT, D], F32, kind="ExternalOutput").ap()

    X = nc.dram_tensor("Xres", [NTOK, D], F32, kind="Internal").ap()
    MROW = nc.dram_tensor("mrow", [DEPTH, 2, 6 * D], F32, kind="Internal").ap()
    H2 = nc.dram_tensor("h2", [NTOK, D], BF16, kind="Internal").ap()
    AFF = nc.dram_tensor("aff", [NTOK, NE], F32, kind="Internal").ap()
    CS = nc.dram_tensor("cs", [NE * NTL, 128], F32, kind="Internal").ap()
    CT = nc.dram_tensor("ct", [NE * NTL], F32, kind="Internal").ap()
    QTD = nc.dram_tensor("qtd", [8, 96, NTOK], BF16, kind="Internal").ap()
    OTD = nc.dram_tensor("otd", [8, 64, NTOK], BF16, kind="Internal").ap()

    S = Sched(nc, es)
    S.limit = limit
    kb = KB(nc, S, es)
    Xb = [Buf("X%d" % t) for t in range(NT)]

    def I(eng, name, reads, writes, *a, **kw):
        S.op(eng, name, reads, writes, *a, **kw)

    def DMA(eng, reads, writes, out, in_):
        S.op(eng, "dma_start", reads, writes, out=out, in_=in_, dma=True)

    def MM(reads, writes, out, lhsT, rhs, start, stop):
        S.op("pe", "matmul", reads, writes, out, lhsT=lhsT, rhs=rhs, start=start, stop=stop)

    def TR(reads, writes, out, in_, ident):
        S.op("pe", "transpose", reads, writes, out=out, in_=in_, identity=ident)

    def ACT(reads, writes, out, in_, func, **kw):
        S.op("act", "activation", reads, writes, out=out, in_=in_, func=func, **kw)

    def TT(eng, reads, writes, out, in0, in1, op):
        S.op(eng, "tensor_tensor", reads, writes, out=out, in0=in0, in1=in1, op=op)

    def TS(eng, reads, writes, out, in0, s1, s2, op0, op1=None, **kw):
        if op1 is None:
            S.op(eng, "tensor_scalar", reads, writes, out=out, in0=in0, scalar1=s1, scalar2=None, op0=op0, **kw)
        else:
            S.op(eng, "tensor_scalar", reads, writes, out=out, in0=in0, scalar1=s1, scalar2=s2, op0=op0, op1=op1, **kw)

    def STT(eng, reads, writes, out, in0, scalar, in1, op0, op1):
        S.op(eng, "scalar_tensor_tensor", reads, writes, out=out, in0=in0, scalar=scalar, in1=in1, op0=op0, op1=op1)

    def RSQ(dst, dst_ap, src, src_ap):
        ACT([src.b], [dst.b], dst_ap, src_ap, AF.Sqrt, bias=epsb.t[0:dst_ap.shape[0], 0:1])
        I("dve", "reciprocal", [dst.b], [dst.b], out=dst_ap, in_=dst_ap)

    def IDMA(reads, writes, out, out_off, in_, in_off, bound, add=False):
        kw = dict(out=out, out_offset=out_off, in_=in_, in_offset=in_off, bounds_check=bound, oob_is_err=False)
        if add:
            kw["compute_op"] = ALU.add
        S.op("pool", "indirect_dma_start", reads, writes, dma=True, **kw)

    PB = [TB(es.enter_context(nc.psum_tensor("pb%d" % i, [128, 512], F32)), "pb%d" % i) for i in range(8)]

    def pbf(i):
        return PB[i].t[:].bitcast(BF16)

    identb = kb.sb(es, [128, 128], BF16, "identb")
    identf = kb.sb(es, [128, 128], F32, "identf")
    onesf = kb.sb(es, [128, 128], F32, "onesf")
    maskneg = kb.sb(es, [128, 2, 512], BF16, "maskneg")
    e65 = kb.sb(es, [1, 65], BF16, "e65")
    epsb = kb.sb(es, [128, 1], F32, "epsb")
    ropeA_sb = kb.sb(es, [128, NT, 64], F32, "ropeA")
    ropeC_sb = kb.sb(es, [128, NT, 32], F32, "ropeC")

    for idt in (identb, identf):
        I("pool", "memset", [], [idt.b], idt.t[:], 0.0)
        I("pool", "affine_select", [idt.b], [idt.b], out=idt.t[:], in_=idt.t[:], pattern=[[-1, 128]],
          compare_op=ALU.not_equal, fill=1.0, base=0, channel_multiplier=1)
    I("pool", "memset", [], [onesf.b], onesf.t[:], 1.0)
    I("pool", "memset", [], [maskneg.b], maskneg.t[:], 0.0)
    I("pool", "affine_select", [maskneg.b], [maskneg.b], out=maskneg.t[:, 0, :], in_=maskneg.t[:, 0, :],
      pattern=[[0, 4], [-1, 128]], compare_op=ALU.is_ge, fill=-30000.0, base=0, channel_multiplier=1)
    I("pool", "affine_select", [maskneg.b], [maskneg.b], out=maskneg.t[:, 1, :], in_=maskneg.t[:, 1, :],
      pattern=[[0, 4], [1, 128]], compare_op=ALU.is_ge, fill=-30000.0, base=0, channel_multiplier=-1)
    I("pool", "memset", [], [epsb.b], epsb.t[:], EPS)
    I("pool", "memset", [], [e65.b], e65.t[:], 0.0)
    I("pool", "memset", [e65.b], [e65.b], e65.t[0:1, 64:65], 1.0)
    DMA("sp", [], [ropeA_sb.b], ropeA_sb.t[:].rearrange("p t f -> p (t f)"), ropeA[:, :])
    DMA("sp", [], [ropeC_sb.b], ropeC_sb.t[:].rearrange("p t f -> p (t f)"), ropeC[:, :])
    for t in range(NT):
        src = x_in[t * 128:(t + 1) * 128, :] if t < NTL else ctx_in[(t - NTL) * 128:(t - NTL + 1) * 128, :]
        DMA("sp", [], [Xb[t]], X[t * 128:(t + 1) * 128, :], src)

    with ExitStack() as ms:
        sc = kb.sb(ms, [128, 8, 2], F32, "sc")
        modb2 = kb.sb(ms, [2, 6 * D], F32, "modb2")
        mrow_sb = kb.sb(ms, [2, 6 * D], F32, "mrow_sb")
        mw = kb.ring(ms, 2, [128, 8, 512], F32, "mw")
        DMA("sp", [], [sc.b], sc.t[:, :, 0:1], c_in.rearrange("(k p o) -> p k o", p=128, o=1))
        DMA("sp", [], [sc.b], sc.t[:, :, 1:2], cctx_in.rearrange("(k p o) -> p k o", p=128, o=1))
        ACT([sc.b], [sc.b], sc.t[:], sc.t[:], AF.Silu)
        i = 0
        for l in range(depth_run):
            for r in range(2):
                DMA("sp", [], [modb2.b], modb2.t[r:r + 1, :], mod_b[l:l + 1, :])
            for n in range(12):
                m = mw[i % 2]
                i += 1
                DMA("sp", [], [m.b], m.t[:], mod_w[l].rearrange("(k p) c -> p k c", p=128)[:, :, n * 512:(n + 1) * 512])
                for k in range(8):
                    MM([sc.b, m.b], [PB[0].b], PB[0].t[0:2, :], sc.t[:, k, :], m.t[:, k, :], k == 0, k == 7)
                TT("dve", [PB[0].b, modb2.b], [mrow_sb.b], mrow_sb.t[:, n * 512:(n + 1) * 512], PB[0].t[0:2, :],
                   modb2.t[:, n * 512:(n + 1) * 512], ALU.add)
            DMA("sp", [mrow_sb.b], [], MROW[l], mrow_sb.t[:])
        S.barrier()

    def bcast_row(dst, row_ap, bufs_w):
        DMA("sp", [], bufs_w, dst, row_ap.partition_broadcast(128))

    def load_mod(ms, l, j0, gvec, want_g=True, want_as=True):
        res = []
        for r in range(2):
            A = Sh = G = None
            if want_as:
                A = kb.sb(ms, [128, D], F32, "A")
                Sh = kb.sb(ms, [128, D], F32, "Sh")
            if want_g:
                G = kb.sb(ms, [128, D], F32, "G")
            res.append((A, Sh, G))
        with ExitStack() as tmpst:
            gb = None
            if want_as:
                gb = kb.sb(tmpst, [128, D], F32, "gb")
                bcast_row(gb.t[:], gvec, [gb.b])
            for r in range(2):
                A, Sh, G = res[r]
                if want_as:
                    bcast_row(Sh.t[:], MROW[l, r, j0 * D:(j0 + 1) * D], [Sh.b])
                    bcast_row(A.t[:], MROW[l, r, (j0 + 1) * D:(j0 + 2) * D], [A.b])
                    STT("dve", [A.b, gb.b], [A.b], A.t[:], A.t[:], 1.0, gb.t[:], ALU.add, ALU.mult)
                if want_g:
                    bcast_row(G.t[:], MROW[l, r, (j0 + 2) * D:(j0 + 3) * D], [G.b])
            if want_as:
                S.barrier()
        return res

    def norm_mod(xt, A, Sh, hout, ms_t, rstd_t, junk, tmp):
        ACT([xt.b], [ms_t.b, hout.b], hout.t[:], xt.t[:], AF.Square, scale=1.0 / 32.0, accum_out=ms_t.t[:, 0:1])
        RSQ(rstd_t, rstd_t.t[:, 0:1], ms_t, ms_t.t[:, 0:1])
        STT("dve", [xt.b, rstd_t.b, A.b], [tmp.b], tmp.t[:], xt.t[:], rstd_t.t[:, 0:1], A.t[:], ALU.mult, ALU.mult)
        TT("dve", [tmp.b, Sh.b], [hout.b], hout.t[:], tmp.t[:], Sh.t[:], ALU.add)

    def transpose8(h, hT, bank):
        pv = pbf(bank)
        for k in range(8):
            TR([h.b, identb.b], [PB[bank].b], pv[:, k * 128:(k + 1) * 128], h.t[:, k * 128:(k + 1) * 128], identb.t[:])
        ACT([PB[bank].b], [hT.b], hT.t[:].rearrange("p k t -> p (k t)"), pv[:, :], AF.Copy)

    def wout_residual(t, ON, cT, Wo_o, Wo_c, G, x, yn, o, ybanks=(0, 1)):
        for half in range(2):
            cs_ = slice(half * 512, (half + 1) * 512)
            yb = ybanks[half]
            for h in range(8):
                MM([ON.b, Wo_o.b], [PB[yb].b], PB[yb].t[:, :], ON.t[:, h, :], Wo_o.t[:, h, cs_], h == 0, False)
            for k in range(4):
                MM([cT.b, Wo_c.b], [PB[yb].b], PB[yb].t[:, :], cT.t[:, k, :], Wo_c.t[:, k, cs_], False, k == 3)
            TT("dve", [PB[yb].b, G.b], [o.b], o.t[:, cs_], PB[yb].t[:, :], G.t[:, cs_], ALU.mult)
        TT("dve", [o.b, x.b], [o.b], o.t[:], o.t[:], x.t[:], ALU.add)
        DMA("sp", [o.b], [Xb[t]], X[t * 128:(t + 1) * 128, :], o.t[:])

    def attn_norm(pbank, bbank, rs, Osb, on_out, nq, on_buf):
        I("dve", "reciprocal", [PB[pbank].b], [rs.b], out=rs.t[64:65, 0:nq], in_=PB[pbank].t[64:65, 0:nq])
        ACT([PB[pbank].b], [Osb.b], Osb.t[:, 0:nq], PB[pbank].t[0:64, 0:nq], AF.Copy)
        MM([onesf.b, rs.b], [PB[bbank].b], PB[bbank].t[0:64, 0:nq], onesf.t[64:65, 0:64], rs.t[64:65, 0:nq], True, True)
        TT("dve", [Osb.b, PB[bbank].b], [on_buf], on_out, Osb.t[:, 0:nq], PB[bbank].t[0:64, 0:nq], ALU.mult)

    def even_mixer(l, need_ctx):
        i = l // 2
        with ExitStack() as ms:
            Win = kb.sb(ms, [128, 8, EV_IN], BF16, "Win")
            Wo_o = kb.sb(ms, [64, 8, D], BF16, "Wo_o")
            Wo_s = kb.sb(ms, [128, 4, D], BF16, "Wo_s")
            Wsn = kb.sb(ms, [128, 4, 128], BF16, "Wsn")
            WsT = kb.sb(ms, [128, 4, 128], BF16, "WsT")
            bsT = kb.sb(ms, [128, 4], F32, "bsT")
            sgug = kb.sb(ms, [128, 512], F32, "sgug")
            snk = kb.sb(ms, [1, 8], F32, "snk")
            esink = kb.sb(ms, [1, 8, 128], BF16, "esink")
            DMA("pool", [], [Win.b], Win.t[:], ev_w_in[i].rearrange("(k p) c -> p k c", p=128))
            DMA("pool", [], [Wo_o.b], Wo_o.t[:], ev_w_out[i, 0:512, :].rearrange("(h d) c -> d h c", d=64))
            DMA("pool", [], [Wo_s.b], Wo_s.t[:], ev_w_out[i, 512:1024, :].rearrange("(k p) c -> p k c", p=128))
            DMA("pool", [], [Wsn.b], Wsn.t[:], ev_sgu_w[i].rearrange("g p q -> p g q"))
            DMA("sp", [], [bsT.b], bsT.t[:].rearrange("p (g o) -> p g o", o=1), ev_sgu_b[i].rearrange("g (p o) -> p g o", o=1))
            bcast_row(sgug.t[:], ev_sgu_norm_g[i], [sgug.b])
            DMA("sp", [], [snk.b], snk.t[:], ev_sink[i:i + 1, :])
            ACT([snk.b], [snk.b], snk.t[:], snk.t[:], AF.Exp)
            I("dve", "tensor_copy", [snk.b], [esink.b], out=esink.t[:],
              in_=snk.t[:].rearrange("o (h u) -> o h u", u=1).to_broadcast([1, 8, 128]))
            pv = pbf(4)
            for g in range(4):
                TR([Wsn.b, identb.b], [PB[4].b], pv[:, g * 128:(g + 1) * 128], Wsn.t[:, g, :], identb.t[:])
            ACT([PB[4].b], [WsT.b], WsT.t[:].rearrange("p g q -> p (g q)"), pv[:, 0:512], AF.Copy)
            mods = load_mod(ms, l, 0, norm1_g[l])

            xt = kb.ring(ms, 4, [128, D], F32, "xt")
            junk = None
            tmp_r = kb.ring(ms, 2, [128, D], F32, "tmp")
            hb_r = kb.ring(ms, 2, [128, D], BF16, "hb")
            hT_r = kb.ring(ms, 2, [128, 8, 128], BF16, "hT")
            msr_r = kb.ring(ms, 2, [128, 1], F32, "msr")
            rstd_r = kb.ring(ms, 2, [128, 1], F32, "rstd")
            zs_r = kb.ring(ms, 2, [128, 12, 64], F32, "zs")
            zr_r = kb.ring(ms, 2, [128, 12, 64], BF16, "zr")
            r1_r = kb.ring(ms, 1, [128, 12, 32], F32, "r1")
            r2_r = kb.ring(ms, 1, [128, 12, 32], F32, "r2")
            r3_r = kb.ring(ms, 1, [128, 12, 32], F32, "r3")
            r4_r = kb.ring(ms, 1, [128, 12, 32], F32, "r4")
            KT = kb.ring(ms, 5, [64, 2, 128], BF16, "KT")
            VA = kb.ring(ms, 5, [128, 2, 65], BF16, "VA")
            KTc = kb.ring(ms, 2, [64, 2, 128], BF16, "KTc")
            VAc = kb.ring(ms, 2, [128, 2, 65], BF16, "VAc")
            QT = kb.ring(ms, 3, [64, 8, 128], BF16, "QT")
            sT = kb.ring(ms, 3, [128, 4, 128], BF16, "sT")
            gu_r = kb.ring(ms, 2, [128, 2, 512], F32, "gu")
            g1_r = kb.ring(ms, 2, [128, 2, 512], F32, "g1")
            bst_r = kb.ring(ms, 2, [128, 6], F32, "bst")
            mv_r = kb.ring(ms, 2, [128, 2], F32, "mv")
            lrs_r = kb.ring(ms, 2, [128, 1], F32, "lrs")
            vn_r = kb.ring(ms, 2, [128, 512], F32, "vn")
            vnb_r = kb.ring(ms, 2, [128, 512], BF16, "vnb")
            sb_r = kb.ring(ms, 2, [128, 512], BF16, "s")
            PT = kb.ring(ms, 3, [128, 512], BF16, "PT")
            rs_r = kb.ring(ms, 2, [65, 512], F32, "rs")
            Osb_r = kb.ring(ms, 2, [64, 512], F32, "Osb")
            ON_r = kb.ring(ms, 2, [64, 8, 128], BF16, "ON")
            xo_r = kb.ring(ms, 2, [128, D], F32, "xo")
            for v in VA + VAc:
                I("pool", "memset", [], [v.b], v.t[:], 1.0)

            def kv_of(t):
                return (KTc[t - NTL], VAc[t - NTL]) if t >= NTL else (KT[t % 5], VA[t % 5])

            def phaseA(t):
                if VERBOSE:
                    print("even phaseA", t, "op", S.nops)
                r = 1 if t >= NTL else 0
                A, Sh, G = mods[r]
                x = xt[t % 4]
                i2 = t % 2
                tmp, hb, hT, msr, rstd, zs, zr = tmp_r[i2], hb_r[i2], hT_r[i2], msr_r[i2], rstd_r[i2], zs_r[i2], zr_r[i2]
                r1, r2, r3, r4 = r1_r[0], r2_r[0], r3_r[0], r4_r[0]
                gu, g1, bst, mv, lrs, vn, vnb, sb_ = gu_r[i2], g1_r[i2], bst_r[i2], mv_r[i2], lrs_r[i2], vn_r[i2], vnb_r[i2], sb_r[i2]
                DMA("sp", [Xb[t]], [x.b], x.t[:], X[t * 128:(t + 1) * 128, :])
                norm_mod(x, A, Sh, hb, msr, rstd, junk, tmp)
                transpose8(hb, hT, 4)
                for (bank, c0, c1) in ((0, 0, 512), (1, 512, 768)):
                    for k in range(8):
                        MM([hT.b, Win.b], [PB[bank].b], PB[bank].t[:, 0:c1 - c0], hT.t[:, k, :], Win.t[:, k, c0:c1], k == 0, k == 7)
                zsf = zs.t[:].rearrange("p h d -> p (h d)")
                ACT([PB[0].b], [zs.b], zsf[:, 0:512], PB[0].t[:, :], AF.Copy)
                ACT([PB[1].b], [zs.b], zsf[:, 512:768], PB[1].t[:, 0:256], AF.Copy)
                for (bank, c0) in ((2, 768), (3, 1280)):
                    for k in range(8):
                        MM([hT.b, Win.b], [PB[bank].b], PB[bank].t[:, :], hT.t[:, k, :], Win.t[:, k, c0:c0 + 512], k == 0, k == 7)
                cos = ropeA_sb.t[:, t, 0:32].rearrange("p (o f) -> p o f", o=1).to_broadcast([128, 12, 32])
                sin = ropeA_sb.t[:, t, 32:64].rearrange("p (o f) -> p o f", o=1).to_broadcast([128, 12, 32])
                x1 = zs.t[:, :, 0:32]
                x2 = zs.t[:, :, 32:64]
                TT("dve", [zs.b, ropeA_sb.b], [r1.b], r1.t[:], x1, cos, ALU.mult)
                TT("pool", [zs.b, ropeA_sb.b], [r2.b], r2.t[:], x2, sin, ALU.mult)
                TT("dve", [zs.b, ropeA_sb.b], [r3.b], r3.t[:], x2, cos, ALU.mult)
                TT("pool", [zs.b, ropeA_sb.b], [r4.b], r4.t[:], x1, sin, ALU.mult)
                TT("dve", [r1.b, r2.b], [zr.b], zr.t[:, :, 0:32], r1.t[:], r2.t[:], ALU.subtract)
                TT("pool", [r3.b, r4.b], [zr.b], zr.t[:, :, 32:64], r3.t[:], r4.t[:], ALU.add)
                kt, va = kv_of(t)
                ACT([zs.b], [va.b], va.t[:, :, 0:64], zs.t[:, 2:4, :], AF.Copy)
                pq = pbf(4)
                pk = pbf(5)
                for h in range(8):
                    TR([zr.b, identb.b], [PB[4].b], pq[0:64, h * 128:(h + 1) * 128], zr.t[:, 4 + h, :], identb.t[:])
                for g in range(2):
                    TR([zr.b, identb.b], [PB[5].b], pk[0:64, g * 128:(g + 1) * 128], zr.t[:, g, :], identb.t[:])
                q = QT[t % 3]
                ACT([PB[4].b], [q.b], q.t[:].rearrange("p h t -> p (h t)"), pq[0:64, :], AF.Copy)
                ACT([PB[5].b], [kt.b], kt.t[:].rearrange("p h t -> p (h t)"), pk[0:64, 0:256], AF.Copy)
                for hh, bank in ((0, 2), (1, 3)):
                    ACT([PB[bank].b], [gu.b], gu.t[:, hh, :], PB[bank].t[:, :], AF.Copy)
                guf = gu.t[:].rearrange("p a f -> p (a f)")
                g1f = g1.t[:].rearrange("p a f -> p (a f)")
                TT("pool", [gu.b], [g1.b], g1f, guf, guf, ALU.mult)
                TS("dve", [g1.b], [g1.b], g1f, g1f, 0.044715, 1.0, ALU.mult, ALU.add)
                TT("pool", [g1.b, gu.b], [g1.b], g1f, g1f, guf, ALU.mult)
                ACT([g1.b], [g1.b], g1f, g1f, AF.Tanh, scale=0.7978845608)
                TS("dve", [g1.b], [g1.b], g1f, g1f, 0.5, 0.5, ALU.mult, ALU.add)
                TT("dve", [g1.b, gu.b], [gu.b], guf, guf, g1f, ALU.mult)
                I("dve", "bn_stats", [gu.b], [bst.b], out=bst.t[:], in_=gu.t[:, 1, :])
                I("dve", "bn_aggr", [bst.b], [mv.b], out=mv.t[:], in_=bst.t[:])
                RSQ(lrs, lrs.t[:], mv, mv.t[:, 1:2])
                TS("dve", [gu.b, mv.b, lrs.b], [vn.b], vn.t[:], gu.t[:, 1, :], mv.t[:, 0:1], lrs.t[:, 0:1], ALU.subtract, ALU.mult)
                TT("pool", [vn.b, sgug.b], [vnb.b], vnb.t[:], vn.t[:], sgug.t[:], ALU.mult)
                for g in range(4):
                    MM([WsT.b, vnb.b], [PB[3].b], PB[3].t[:, g * 128:(g + 1) * 128], WsT.t[:, g, :], vnb.t[:, g * 128:(g + 1) * 128], True, True)
                for g in range(4):
                    STT("dve", [PB[3].b, bsT.b, gu.b], [sb_.b], sb_.t[:, g * 128:(g + 1) * 128], PB[3].t[:, g * 128:(g + 1) * 128],
                        bsT.t[:, g:g + 1], gu.t[:, 0, g * 128:(g + 1) * 128], ALU.add, ALU.mult)
                pv2 = pbf(4)
                for k in range(4):
                    TR([sb_.b, identb.b], [PB[4].b], pv2[:, k * 128:(k + 1) * 128], sb_.t[:, k * 128:(k + 1) * 128], identb.t[:])
                st_ = sT[t % 3]
                ACT([PB[4].b], [st_.b], st_.t[:].rearrange("p k t -> p (k t)"), pv2[:, 0:512], AF.Copy)

            def phaseB(t):
                if VERBOSE:
                    print("even phaseB", t, "op", S.nops)
                r = 1 if t >= NTL else 0
                A, Sh, G = mods[r]
                x = xt[t % 4]
                q = QT[t % 3]
                i2 = t % 2
                tmp, ON, xo = tmp_r[i2], ON_r[i2], xo_r[i2]
                if t >= NTL:
                    keys = [(NTL, None), (NTL + 1, None)]
                else:
                    keys = []
                    if t > 0:
                        keys.append((t - 1, 0))
                    keys.append((t, None))
                    if t < NTL - 1:
                        keys.append((t + 1, 1))
                    keys += [(NTL, None), (NTL + 1, None)]
                npt = 0
                for g in range(2):
                    for j, (kt_i, mk) in enumerate(keys):
                        kt, va = kv_of(kt_i)
                        bank = 5 + (npt % 2)
                        pt_ = PT[npt % 3]
                        npt += 1
                        MM([kt.b, q.b], [PB[bank].b], PB[bank].t[:, :], kt.t[:, g, :],
                           q.t[:, 4 * g:4 * g + 4, :].rearrange("p h t -> p (h t)"), True, mk is None)
                        if mk is not None:
                            MM([identb.b, maskneg.b], [PB[bank].b], PB[bank].t[:, :], identb.t[:], maskneg.t[:, mk, :], False, True)
                        ACT([PB[bank].b], [pt_.b], pt_.t[:], PB[bank].t[:, :], AF.Exp, scale=0.125)
                        MM([va.b, pt_.b], [PB[7].b], PB[7].t[0:65, :], va.t[:, g, :], pt_.t[:], j == 0, False)
                    MM([e65.b, esink.b], [PB[7].b], PB[7].t[0:65, :], e65.t[0:1, :],
                       esink.t[0:1, 4 * g:4 * g + 4, :].rearrange("p h t -> p (h t)"), False, True)
                    attn_norm(7, 2, rs_r[g], Osb_r[g], ON.t[:, 4 * g:4 * g + 4, :].rearrange("p h t -> p (h t)"), 512, ON.b)
                wout_residual(t, ON, sT[t % 3], Wo_o, Wo_s, G, x, tmp, xo)

            phaseA(NTL)
            phaseA(NTL + 1)
            if need_ctx:
                phaseB(NTL)
                phaseB(NTL + 1)
            phaseA(0)
            for t in range(NTL):
                if t + 1 < NTL:
                    phaseA(t + 1)
                phaseB(t)
            S.barrier()

    def odd_mixer(l, need_ctx):
        i = l // 2
        SCALE = 96.0 ** -0.5
        with ExitStack() as ks:
            KT = kb.sb(ks, [96, 8, NTOK], BF16, "KTm")
            VA = kb.sb(ks, [128, NT, 8, 65], BF16, "VAm")
            I("pool", "memset", [], [VA.b], VA.t[:].rearrange("p t h d -> p (t h d)"), 1.0)
            with ExitStack() as ms:
                Wa = kb.sb(ms, [128, 8, 672], BF16, "Wa")
                Wukv = kb.sb(ms, [128, 2, 1024], BF16, "Wukv")
                Wuq = kb.sb(ms, [128, 3, 768], BF16, "Wuq")
                gkv = kb.sb(ms, [128, 256], F32, "gkv")
                gq = kb.sb(ms, [128, 384], F32, "gq")
                DMA("pool", [], [Wa.b], Wa.t[:], od_w_in[i].rearrange("(k p) c -> p k c", p=128)[:, :, 0:672])
                DMA("pool", [], [Wukv.b], Wukv.t[:], od_w_ukv[i].rearrange("(k p) c -> p k c", p=128))
                DMA("pool", [], [Wuq.b], Wuq.t[:], od_w_uq[i].rearrange("(k p) c -> p k c", p=128))
                bcast_row(gkv.t[:], od_kv_norm_g[i], [gkv.b])
                bcast_row(gq.t[:], od_q_norm_g[i], [gq.b])
                mods = load_mod(ms, l, 0, norm1_g[l], want_g=False)
                xt = kb.ring(ms, 2, [128, D], F32, "oxt")
                junk = kb.sb(ms, [128, D], BF16, "ojunk")
                tmp = kb.sb(ms, [128, D], F32, "otmp")
                hb = kb.sb(ms, [128, D], BF16, "ohb")
                hT = kb.sb(ms, [128, 8, 128], BF16, "ohT")
                msr = kb.sb(ms, [128, 1], F32, "omsr")
                rstd = kb.sb(ms, [128, 1], F32, "orstd")
                zc = kb.sb(ms, [128, 672], F32, "zc")
                ms2 = kb.sb(ms, [128, 2], F32, "ms2")
                rs2 = kb.sb(ms, [128, 2], F32, "rs2")
                cn = kb.sb(ms, [128, 640], BF16, "cn")
                cT = kb.sb(ms, [128, 5, 128], BF16, "cT")
                qs = kb.sb(ms, [128, 9, 96], F32, "qs")
                qr = kb.sb(ms, [128, 9, 96], BF16, "qr")
                kf = kb.sb(ms, [128, 8, 96], BF16, "kf")
                p1 = kb.sb(ms, [128, 9, 16], F32, "p1")
                p2 = kb.sb(ms, [128, 9, 16], F32, "p2")
                p3 = kb.sb(ms, [128, 9, 16], F32, "p3")
                p4 = kb.sb(ms, [128, 9, 16], F32, "p4")
                qT = kb.ring(ms, 2, [96, 8, 128], BF16, "qT")
                I("pool", "memset", [], [qs.b], qs.t[:].rearrange("p h d -> p (h d)"), 0.0)
                for t in range(NT):
                    r = 1 if t >= NTL else 0
                    A, Sh, _ = mods[r]
                    x = xt[t % 2]
                    DMA("sp", [Xb[t]], [x.b], x.t[:], X[t * 128:(t + 1) * 128, :])
                    norm_mod(x, A, Sh, hb, msr, rstd, junk, tmp)
                    transpose8(hb, hT, 4)
                    for (bank, c0, c1) in ((0, 0, 512), (1, 512, 672)):
                        for k in range(8):
                            MM([hT.b, Wa.b], [PB[bank].b], PB[bank].t[:, 0:c1 - c0], hT.t[:, k, :], Wa.t[:, k, c0:c1], k == 0, k == 7)
                    ACT([PB[0].b], [zc.b], zc.t[:, 0:512], PB[0].t[:, :], AF.Copy)
                    ACT([PB[1].b], [zc.b], zc.t[:, 512:672], PB[1].t[:, 0:160], AF.Copy)
                    ACT([zc.b], [junk.b, ms2.b], junk.t[:, 0:256], zc.t[:, 0:256], AF.Square, scale=1.0 / 16.0, accum_out=ms2.t[:, 0:1])
                    ACT([zc.b], [junk.b, ms2.b], junk.t[:, 256:640], zc.t[:, 288:672], AF.Square, scale=384.0 ** -0.5, accum_out=ms2.t[:, 1:2])
                    RSQ(rs2, rs2.t[:], ms2, ms2.t[:])
                    STT("dve", [zc.b, rs2.b, gkv.b], [cn.b], cn.t[:, 0:256], zc.t[:, 0:256], rs2.t[:, 0:1], gkv.t[:], ALU.mult, ALU.mult)
                    STT("dve", [zc.b, rs2.b, gq.b], [cn.b], cn.t[:, 256:640], zc.t[:, 288:672], rs2.t[:, 1:2], gq.t[:], ALU.mult, ALU.mult)
                    pv = pbf(4)
                    for k in range(5):
                        TR([cn.b, identb.b], [PB[4].b], pv[:, k * 128:(k + 1) * 128], cn.t[:, k * 128:(k + 1) * 128], identb.t[:])
                    ACT([PB[4].b], [cT.b], cT.t[:].rearrange("p k t -> p (k t)"), pv[:, 0:640], AF.Copy)
                    for half in range(2):
                        for kc in range(2):
                            MM([cT.b, Wukv.b], [PB[2 + half].b], PB[2 + half].t[:, :], cT.t[:, kc, :],
                               Wukv.t[:, kc, half * 512:(half + 1) * 512], kc == 0, kc == 1)
                    for (bank, c0, c1) in ((5, 0, 512), (6, 512, 768)):
                        for kc in range(3):
                            MM([cT.b, Wuq.b], [PB[bank].b], PB[bank].t[:, 0:c1 - c0], cT.t[:, 2 + kc, :], Wuq.t[:, kc, c0:c1], kc == 0, kc == 2)
                    for half in range(2):
                        pvv = PB[2 + half].t[:, :].rearrange("p (h d) -> p h d", d=128)
                        ACT([PB[2 + half].b], [kf.b], kf.t[:, 4 * half:4 * half + 4, 0:64], pvv[:, :, 0:64], AF.Copy)
                        ACT([PB[2 + half].b], [VA.b], VA.t[:, t, 4 * half:4 * half + 4, 0:64], pvv[:, :, 64:128], AF.Copy)
                    qsf = qs.t[:].rearrange("p h d -> p (h d)")
                    ACT([PB[5].b], [qs.b], qsf[:, 0:512], PB[5].t[:, :], AF.Copy)
                    ACT([PB[6].b], [qs.b], qsf[:, 512:768], PB[6].t[:, 0:256], AF.Copy)
                    I("dve", "tensor_copy", [zc.b], [qs.b], out=qs.t[:, 8, 64:96], in_=zc.t[:, 256:288])
                    cos = ropeC_sb.t[:, t, 0:16].rearrange("p (o f) -> p o f", o=1).to_broadcast([128, 9, 16])
                    sin = ropeC_sb.t[:, t, 16:32].rearrange("p (o f) -> p o f", o=1).to_broadcast([128, 9, 16])
                    x1 = qs.t[:, :, 64:80]
                    x2 = qs.t[:, :, 80:96]
                    TT("dve", [qs.b, ropeC_sb.b], [p1.b], p1.t[:], x1, cos, ALU.mult)
                    TT("pool", [qs.b, ropeC_sb.b], [p2.b], p2.t[:], x2, sin, ALU.mult)
                    TT("dve", [qs.b, ropeC_sb.b], [p3.b], p3.t[:], x2, cos, ALU.mult)
                    TT("pool", [qs.b, ropeC_sb.b], [p4.b], p4.t[:], x1, sin, ALU.mult)
                    TT("dve", [p1.b, p2.b], [qr.b], qr.t[:, :, 64:80], p1.t[:], p2.t[:], ALU.subtract)
                    TT("pool", [p3.b, p4.b], [qr.b], qr.t[:, :, 80:96], p3.t[:], p4.t[:], ALU.add)
                    ACT([qs.b], [qr.b], qr.t[:, 0:8, 0:64], qs.t[:, 0:8, 0:64], AF.Copy)
                    I("dve", "tensor_copy", [qr.b], [kf.b], out=kf.t[:, :, 64:96],
                      in_=qr.t[:, 8:9, 64:96].to_broadcast([128, 8, 32]))
                    pk = pbf(4)
                    pq = pbf(7)
                    for h in range(8):
                        TR([kf.b, identb.b], [PB[4].b], pk[0:96, h * 128:(h + 1) * 128], kf.t[:, h, :], identb.t[:])
                    for h in range(8):
                        TR([qr.b, identb.b], [PB[7].b], pq[0:96, h * 128:(h + 1) * 128], qr.t[:, h, :], identb.t[:])
                    ACT([PB[4].b], [KT.b], KT.t[:, :, t * 128:(t + 1) * 128], pk[0:96, :].rearrange("p (h t) -> p h t", t=128), AF.Copy)
                    q_ = qT[t % 2]
                    ACT([PB[7].b], [q_.b], q_.t[:].rearrange("p h t -> p (h t)"), pq[0:96, :], AF.Copy)
                    DMA("sp", [q_.b], [], QTD[:, :, t * 128:(t + 1) * 128].rearrange("h d t -> d h t"), q_.t[:])
                S.barrier()
            with ExitStack() as ms:
                qt = kb.ring(ms, 2, [96, 8, 512], BF16, "qt")
                PT = kb.ring(ms, 6, [128, 512], BF16, "PTm")
                rs = kb.sb(ms, [65, 512], F32, "rsm")
                Osb = kb.sb(ms, [64, 512], F32, "Osbm")
                on = kb.ring(ms, 2, [64, 512], BF16, "onm")
                blocks = [(b * 512, 512, list(range(NT))) for b in range(NLAT // 512)]
                if need_ctx:
                    blocks.append((NLAT, NCTX, [NTL, NTL + 1]))
                npt = 0
                nh = 0
                for bi, (q0, nq, keys) in enumerate(blocks):
                    q = qt[bi % 2]
                    DMA("sp", [], [q.b], q.t[:, :, 0:nq], QTD[:, :, q0:q0 + nq].rearrange("h d t -> d h t"))
                    for h in range(8):
                        pbank = 7 if nh % 2 == 0 else 3
                        bbank = 2 if nh % 2 == 0 else 1
                        o_ = on[nh % 2]
                        nh += 1
                        for j, kt_i in enumerate(keys):
                            bank = (5, 6, 0, 4)[npt % 4]
                            pt_ = PT[npt % 6]
                            npt += 1
                            MM([KT.b, q.b], [PB[bank].b], PB[bank].t[:, 0:nq], KT.t[:, h, kt_i * 128:(kt_i + 1) * 128], q.t[:, h, 0:nq], True, True)
                            ACT([PB[bank].b], [pt_.b], pt_.t[:, 0:nq], PB[bank].t[:, 0:nq], AF.Exp, scale=SCALE)
                            MM([VA.b, pt_.b], [PB[pbank].b], PB[pbank].t[0:65, 0:nq], VA.t[:, kt_i, h, :], pt_.t[:, 0:nq], j == 0, j == len(keys) - 1)
                        attn_norm(pbank, bbank, rs, Osb, o_.t[:, 0:nq], nq, o_.b)
                        DMA("sp", [o_.b], [], OTD[h, :, q0:q0 + nq], o_.t[:, 0:nq])
                S.barrier()
        with ExitStack() as ms:
            Wb = kb.sb(ms, [128, 8, 1536], BF16, "Wb")
            Wo_o = kb.sb(ms, [64, 8, D], BF16, "Wo_o2")
            Wo_c = kb.sb(ms, [128, 4, D], BF16, "Wo_c")
            cw = kb.sb(ms, [128, 3, 4], F32, "cw")
            cwb = kb.sb(ms, [128, 3, 4, 128], F32, "cwb")
            DMA("pool", [], [Wb.b], Wb.t[:], od_w_in[i].rearrange("(k p) c -> p k c", p=128)[:, :, 672:2208])
            DMA("pool", [], [Wo_o.b], Wo_o.t[:], od_w_out[i, 0:512, :].rearrange("(h d) c -> d h c", d=64))
            DMA("pool", [], [Wo_c.b], Wo_c.t[:], od_w_out[i, 512:1024, :].rearrange("(k p) c -> p k c", p=128))
            DMA("sp", [], [cw.b], cw.t[:].rearrange("p j (c o) -> p j c o", o=1), od_conv_w[i].rearrange("j (c p o) -> p j c o", p=128, o=1))
            I("dve", "tensor_copy", [cw.b], [cwb.b], out=cwb.t[:].rearrange("p j c t -> p (j c) t"),
              in_=cw.t[:].rearrange("p j (c o) -> p (j c) o", o=1).to_broadcast([128, 12, 128]))
            mods = load_mod(ms, l, 0, norm1_g[l])
            xt = kb.ring(ms, 3, [128, D], F32, "cxt")
            junk = kb.sb(ms, [128, D], BF16, "cjunk")
            tmp_r = kb.ring(ms, 2, [128, D], F32, "ctmp")
            hb_r = kb.ring(ms, 2, [128, D], BF16, "chb")
            hT_r = kb.ring(ms, 2, [128, 8, 128], BF16, "chT")
            msr_r = kb.ring(ms, 2, [128, 1], F32, "cmsr")
            rstd_r = kb.ring(ms, 2, [128, 1], F32, "crstd")
            UT = kb.ring(ms, 3, [128, 4, 130], F32, "UT")
            BT = kb.ring(ms, 3, [128, 4, 128], F32, "BT")
            Cs_r = kb.ring(ms, 2, [128, 4, 128], F32, "Cs")
            c1_r = kb.ring(ms, 2, [128, 4, 128], F32, "c1")
            c2_r = kb.ring(ms, 2, [128, 4, 128], F32, "c2")
            cTt_r = kb.ring(ms, 2, [128, 4, 128], BF16, "cTt")
            ot = kb.ring(ms, 3, [64, 8, 128], BF16, "ot")
            xo_r = kb.ring(ms, 2, [128, D], F32, "cxo")
            last = NT if need_ctx else NTL

            def seg_first(t):
                return t == 0 or t == NTL

            def seg_last(t):
                return t == NTL - 1 or t == NT - 1

            def phaseA(t):
                r = 1 if t >= NTL else 0
                A, Sh, G = mods[r]
                x = xt[t % 3]
                u = UT[t % 3]
                i2 = t % 2
                tmp, hb, hT, msr, rstd, Cs = tmp_r[i2], hb_r[i2], hT_r[i2], msr_r[i2], rstd_r[i2], Cs_r[i2]
                DMA("sp", [Xb[t]], [x.b], x.t[:], X[t * 128:(t + 1) * 128, :])
                norm_mod(x, A, Sh, hb, msr, rstd, junk, tmp)
                transpose8(hb, hT, 6)
                zb = 3 * i2
                for part in range(3):
                    for c in range(4):
                        c0 = part * 512 + c * 128
                        for k in range(8):
                            MM([hT.b, Wb.b], [PB[zb + part].b], PB[zb + part].t[:, c * 128:(c + 1) * 128], Wb.t[:, k, c0:c0 + 128], hT.t[:, k, :], k == 0, k == 7)
                b_ = BT[t % 3]
                ACT([PB[zb].b], [b_.b], b_.t[:].rearrange("p c t -> p (c t)"), PB[zb].t[:, :], AF.Copy)
                ACT([PB[zb + 1].b], [Cs.b], Cs.t[:].rearrange("p c t -> p (c t)"), PB[zb + 1].t[:, :], AF.Copy)
                TT("dve", [Cs.b, PB[zb + 2].b], [u.b], u.t[:, :, 1:129], Cs.t[:], PB[zb + 2].t[:, :].rearrange("p (c t) -> p c t", t=128), ALU.mult)
                if seg_first(t):
                    I("pool", "memset", [u.b], [u.b], u.t[:, :, 0:1], 0.0)
                else:
                    up = UT[(t - 1) % 3]
                    I("pool", "tensor_copy", [u.b, up.b], [u.b], out=u.t[:, :, 0:1], in_=up.t[:, :, 128:129])
                    I("pool", "tensor_copy", [u.b, up.b], [up.b], out=up.t[:, :, 129:130], in_=u.t[:, :, 1:2])
                if seg_last(t):
                    I("pool", "memset", [u.b], [u.b], u.t[:, :, 129:130], 0.0)

            def phaseB(t):
                r = 1 if t >= NTL else 0
                A, Sh, G = mods[r]
                x = xt[t % 3]
                u = UT[t % 3]
                b_ = BT[t % 3]
                i2 = t % 2
                tmp, c1, c2, cTt, xo = tmp_r[i2], c1_r[i2], c2_r[i2], cTt_r[i2], xo_r[i2]
                TT("dve", [u.b, cwb.b], [c1.b], c1.t[:], u.t[:, :, 0:128], cwb.t[:, 0], ALU.mult)
                TT("pool", [u.b, cwb.b], [c2.b], c2.t[:], u.t[:, :, 1:129], cwb.t[:, 1], ALU.mult)
                TT("dve", [c1.b, c2.b], [c1.b], c1.t[:], c1.t[:], c2.t[:], ALU.add)
                TT("pool", [u.b, cwb.b], [c2.b], c2.t[:], u.t[:, :, 2:130], cwb.t[:, 2], ALU.mult)
                TT("dve", [c1.b, c2.b], [c1.b], c1.t[:], c1.t[:], c2.t[:], ALU.add)
                TT("dve", [c1.b, b_.b], [cTt.b], cTt.t[:], c1.t[:], b_.t[:], ALU.mult)
                o_ = ot[t % 3]
                DMA("sp", [], [o_.b], o_.t[:], OTD[:, :, t * 128:(t + 1) * 128].rearrange("h d t -> d h t"))
                wout_residual(t, o_, cTt, Wo_o, Wo_c, G, x, tmp, xo, ybanks=(7, 7))

            order = list(range(NTL)) + ([NTL, NTL + 1] if need_ctx else [])
            for idx, t in enumerate(order):
                if idx == 0 or seg_first(t):
                    phaseA(t)
                if not seg_last(t):
                    phaseA(t + 1)
                phaseB(t)
            S.barrier()

    def ffn(l, need_ctx):
        with ExitStack() as ms:
            mods = load_mod(ms, l, 3, norm2_g[l], want_as=False)
            G2 = [mods[0][2], mods[1][2]]
            affT = kb.sb(ms, [128, NT, NE], F32, "affT")
            affE = kb.sb(ms, [NE, NTOK], F32, "affE")
            tok_i = kb.sb(ms, [128, 2, 4, NE], I32, "tok_i")
            ntiles = NT if need_ctx else NTL
            with ExitStack() as f1:
                modsA = load_mod(f1, l, 3, norm2_g[l], want_g=False)
                Wr = kb.sb(f1, [128, 8, NE], BF16, "Wr")
                DMA("pool", [], [Wr.b], Wr.t[:], router_w[l].rearrange("(k p) c -> p k c", p=128))
                xt = kb.ring(f1, 3, [128, D], F32, "fxt")
                junk = kb.sb(f1, [128, D], BF16, "fjunk")
                tmp_r = kb.ring(f1, 2, [128, D], F32, "ftmp")
                hb = kb.ring(f1, 3, [128, D], BF16, "fhb")
                hT_r = kb.ring(f1, 2, [128, 8, 128], BF16, "fhT")
                msr_r = kb.ring(f1, 2, [128, 1], F32, "fmsr")
                rstd_r = kb.ring(f1, 2, [128, 1], F32, "frstd")
                mx_r = kb.ring(f1, 2, [128, 1], F32, "mx")
                ssum_r = kb.ring(f1, 2, [128, 1], F32, "ssum")
                ee_r = kb.ring(f1, 2, [128, NE], F32, "ee")
                for t in range(ntiles):
                    r = 1 if t >= NTL else 0
                    A, Sh, _ = modsA[r]
                    x = xt[t % 3]
                    h = hb[t % 3]
                    i2 = t % 2
                    tmp, hT, msr, rstd, mx, ssum, ee = tmp_r[i2], hT_r[i2], msr_r[i2], rstd_r[i2], mx_r[i2], ssum_r[i2], ee_r[i2]
                    DMA("sp", [Xb[t]], [x.b], x.t[:], X[t * 128:(t + 1) * 128, :])
                    norm_mod(x, A, Sh, h, msr, rstd, junk, tmp)
                    DMA("sp", [h.b], [], H2[t * 128:(t + 1) * 128, :], h.t[:])
                    transpose8(h, hT, 4 + i2)
                    for k in range(8):
                        MM([hT.b, Wr.b], [PB[i2].b], PB[i2].t[:, 0:NE], hT.t[:, k, :], Wr.t[:, k, :], k == 0, k == 7)
                    I("dve", "tensor_reduce", [PB[i2].b], [mx.b], out=mx.t[:], in_=PB[i2].t[:, 0:NE], axis=AX.X, op=ALU.max)
                    TS("dve", [mx.b], [mx.b], mx.t[:], mx.t[:], -1.0, None, ALU.mult)
                    ACT([PB[i2].b, mx.b], [ee.b, ssum.b], ee.t[:], PB[i2].t[:, 0:NE], AF.Exp, bias=mx.t[:, 0:1], accum_out=ssum.t[:, 0:1])
                    I("dve", "reciprocal", [ssum.b], [ssum.b], out=ssum.t[:], in_=ssum.t[:])
                    TS("dve", [ee.b, ssum.b], [affT.b], affT.t[:, t, :], ee.t[:], ssum.t[:, 0:1], None, ALU.mult)
                    TR([affT.b, identf.b], [PB[2 + i2].b], PB[2 + i2].t[0:NE, 0:128], affT.t[:, t, :], identf.t[:])
                    ACT([PB[2 + i2].b], [affE.b], affE.t[:, t * 128:(t + 1) * 128], PB[2 + i2].t[0:NE, 0:128], AF.Copy)
                for t0 in range(0, ntiles, 8):
                    t1 = min(t0 + 8, ntiles)
                    DMA("sp", [affT.b], [], AFF[t0 * 128:t1 * 128, :].rearrange("(t p) e -> p t e", p=128), affT.t[:, t0:t1, :])
                S.barrier()
            segs = [(0, NLAT, 512)]
            if need_ctx:
                segs.append((NLAT, NCTX, 32))
            with ExitStack() as ws:
                lo = kb.sb(ws, [NE, 1], F32, "lo")
                mid = kb.sb(ws, [NE, 1], F32, "mid")
                cnt = kb.sb(ws, [NE, 1], F32, "cnt")
                stp = kb.sb(ws, [NE, 1], F32, "stp")
                mask = kb.sb(ws, [NE, NLAT], F32, "mask")
                csum = kb.sb(ws, [NE, NLAT], F32, "csum")
                ctl = kb.sb(ws, [NE, NTL], F32, "ctl")
                ctb = kb.sb(ws, [128, NE * NTL], F32, "ctb")
                le = kb.sb(ws, [128, NE, 128], F32, "le")
                Tf = kb.sb(ws, [128, NE], F32, "Tf")
                ridx = kb.sb(ws, [128, NE], F32, "ridx")
                ridx_i = kb.sb(ws, [128, NE], I32, "ridx_i")
                Gall = kb.sb(ws, [128, NE, 128], F32, "Gall")
                loc = kb.sb(ws, [128, NE], F32, "loc")
                cslot = kb.sb(ws, [128, 4], F32, "cslot")
                e32 = kb.sb(ws, [128, NE], F32, "e32")
                I("pool", "iota", [], [cslot.b], cslot.t[:], pattern=[[128, 4]], base=0, channel_multiplier=1,
                  allow_small_or_imprecise_dtypes=True)
                csb = Buf("CS")
                ctbuf = Buf("CT")
                Gb = [Buf("G%d" % ex) for ex in range(NE)]
                for si, (n0, n, cap) in enumerate(segs):
                    ntl = n // 128
                    nst = (cap + 127) // 128
                    I("pool", "iota", [], [e32.b], e32.t[:], pattern=[[ntl, NE]], base=0, channel_multiplier=0,
                      allow_small_or_imprecise_dtypes=True)
                    av = affE.t[:, n0:n0 + n]
                    I("dve", "memset", [], [lo.b], lo.t[:], 0.0)
                    for it in range(23):
                        w = 2.0 ** (-(it + 1))
                        TS("dve", [lo.b], [mid.b], mid.t[:], lo.t[:], w, None, ALU.add)
                        TS("dve", [affE.b, mid.b], [mask.b, cnt.b], mask.t[:, 0:n], av, mid.t[:, 0:1], 0.0, ALU.is_ge, ALU.add,
                           accum_out=cnt.t[:, 0:1])
                        TS("dve", [cnt.b], [stp.b], stp.t[:], cnt.t[:], float(cap) - 0.5, w, ALU.is_ge, ALU.mult)
                        TT("dve", [lo.b, stp.b], [lo.b], lo.t[:], lo.t[:], stp.t[:], ALU.add)
                    TS("dve", [affE.b, lo.b], [mask.b], mask.t[:, 0:n], av, lo.t[:, 0:1], None, ALU.is_ge)
                    I("dve", "tensor_tensor_scan", [mask.b], [csum.b], out=csum.t[:, 0:n], data0=mask.t[:, 0:n], data1=mask.t[:, 0:n],
                      initial=0.0, op0=ALU.add, op1=ALU.max)
                    DMA("sp", [csum.b], [csb], CS[0:NE * ntl, :].rearrange("(e t) p -> e t p", t=ntl),
                        csum.t[:, 0:n].rearrange("e (t p) -> e t p", p=128))
                    I("dve", "tensor_copy", [csum.b], [ctl.b], out=ctl.t[:, 0:ntl],
                      in_=csum.t[:, 0:n].rearrange("e (t p) -> e t p", p=128)[:, :, 127])
                    DMA("sp", [ctl.b], [ctbuf], CT[0:NE * ntl].rearrange("(e t) -> e t", t=ntl), ctl.t[:, 0:ntl])
                    DMA("sp", [ctbuf], [ctb.b], ctb.t[:, 0:NE * ntl], CT[0:NE * ntl].partition_broadcast(128))
                    ctbv = ctb.t[:, 0:NE * ntl].rearrange("p (e t) -> p e t", t=ntl)
                    for j in range(nst):
                        TS("dve", [ctb.b, cslot.b], [le.b], le.t[:, :, 0:ntl], ctbv, cslot.t[:, j:j + 1], None, ALU.is_le)
                        I("dve", "tensor_reduce", [le.b], [Tf.b], out=Tf.t[:], in_=le.t[:, :, 0:ntl], axis=AX.X, op=ALU.add)
                        TT("dve", [Tf.b, e32.b], [ridx.b], ridx.t[:], Tf.t[:], e32.t[:], ALU.add)
                        I("dve", "tensor_copy", [ridx.b], [ridx_i.b], out=ridx_i.t[:], in_=ridx.t[:])
                        for ex in range(NE):
                            IDMA([ridx_i.b, csb], [Gb[ex]], Gall.t[:, ex, :], None, CS[:, :],
                                 bass.IndirectOffsetOnAxis(ap=ridx_i.t[:, ex:ex + 1], axis=0), NE * ntl - 1)
                        TS("dve", Gb + [cslot.b], [le.b], le.t[:], Gall.t[:], cslot.t[:, j:j + 1], None, ALU.is_le)
                        I("dve", "tensor_reduce", [le.b], [loc.b], out=loc.t[:], in_=le.t[:], axis=AX.X, op=ALU.add)
                        STT("dve", [Tf.b, loc.b], [loc.b], loc.t[:], Tf.t[:], 128.0, loc.t[:], ALU.mult, ALU.add)
                        if n0:
                            TS("dve", [loc.b], [loc.b], loc.t[:], loc.t[:], float(n0), None, ALU.add)
                        I("dve", "tensor_copy", [loc.b], [tok_i.b], out=tok_i.t[:, si, j, :], in_=loc.t[:])
                S.barrier()
            with ExitStack() as ws:
                W1 = kb.ring(ws, 2, [128, 8, D], BF16, "W1")
                W3 = kb.ring(ws, 2, [128, 8, D], BF16, "W3")
                W2 = kb.ring(ws, 2, [128, 8, D], BF16, "W2")
                xg = kb.ring(ws, 4, [128, D], BF16, "xg")
                ag = kb.ring(ws, 5, [128, NE], F32, "ag")
                xsT_r = kb.ring(ws, 2, [128, 8, 512], BF16, "xsT")
                gT_r = kb.ring(ws, 2, [128, 8, 512], BF16, "gT")
                sa = kb.ring(ws, 3, [128, 512], F32, "sa")
                yo = kb.ring(ws, 3, [128, D], F32, "yo")
                prev_marks = []
                nxg = 0
                nyo = 0
                nsa = 0
                nb = 0
                for ex in range(NE):
                    w1, w3, w2 = W1[ex % 2], W3[ex % 2], W2[ex % 2]
                    for (wt, src) in ((w1, exp_w1), (w3, exp_w3), (w2, exp_w2)):
                        DMA("pool", [], [wt.b], wt.t[:], src[l, ex].rearrange("(k p) c -> p k c", p=128))
                    for si, (n0, n, cap) in enumerate(segs):
                        nst = (cap + 127) // 128
                        sp_ = min(cap, 128)
                        ns = cap
                        xsT = xsT_r[nb % 2]
                        gT = gT_r[nb % 2]
                        nb += 1
                        ags = []
                        for j in range(nst):
                            g_ = xg[nxg % 4]
                            a_ = ag[nxg % 5]
                            nxg += 1
                            ags.append(a_)
                            off = bass.IndirectOffsetOnAxis(ap=tok_i.t[0:sp_, si, j, ex:ex + 1], axis=0)
                            IDMA([tok_i.b], [g_.b], g_.t[0:sp_, :], None, H2[:, :], off, NTOK - 1)
                            IDMA([tok_i.b], [a_.b], a_.t[0:sp_, :], None, AFF[:, :], off, NTOK - 1)
                            pv = pbf(4)
                            for k in range(8):
                                TR([g_.b, identb.b], [PB[4].b], pv[:, k * 128:k * 128 + sp_], g_.t[0:sp_, k * 128:(k + 1) * 128],
                                   identb.t[0:sp_, 0:sp_])
                            ACT([PB[4].b], [xsT.b], xsT.t[:, :, j * 128:j * 128 + sp_],
                                pv[:, :].rearrange("p (k t) -> p k t", t=128)[:, :, 0:sp_], AF.Copy)
                        for f in range(8):
                            fs = slice(f * 128, (f + 1) * 128)
                            ba, bb = (0, 1) if f % 2 == 0 else (2, 3)
                            for k in range(8):
                                MM([w1.b, xsT.b], [PB[ba].b], PB[ba].t[:, 0:ns], w1.t[:, k, fs], xsT.t[:, k, 0:ns], k == 0, k == 7)
                            for k in range(8):
                                MM([w3.b, xsT.b], [PB[bb].b], PB[bb].t[:, 0:ns], w3.t[:, k, fs], xsT.t[:, k, 0:ns], k == 0, k == 7)
                            s_ = sa[nsa % 3]
                            nsa += 1
                            ACT([PB[ba].b], [s_.b], s_.t[:, 0:ns], PB[ba].t[:, 0:ns], AF.Silu)
                            TT("dve", [s_.b, PB[bb].b], [gT.b], gT.t[:, f, 0:ns], s_.t[:, 0:ns], PB[bb].t[:, 0:ns], ALU.mult)
                        marks = []
                        for j in range(nst):
                            y_ = yo[nyo % 3]
                            nyo += 1
                            a_ = ags[j]
                            for half in range(2):
                                cs_ = slice(half * 512, (half + 1) * 512)
                                bank = 5 + half
                                for f in range(8):
                                    MM([gT.b, w2.b], [PB[bank].b], PB[bank].t[0:sp_, :], gT.t[:, f, j * 128:j * 128 + sp_], w2.t[:, f, cs_],
                                       f == 0, f == 7)
                                STT("dve", [PB[bank].b, a_.b, G2[si].b], [y_.b], y_.t[0:sp_, cs_], PB[bank].t[0:sp_, :],
                                    a_.t[0:sp_, ex:ex + 1], G2[si].t[0:sp_, cs_], ALU.mult, ALU.mult)
                            mk = Buf("mk")
                            marks.append(mk)
                            off = bass.IndirectOffsetOnAxis(ap=tok_i.t[0:sp_, si, j, ex:ex + 1], axis=0)
                            IDMA([y_.b, tok_i.b] + prev_marks, [mk], X[:, :], off, y_.t[0:sp_, :], None, NTOK - 1, add=True)
                        prev_marks = marks
                S.barrier()

    def final_norm():
        with ExitStack() as ms:
            fg = kb.sb(ms, [128, D], F32, "fg")
            bcast_row(fg.t[:], final_g, [fg.b])
            xt = kb.ring(ms, 2, [128, D], F32, "nxt")
            junk = kb.sb(ms, [128, D], BF16, "njunk")
            ot = kb.ring(ms, 2, [128, D], F32, "not")
            msr = kb.sb(ms, [128, 1], F32, "nmsr")
            rstd = kb.sb(ms, [128, 1], F32, "nrstd")
            for t in range(NTL):
                x = xt[t % 2]
                o = ot[t % 2]
                DMA("sp", [], [x.b], x.t[:], X[t * 128:(t + 1) * 128, :])
                ACT([x.b], [junk.b, msr.b], junk.t[:], x.t[:], AF.Square, scale=1.0 / 32.0, accum_out=msr.t[:, 0:1])
                RSQ(rstd, rstd.t[:, 0:1], msr, msr.t[:, 0:1])
                STT("dve", [x.b, rstd.b, fg.b], [o.b], o.t[:], x.t[:], rstd.t[:, 0:1], fg.t[:], ALU.mult, ALU.mult)
                DMA("sp", [o.b], [], out[t * 128:(t + 1) * 128, :], o.t[:])
            S.barrier()

    S.barrier()
    for l in range(depth_run):
        need_ctx = l < DEPTH - 1
        if l % 2 == 0:
            even_mixer(l, need_ctx)
        else:
            odd_mixer(l, need_ctx)
        if stop_after == (l, "mixer"):
            break
        ffn(l, need_ctx)
    print("[kernel] ops before final:", S.nops)
    S.force = True
    if dbg:
        xd = nc.dram_tensor("xdbg", [NTOK, D], F32, kind="ExternalOutput").ap()
        DMA("sp", [], [], xd[:, :], X[:, :])
    final_norm()
    S.barrier()
    with nc.allow_non_contiguous_dma(reason="tiny strided parameter vectors"):
        with nc.Block() as block:
            S.emit(block)


def _rope_tables():
    def tab(rot_dim):
        rows = NLAT // 64
        row = np.repeat(np.arange(rows, dtype=np.float32), 64)
        col = np.tile(np.arange(64, dtype=np.float32), rows)
        n_freq = rot_dim // 4
        inv = (np.float32(10000.0) ** (-np.arange(n_freq, dtype=np.float32) / np.float32(n_freq))).astype(np.float32)
        ang = np.concatenate([row[:, None] * inv[None, :], col[:, None] * inv[None, :]], axis=-1).astype(np.float32)
        t = np.concatenate([np.cos(ang), np.sin(ang)], axis=-1).astype(np.float32)
        c = np.concatenate([np.ones((NCTX, rot_dim // 2), np.float32), np.zeros((NCTX, rot_dim // 2), np.float32)], axis=-1)
        full = np.concatenate([t, c], axis=0)
        return np.ascontiguousarray(full.reshape(NT, 128, rot_dim).transpose(1, 0, 2).reshape(128, NT * rot_dim))
    return tab(64), tab(32)


_SHARED = ["c_ctx", "mod_w", "mod_b", "norm1_g", "norm2_g", "ev_w_in", "ev_sink", "ev_sgu_norm_g", "ev_sgu_w", "ev_sgu_b",
           "ev_w_out", "od_w_in", "od_q_norm_g", "od_w_uq", "od_kv_norm_g", "od_w_ukv", "od_conv_w", "od_w_out", "router_w",
           "exp_w1", "exp_w3", "exp_w2", "final_g"]


def make_in_maps(inputs, cores):
    ropeA, ropeC = _rope_tables()
    shared = {k: np.ascontiguousarray(np.asarray(inputs[k], dtype=np.float32)) for k in _SHARED}
    maps = []
    for b in cores:
        m = dict(shared)
        m["x"] = np.ascontiguousarray(np.asarray(inputs["x"][b], dtype=np.float32))
        m["ctx"] = np.ascontiguousarray(np.asarray(inputs["ctx"][b], dtype=np.float32))
        m["c"] = np.ascontiguousarray(np.asarray(inputs["c"][b], dtype=np.float32))
        m["ropeA"] = ropeA
        m["ropeC"] = ropeC
        maps.append(m)
    return maps


def kernel(**inputs):
    nc = build_program()
    maps = make_in_maps(inputs, list(range(8)))
    res = run_bass_kernel_spmd(nc, maps, core_ids=list(range(8)))
    return np.stack([np.asarray(r["out"], dtype=np.float32) for r in res.results], axis=0)
```

```python
import os
import numpy as np
from contextlib import ExitStack
import concourse.bass as bass
import concourse.mybir as mybir
from concourse.bass_utils import run_bass_kernel_spmd

F32 = mybir.dt.float32
BF16 = mybir.dt.bfloat16
I32 = mybir.dt.int32
AF = mybir.ActivationFunctionType
ALU = mybir.AluOpType
AX = mybir.AxisListType

D = 1024
NLAT = 4096
NCTX = 256
NTOK = NLAT + NCTX
NT = NTOK // 128
NTL = NLAT // 128
DEPTH = 4
NE = 16
EPS = 1e-6
EV_IN = 1792
OD_IN = 2208


class Buf:
    __slots__ = ("name", "w", "r")

    def __init__(self, name=""):
        self.name = name
        self.w = None
        self.r = []


class TB:
    def __init__(self, t, name):
        self.t = t
        self.b = Buf(name)


def _free(ap):
    n = 1
    for d in tuple(ap.shape)[1:]:
        n *= int(d)
    return n


_DTB = {F32: 4, BF16: 2, I32: 4}


class Sched:
    ENGS = ("pe", "act", "dve", "pool", "sp")

    def __init__(self, nc, es, ndma=None):
        ndma = ndma or {"sp": 14, "pool": 14}
        self.nc = nc
        self.sems = []
        self.esem = {}
        for e in self.ENGS:
            self.esem[e] = len(self.sems)
            self.sems.append(es.enter_context(nc.semaphore("s_" + e)))
        self.dpool = {}
        for e, n in ndma.items():
            self.dpool[e] = []
            for i in range(n):
                self.dpool[e].append(len(self.sems))
                self.sems.append(es.enter_context(nc.semaphore("d_%s%d" % (e, i))))
        self.nodes = []
        self.tbl = {}
        self.segs = [0]
        self.nops = 0
        self.limit = None
        self.force = False
        self.ninst = 0
        self.reorder = True
        self.noreorder = set(int(x) for x in os.environ.get("KNOREORDER", "").split(",") if x)
        self.cur_seg = -1

    def _cost(self, eng, name, a, kw, dma):
        try:
            if dma:
                nb = None
                for ap in (kw["out"], kw["in_"]):
                    n_ = 1
                    for d in tuple(ap.shape):
                        n_ *= int(d)
                    n_ *= _DTB.get(ap.dtype, 4)
                    nb = n_ if nb is None else min(nb, n_)
                if "indirect" in name:
                    return 1.0, 3.0 + nb / 150e3
                occ = 0.6 if eng == "pool" else 0.08
                return occ, 2.2 + nb / 380e3
            if eng == "pe":
                if name == "matmul":
                    rhs = kw["rhs"]
                    n = max(_free(rhs), 64)
                    if rhs.dtype == F32:
                        n *= 4
                    t = 0.03 + n / 2400.0
                else:
                    t = 0.09
                return t, t + 0.05
            out = kw.get("out", a[0] if a else None)
            f = _free(out) if out is not None else 64
            if eng == "act":
                t = 0.22 + f / 1200.0
            elif eng == "dve":
                t = 0.08 + f / 960.0
            else:
                t = 0.12 + f / 450.0
            return t, t + 0.05
        except Exception:
            return 0.3, 0.4

    def op(self, eng, name, reads, writes, *a, dma=False, **kw):
        self.nops += 1
        if self.limit is not None and self.nops > self.limit and not self.force:
            return None
        nid = len(self.nodes)
        deps = set()
        for b in reads:
            if b.w is not None:
                deps.add(b.w)
        for b in writes:
            if b.w is not None:
                deps.add(b.w)
            deps.update(b.r)
        occ, lat = self._cost(eng, name, a, kw, dma)
        tbl = None
        if eng == "act" and name == "activation":
            f = kw.get("func")
            if f == AF.Sqrt:
                tbl = "sqrt"
            elif f in (AF.Exp, AF.Tanh):
                tbl = "exp"
            elif f == AF.Silu:
                tbl = "silu"
            elif f == AF.Sigmoid:
                tbl = "sig"
        self.tbl[nid] = tbl
        self.nodes.append((eng, (name, a, kw), dma, deps, occ, lat))
        for b in reads:
            b.r.append(nid)
        for b in writes:
            b.w = nid
            b.r = []
        return nid

    def barrier(self):
        if self.segs[-1] != len(self.nodes):
            self.segs.append(len(self.nodes))

    def _schedule(self, lo, hi):
        import heapq
        nodes = self.nodes
        if not self.reorder or self.cur_seg in self.noreorder:
            return list(range(lo, hi))
        indeg = {}
        succ = {}
        ready_t = {}
        for n in range(lo, hi):
            c = 0
            for d in nodes[n][3]:
                if d >= lo:
                    c += 1
                    succ.setdefault(d, []).append(n)
            indeg[n] = c
            ready_t[n] = 0.0
        pend = {e: [] for e in self.ENGS}
        avail = {e: [] for e in self.ENGS}
        for n in range(lo, hi):
            if indeg[n] == 0:
                heapq.heappush(pend[nodes[n][0]], (0.0, n))
        t_eng = {e: 0.0 for e in self.ENGS}
        dma_free = 0.0
        order = []
        cur_tbl = None
        tblmap = self.tbl
        table_aware = not os.environ.get("KNOTBL")
        total = hi - lo
        while len(order) < total:
            best = None
            for e in self.ENGS:
                pe_, av = pend[e], avail[e]
                te = t_eng[e]
                while pe_ and pe_[0][0] <= te:
                    heapq.heappush(av, heapq.heappop(pe_)[1])
                if av:
                    pick = av[0]
                    if e == "act" and table_aware and len(av) > 1:
                        t0_ = tblmap.get(pick)
                        if t0_ is not None and t0_ != cur_tbl:
                            best_alt = None
                            for x_ in av:
                                tx = tblmap.get(x_)
                                if (tx is None or tx == cur_tbl) and x_ < pick + 400 and (best_alt is None or x_ < best_alt):
                                    best_alt = x_
                            if best_alt is not None:
                                pick = best_alt
                    cand = (te, pick, e, True)
                elif pe_:
                    cand = (pe_[0][0], pe_[0][1], e, False)
                else:
                    continue
                if best is None or cand[:2] < best[:2]:
                    best = cand
            st, n, e, from_av = best
            if from_av:
                if avail[e][0] == n:
                    heapq.heappop(avail[e])
                else:
                    avail[e].remove(n)
                    heapq.heapify(avail[e])
            else:
                heapq.heappop(pend[e])
            eng, fn, dma, deps, occ, lat = nodes[n]
            if e == "act":
                tn = tblmap.get(n)
                if tn is not None and tn != cur_tbl:
                    cur_tbl = tn
                    st += 1.3
            if dma:
                s2 = max(st, dma_free)
                fin = s2 + lat
                dma_free = s2 + max(lat - (3.0 if "indirect" in fn[0] else 2.2), 0.0)
            else:
                fin = st + lat
            t_eng[e] = st + occ
            order.append(n)
            if os.environ.get("KTRACE") and self.cur_seg == int(os.environ["KTRACE"]) and len(order) % 150 == 0:
                print("SIM %5d %-5s %-22s st=%8.2f fin=%8.2f" % (n, e, fn[0], st, fin))
            for m in succ.get(n, ()):
                hop = 0.05 if nodes[m][0] == e and not dma else 0.35
                rt = fin + hop
                if rt > ready_t[m]:
                    ready_t[m] = rt
                indeg[m] -= 1
                if indeg[m] == 0:
                    heapq.heappush(pend[nodes[m][0]], (ready_t[m], m))
        self.est_time = getattr(self, "est_time", 0.0) + max(t_eng.values())
        if os.environ.get("KVERBOSE"):
            print("[kernel] seg", self.cur_seg, lo, hi, "est", round(max(t_eng.values()), 1), {e: round(v, 1) for e, v in t_eng.items()})
        return order

    def emit(self, block):
        nodes = self.nodes
        if self.segs[-1] != len(nodes):
            self.segs.append(len(nodes))
        prog = {e: [] for e in self.ENGS}
        cnt = {e: 0 for e in self.ENGS}
        known = {e: {} for e in self.ENGS}
        dval = {k: 0 for e in self.dpool for k in self.dpool[e]}
        dnext = {e: 0 for e in self.dpool}
        ev = {}
        pe_own = self.esem["pe"]
        for si in range(len(self.segs) - 1):
            lo, hi = self.segs[si], self.segs[si + 1]
            self.cur_seg = si
            if os.environ.get("KVERBOSE"):
                print("[kernel] segment", si, lo, hi)
            for n in self._schedule(lo, hi):
                eng, fn, dma, deps, occ, lat = nodes[n]
                kn = known[eng]
                waits = {}
                for d in deps:
                    k, v = ev[d]
                    if eng == "pe" and k == pe_own:
                        continue
                    if kn.get(k, 0) >= v:
                        continue
                    if waits.get(k, 0) < v:
                        waits[k] = v
                if dma:
                    pool = self.dpool[eng]
                    k = pool[dnext[eng] % len(pool)]
                    dnext[eng] += 1
                    if dval[k] > 0 and kn.get(k, 0) < dval[k] and waits.get(k, 0) < dval[k]:
                        waits[k] = dval[k]
                    dval[k] += 16
                    ev[n] = (k, dval[k])
                    inc = 16
                else:
                    k = self.esem[eng]
                    cnt[eng] += 1
                    ev[n] = (k, cnt[eng])
                    inc = 1
                for k2, v in waits.items():
                    kn[k2] = v
                prog[eng].append((list(waits.items()), fn, k, inc))
                self.ninst += 1 + len(waits)
            targets = [(self.esem[e], cnt[e]) for e in self.ENGS if cnt[e] > 0]
            targets += [(k, v) for k, v in dval.items() if v > 0]
            for e in self.ENGS:
                waits = []
                for k, v in targets:
                    if known[e].get(k, 0) >= v:
                        continue
                    known[e][k] = v
                    waits.append((k, v))
                if waits:
                    prog[e].append((waits, None, None, 0))
                    self.ninst += len(waits)
        sems = self.sems

        def mk(p):
            def body(engine):
                regcache = {}
                for waits, fn, k, inc in p:
                    for k2, v in waits:
                        engine.wait_ge(sems[k2], v)
                    if fn is not None:
                        kw = fn[2]
                        if "bounds_check" in kw:
                            bv = kw["bounds_check"]
                            if bv not in regcache:
                                regcache[bv] = engine.to_reg(bv)
                            kw = dict(kw)
                            kw["bounds_check"] = regcache[bv]
                        fn = (fn[0], fn[1], kw)
                        try:
                            ins = getattr(engine, fn[0])(*fn[1], **fn[2])
                        except Exception:
                            print("[kernel] failed to emit", fn[0], fn[1], fn[2])
                            raise
                        ins.then_inc(sems[k], inc)
            return body

        block.tensor(mk(prog["pe"]))
        block.scalar(mk(prog["act"]))
        block.vector(mk(prog["dve"]))
        block.gpsimd(mk(prog["pool"]))
        block.sync(mk(prog["sp"]))
        print("[kernel] instructions incl. waits:", self.ninst, "est us:", round(getattr(self, "est_time", 0.0), 1))


class KB:
    def __init__(self, nc, S, es):
        self.nc = nc
        self.S = S
        self.es = es
        self.n = 0

    def sb(self, es, shape, dt, name=None):
        self.n += 1
        name = (name or "t") + "_%d" % self.n
        return TB(es.enter_context(self.nc.sbuf_tensor(name, list(shape), dt)), name)

    def ring(self, es, n, shape, dt, name):
        return [self.sb(es, shape, dt, name + str(i)) for i in range(n)]


import os
VERBOSE = bool(os.environ.get("KVERBOSE"))


def build_program(depth_run=DEPTH, dbg=False, stop_after=None, limit=None):
    nc = bass.Bass("TRN2", target_bir_lowering=False)
    es = ExitStack()
    with es:
        _build(nc, es, depth_run, dbg, stop_after, limit)
    return nc


def _dram_in(nc, name, shape, dt=F32):
    return nc.dram_tensor(name, list(shape), dt, kind="ExternalInput").ap()


def _build(nc, es, depth_run, dbg, stop_after, limit=None):
    x_in = _dram_in(nc, "x", [NLAT, D])
    ctx_in = _dram_in(nc, "ctx", [NCTX, D])
    c_in = _dram_in(nc, "c", [D])
    cctx_in = _dram_in(nc, "c_ctx", [D])
    mod_w = _dram_in(nc, "mod_w", [DEPTH, D, 6 * D])
    mod_b = _dram_in(nc, "mod_b", [DEPTH, 6 * D])
    norm1_g = _dram_in(nc, "norm1_g", [DEPTH, D])
    norm2_g = _dram_in(nc, "norm2_g", [DEPTH, D])
    ev_w_in = _dram_in(nc, "ev_w_in", [2, D, EV_IN])
    ev_sink = _dram_in(nc, "ev_sink", [2, 8])
    ev_sgu_norm_g = _dram_in(nc, "ev_sgu_norm_g", [2, 512])
    ev_sgu_w = _dram_in(nc, "ev_sgu_w", [2, 4, 128, 128])
    ev_sgu_b = _dram_in(nc, "ev_sgu_b", [2, 4, 128])
    ev_w_out = _dram_in(nc, "ev_w_out", [2, D, D])
    od_w_in = _dram_in(nc, "od_w_in", [2, D, OD_IN])
    od_q_norm_g = _dram_in(nc, "od_q_norm_g", [2, 384])
    od_w_uq = _dram_in(nc, "od_w_uq", [2, 384, 768])
    od_kv_norm_g = _dram_in(nc, "od_kv_norm_g", [2, 256])
    od_w_ukv = _dram_in(nc, "od_w_ukv", [2, 256, 1024])
    od_conv_w = _dram_in(nc, "od_conv_w", [2, 3, 512])
    od_w_out = _dram_in(nc, "od_w_out", [2, D, D])
    router_w = _dram_in(nc, "router_w", [DEPTH, D, NE])
    exp_w1 = _dram_in(nc, "exp_w1", [DEPTH, NE, D, D])
    exp_w3 = _dram_in(nc, "exp_w3", [DEPTH, NE, D, D])
    exp_w2 = _dram_in(nc, "exp_w2", [DEPTH, NE, D, D])
    final_g = _dram_in(nc, "final_g", [D])
    ropeA = _dram_in(nc, "ropeA", [128, NT * 64])
    ropeC = _dram_in(nc, "ropeC", [128, NT * 32])
    out = nc.dram_tensor("out", [NLAT, D], F32, kind="ExternalOutput").ap()

    X = nc.dram_tensor("Xres", [NTOK, D], F32, kind="Internal").ap()
    MROW = nc.dram_tensor("mrow", [DEPTH, 2, 6 * D], F32, kind="Internal").ap()
    H2 = nc.dram_tensor("h2", [NTOK, D], BF16, kind="Internal").ap()
    AFF = nc.dram_tensor("aff", [NTOK, NE], F32, kind="Internal").ap()
    CS = nc.dram_tensor("cs", [NE * NTL, 128], F32, kind="Internal").ap()
    CT = nc.dram_tensor("ct", [NE * NTL], F32, kind="Internal").ap()
    QTD = nc.dram_tensor("qtd", [8, 96, NTOK], BF16, kind="Internal").ap()
    OTD = nc.dram_tensor("otd", [8, 64, NTOK], BF16, kind="Internal").ap()

    S = Sched(nc, es)
    S.limit = limit
    kb = KB(nc, S, es)
    Xb = [Buf("X%d" % t) for t in range(NT)]

    def I(eng, name, reads, writes, *a, **kw):
        S.op(eng, name, reads, writes, *a, **kw)

    def DMA(eng, reads, writes, out, in_):
        S.op(eng, "dma_start", reads, writes, out=out, in_=in_, dma=True)

    def MM(reads, writes, out, lhsT, rhs, start, stop):
        S.op("pe", "matmul", reads, writes, out, lhsT=lhsT, rhs=rhs, start=start, stop=stop)

    def TR(reads, writes, out, in_, ident):
        S.op("pe", "transpose", reads, writes, out=out, in_=in_, identity=ident)

    def ACT(reads, writes, out, in_, func, **kw):
        S.op("act", "activation", reads, writes, out=out, in_=in_, func=func, **kw)

    def TT(eng, reads, writes, out, in0, in1, op):
        S.op(eng, "tensor_tensor", reads, writes, out=out, in0=in0, in1=in1, op=op)

    def TS(eng, reads, writes, out, in0, s1, s2, op0, op1=None, **kw):
        if op1 is None:
            S.op(eng, "tensor_scalar", reads, writes, out=out, in0=in0, scalar1=s1, scalar2=None, op0=op0, **kw)
        else:
            S.op(eng, "tensor_scalar", reads, writes, out=out, in0=in0, scalar1=s1, scalar2=s2, op0=op0, op1=op1, **kw)

    def STT(eng, reads, writes, out, in0, scalar, in1, op0, op1):
        S.op(eng, "scalar_tensor_tensor", reads, writes, out=out, in0=in0, scalar=scalar, in1=in1, op0=op0, op1=op1)

    def RSQ(dst, dst_ap, src, src_ap):
        ACT([src.b], [dst.b], dst_ap, src_ap, AF.Sqrt, bias=epsb.t[0:dst_ap.shape[0], 0:1])
        I("dve", "reciprocal", [dst.b], [dst.b], out=dst_ap, in_=dst_ap)

    def IDMA(reads, writes, out, out_off, in_, in_off, bound, add=False):
        kw = dict(out=out, out_offset=out_off, in_=in_, in_offset=in_off, bounds_check=bound, oob_is_err=False)
        if add:
            kw["compute_op"] = ALU.add
        S.op("pool", "indirect_dma_start", reads, writes, dma=True, **kw)

    PB = [TB(es.enter_context(nc.psum_tensor("pb%d" % i, [128, 512], F32)), "pb%d" % i) for i in range(8)]

    def pbf(i):
        return PB[i].t[:].bitcast(BF16)

    identb = kb.sb(es, [128, 128], BF16, "identb")
    identf = kb.sb(es, [128, 128], F32, "identf")
    onesf = kb.sb(es, [128, 128], F32, "onesf")
    maskneg = kb.sb(es, [128, 2, 512], BF16, "maskneg")
    e65 = kb.sb(es, [1, 65], BF16, "e65")
    epsb = kb.sb(es, [128, 1], F32, "epsb")
    ropeA_sb = kb.sb(es, [128, NT, 64], F32, "ropeA")
    ropeC_sb = kb.sb(es, [128, NT, 32], F32, "ropeC")

    for idt in (identb, identf):
        I("pool", "memset", [], [idt.b], idt.t[:], 0.0)
        I("pool", "affine_select", [idt.b], [idt.b], out=idt.t[:], in_=idt.t[:], pattern=[[-1, 128]],
          compare_op=ALU.not_equal, fill=1.0, base=0, channel_multiplier=1)
    I("pool", "memset", [], [onesf.b], onesf.t[:], 1.0)
    I("pool", "memset", [], [maskneg.b], maskneg.t[:], 0.0)
    I("pool", "affine_select", [maskneg.b], [maskneg.b], out=maskneg.t[:, 0, :], in_=maskneg.t[:, 0, :],
      pattern=[[0, 4], [-1, 128]], compare_op=ALU.is_ge, fill=-30000.0, base=0, channel_multiplier=1)
    I("pool", "affine_select", [maskneg.b], [maskneg.b], out=maskneg.t[:, 1, :], in_=maskneg.t[:, 1, :],
      pattern=[[0, 4], [1, 128]], compare_op=ALU.is_ge, fill=-30000.0, base=0, channel_multiplier=-1)
    I("pool", "memset", [], [epsb.b], epsb.t[:], EPS)
    I("pool", "memset", [], [e65.b], e65.t[:], 0.0)
    I("pool", "memset", [e65.b], [e65.b], e65.t[0:1, 64:65], 1.0)
    DMA("sp", [], [ropeA_sb.b], ropeA_sb.t[:].rearrange("p t f -> p (t f)"), ropeA[:, :])
    DMA("sp", [], [ropeC_sb.b], ropeC_sb.t[:].rearrange("p t f -> p (t f)"), ropeC[:, :])
    for t in range(NT):
        src = x_in[t * 128:(t + 1) * 128, :] if t < NTL else ctx_in[(t - NTL) * 128:(t - NTL + 1) * 128, :]
        DMA("sp", [], [Xb[t]], X[t * 128:(t + 1) * 128, :], src)

    with ExitStack() as ms:
        sc = kb.sb(ms, [128, 8, 2], F32, "sc")
        modb2 = kb.sb(ms, [2, 6 * D], F32, "modb2")
        mrow_sb = kb.sb(ms, [2, 6 * D], F32, "mrow_sb")
        mw = kb.ring(ms, 2, [128, 8, 512], F32, "mw")
        DMA("sp", [], [sc.b], sc.t[:, :, 0:1], c_in.rearrange("(k p o) -> p k o", p=128, o=1))
        DMA("sp", [], [sc.b], sc.t[:, :, 1:2], cctx_in.rearrange("(k p o) -> p k o", p=128, o=1))
        ACT([sc.b], [sc.b], sc.t[:], sc.t[:], AF.Silu)
        i = 0
        for l in range(depth_run):
            for r in range(2):
                DMA("sp", [], [modb2.b], modb2.t[r:r + 1, :], mod_b[l:l + 1, :])
            for n in range(12):
                m = mw[i % 2]
                i += 1
                DMA("sp", [], [m.b], m.t[:], mod_w[l].rearrange("(k p) c -> p k c", p=128)[:, :, n * 512:(n + 1) * 512])
                for k in range(8):
                    MM([sc.b, m.b], [PB[0].b], PB[0].t[0:2, :], sc.t[:, k, :], m.t[:, k, :], k == 0, k == 7)
                TT("dve", [PB[0].b, modb2.b], [mrow_sb.b], mrow_sb.t[:, n * 512:(n + 1) * 512], PB[0].t[0:2, :],
                   modb2.t[:, n * 512:(n + 1) * 512], ALU.add)
            DMA("sp", [mrow_sb.b], [], MROW[l], mrow_sb.t[:])
        S.barrier()

    def bcast_row(dst, row_ap, bufs_w):
        DMA("sp", [], bufs_w, dst, row_ap.partition_broadcast(128))

    def load_mod(ms, l, j0, gvec, want_g=True, want_as=True):
        res = []
        for r in range(2):
            A = Sh = G = None
            if want_as:
                A = kb.sb(ms, [128, D], F32, "A")
                Sh = kb.sb(ms, [128, D], F32, "Sh")
            if want_g:
                G = kb.sb(ms, [128, D], F32, "G")
            res.append((A, Sh, G))
        with ExitStack() as tmpst:
            gb = None
            if want_as:
                gb = kb.sb(tmpst, [128, D], F32, "gb")
                bcast_row(gb.t[:], gvec, [gb.b])
            for r in range(2):
                A, Sh, G = res[r]
                if want_as:
                    bcast_row(Sh.t[:], MROW[l, r, j0 * D:(j0 + 1) * D], [Sh.b])
                    bcast_row(A.t[:], MROW[l, r, (j0 + 1) * D:(j0 + 2) * D], [A.b])
                    STT("dve", [A.b, gb.b], [A.b], A.t[:], A.t[:], 1.0, gb.t[:], ALU.add, ALU.mult)
                if want_g:
                    bcast_row(G.t[:], MROW[l, r, (j0 + 2) * D:(j0 + 3) * D], [G.b])
            if want_as:
                S.barrier()
        return res

    def norm_mod(xt, A, Sh, hout, ms_t, rstd_t, junk, tmp):
        ACT([xt.b], [ms_t.b, hout.b], hout.t[:], xt.t[:], AF.Square, scale=1.0 / 32.0, accum_out=ms_t.t[:, 0:1])
        RSQ(rstd_t, rstd_t.t[:, 0:1], ms_t, ms_t.t[:, 0:1])
        STT("dve", [xt.b, rstd_t.b, A.b], [tmp.b], tmp.t[:], xt.t[:], rstd_t.t[:, 0:1], A.t[:], ALU.mult, ALU.mult)
        TT("dve", [tmp.b, Sh.b], [hout.b], hout.t[:], tmp.t[:], Sh.t[:], ALU.add)

    def transpose8(h, hT, bank):
        pv = pbf(bank)
        for k in range(8):
            TR([h.b, identb.b], [PB[bank].b], pv[:, k * 128:(k + 1) * 128], h.t[:, k * 128:(k + 1) * 128], identb.t[:])
        ACT([PB[bank].b], [hT.b], hT.t[:].rearrange("p k t -> p (k t)"), pv[:, :], AF.Copy)

    def wout_residual(t, ON, cT, Wo_o, Wo_c, G, x, yn, o, ybanks=(0, 1)):
        for half in range(2):
            cs_ = slice(half * 512, (half + 1) * 512)
            yb = ybanks[half]
            for h in range(8):
                MM([ON.b, Wo_o.b], [PB[yb].b], PB[yb].t[:, :], ON.t[:, h, :], Wo_o.t[:, h, cs_], h == 0, False)
            for k in range(4):
                MM([cT.b, Wo_c.b], [PB[yb].b], PB[yb].t[:, :], cT.t[:, k, :], Wo_c.t[:, k, cs_], False, k == 3)
            TT("dve", [PB[yb].b, G.b], [o.b], o.t[:, cs_], PB[yb].t[:, :], G.t[:, cs_], ALU.mult)
        TT("dve", [o.b, x.b], [o.b], o.t[:], o.t[:], x.t[:], ALU.add)
        DMA("sp", [o.b], [Xb[t]], X[t * 128:(t + 1) * 128, :], o.t[:])

    def attn_norm(pbank, bbank, rs, Osb, on_out, nq, on_buf):
        I("dve", "reciprocal", [PB[pbank].b], [rs.b], out=rs.t[64:65, 0:nq], in_=PB[pbank].t[64:65, 0:nq])
        ACT([PB[pbank].b], [Osb.b], Osb.t[:, 0:nq], PB[pbank].t[0:64, 0:nq], AF.Copy)
        MM([onesf.b, rs.b], [PB[bbank].b], PB[bbank].t[0:64, 0:nq], onesf.t[64:65, 0:64], rs.t[64:65, 0:nq], True, True)
        TT("dve", [Osb.b, PB[bbank].b], [on_buf], on_out, Osb.t[:, 0:nq], PB[bbank].t[0:64, 0:nq], ALU.mult)

    def even_mixer(l, need_ctx):
        i = l // 2
        with ExitStack() as ms:
            Win = kb.sb(ms, [128, 8, EV_IN], BF16, "Win")
            Wo_o = kb.sb(ms, [64, 8, D], BF16, "Wo_o")
            Wo_s = kb.sb(ms, [128, 4, D], BF16, "Wo_s")
            Wsn = kb.sb(ms, [128, 4, 128], BF16, "Wsn")
            WsT = kb.sb(ms, [128, 4, 128], BF16, "WsT")
            bsT = kb.sb(ms, [128, 4], F32, "bsT")
            sgug = kb.sb(ms, [128, 512], F32, "sgug")
            snk = kb.sb(ms, [1, 8], F32, "snk")
            esink = kb.sb(ms, [1, 8, 128], BF16, "esink")
            DMA("pool", [], [Win.b], Win.t[:], ev_w_in[i].rearrange("(k p) c -> p k c", p=128))
            DMA("pool", [], [Wo_o.b], Wo_o.t[:], ev_w_out[i, 0:512, :].rearrange("(h d) c -> d h c", d=64))
            DMA("pool", [], [Wo_s.b], Wo_s.t[:], ev_w_out[i, 512:1024, :].rearrange("(k p) c -> p k c", p=128))
            DMA("pool", [], [Wsn.b], Wsn.t[:], ev_sgu_w[i].rearrange("g p q -> p g q"))
            DMA("sp", [], [bsT.b], bsT.t[:].rearrange("p (g o) -> p g o", o=1), ev_sgu_b[i].rearrange("g (p o) -> p g o", o=1))
            bcast_row(sgug.t[:], ev_sgu_norm_g[i], [sgug.b])
            DMA("sp", [], [snk.b], snk.t[:], ev_sink[i:i + 1, :])
            ACT([snk.b], [snk.b], snk.t[:], snk.t[:], AF.Exp)
            I("dve", "tensor_copy", [snk.b], [esink.b], out=esink.t[:],
              in_=snk.t[:].rearrange("o (h u) -> o h u", u=1).to_broadcast([1, 8, 128]))
            pv = pbf(4)
            for g in range(4):
                TR([Wsn.b, identb.b], [PB[4].b], pv[:, g * 128:(g + 1) * 128], Wsn.t[:, g, :], identb.t[:])
            ACT([PB[4].b], [WsT.b], WsT.t[:].rearrange("p g q -> p (g q)"), pv[:, 0:512], AF.Copy)
            mods = load_mod(ms, l, 0, norm1_g[l])

            xt = kb.ring(ms, 4, [128, D], F32, "xt")
            junk = None
            tmp_r = kb.ring(ms, 2, [128, D], F32, "tmp")
            hb_r = kb.ring(ms, 2, [128, D], BF16, "hb")
            hT_r = kb.ring(ms, 2, [128, 8, 128], BF16, "hT")
            msr_r = kb.ring(ms, 2, [128, 1], F32, "msr")
            rstd_r = kb.ring(ms, 2, [128, 1], F32, "rstd")
            zs_r = kb.ring(ms, 2, [128, 12, 64], F32, "zs")
            zr_r = kb.ring(ms, 2, [128, 12, 64], BF16, "zr")
            r1_r = kb.ring(ms, 1, [128, 12, 32], F32, "r1")
            r2_r = kb.ring(ms, 1, [128, 12, 32], F32, "r2")
            r3_r = kb.ring(ms, 1, [128, 12, 32], F32, "r3")
            r4_r = kb.ring(ms, 1, [128, 12, 32], F32, "r4")
            KT = kb.ring(ms, 5, [64, 2, 128], BF16, "KT")
            VA = kb.ring(ms, 5, [128, 2, 65], BF16, "VA")
            KTc = kb.ring(ms, 2, [64, 2, 128], BF16, "KTc")
            VAc = kb.ring(ms, 2, [128, 2, 65], BF16, "VAc")
            QT = kb.ring(ms, 3, [64, 8, 128], BF16, "QT")
            sT = kb.ring(ms, 3, [128, 4, 128], BF16, "sT")
            gu_r = kb.ring(ms, 2, [128, 2, 512], F32, "gu")
            g1_r = kb.ring(ms, 2, [128, 2, 512], F32, "g1")
            bst_r = kb.ring(ms, 2, [128, 6], F32, "bst")
            mv_r = kb.ring(ms, 2, [128, 2], F32, "mv")
            lrs_r = kb.ring(ms, 2, [128, 1], F32, "lrs")
            vn_r = kb.ring(ms, 2, [128, 512], F32, "vn")
            vnb_r = kb.ring(ms, 2, [128, 512], BF16, "vnb")
            sb_r = kb.ring(ms, 2, [128, 512], BF16, "s")
            PT = kb.ring(ms, 3, [128, 512], BF16, "PT")
            rs_r = kb.ring(ms, 2, [65, 512], F32, "rs")
            Osb_r = kb.ring(ms, 2, [64, 512], F32, "Osb")
            ON_r = kb.ring(ms, 2, [64, 8, 128], BF16, "ON")
            xo_r = kb.ring(ms, 2, [128, D], F32, "xo")
            for v in VA + VAc:
                I("pool", "memset", [], [v.b], v.t[:], 1.0)

            def kv_of(t):
                return (KTc[t - NTL], VAc[t - NTL]) if t >= NTL else (KT[t % 5], VA[t % 5])

            def phaseA(t):
                if VERBOSE:
                    print("even phaseA", t, "op", S.nops)
                r = 1 if t >= NTL else 0
                A, Sh, G = mods[r]
                x = xt[t % 4]
                i2 = t % 2
                tmp, hb, hT, msr, rstd, zs, zr = tmp_r[i2], hb_r[i2], hT_r[i2], msr_r[i2], rstd_r[i2], zs_r[i2], zr_r[i2]
                r1, r2, r3, r4 = r1_r[0], r2_r[0], r3_r[0], r4_r[0]
                gu, g1, bst, mv, lrs, vn, vnb, sb_ = gu_r[i2], g1_r[i2], bst_r[i2], mv_r[i2], lrs_r[i2], vn_r[i2], vnb_r[i2], sb_r[i2]
                DMA("sp", [Xb[t]], [x.b], x.t[:], X[t * 128:(t + 1) * 128, :])
                norm_mod(x, A, Sh, hb, msr, rstd, junk, tmp)
                transpose8(hb, hT, 4)
                for (bank, c0, c1) in ((0, 0, 512), (1, 512, 768)):
                    for k in range(8):
                        MM([hT.b, Win.b], [PB[bank].b], PB[bank].t[:, 0:c1 - c0], hT.t[:, k, :], Win.t[:, k, c0:c1], k == 0, k == 7)
                zsf = zs.t[:].rearrange("p h d -> p (h d)")
                ACT([PB[0].b], [zs.b], zsf[:, 0:512], PB[0].t[:, :], AF.Copy)
                ACT([PB[1].b], [zs.b], zsf[:, 512:768], PB[1].t[:, 0:256], AF.Copy)
                for (bank, c0) in ((2, 768), (3, 1280)):
                    for k in range(8):
                        MM([hT.b, Win.b], [PB[bank].b], PB[bank].t[:, :], hT.t[:, k, :], Win.t[:, k, c0:c0 + 512], k == 0, k == 7)
                cos = ropeA_sb.t[:, t, 0:32].rearrange("p (o f) -> p o f", o=1).to_broadcast([128, 12, 32])
                sin = ropeA_sb.t[:, t, 32:64].rearrange("p (o f) -> p o f", o=1).to_broadcast([128, 12, 32])
                x1 = zs.t[:, :, 0:32]
                x2 = zs.t[:, :, 32:64]
                TT("dve", [zs.b, ropeA_sb.b], [r1.b], r1.t[:], x1, cos, ALU.mult)
                TT("pool", [zs.b, ropeA_sb.b], [r2.b], r2.t[:], x2, sin, ALU.mult)
                TT("dve", [zs.b, ropeA_sb.b], [r3.b], r3.t[:], x2, cos, ALU.mult)
                TT("pool", [zs.b, ropeA_sb.b], [r4.b], r4.t[:], x1, sin, ALU.mult)
                TT("dve", [r1.b, r2.b], [zr.b], zr.t[:, :, 0:32], r1.t[:], r2.t[:], ALU.subtract)
                TT("pool", [r3.b, r4.b], [zr.b], zr.t[:, :, 32:64], r3.t[:], r4.t[:], ALU.add)
                kt, va = kv_of(t)
                ACT([zs.b], [va.b], va.t[:, :, 0:64], zs.t[:, 2:4, :], AF.Copy)
                pq = pbf(4)
                pk = pbf(5)
                for h in range(8):
                    TR([zr.b, identb.b], [PB[4].b], pq[0:64, h * 128:(h + 1) * 128], zr.t[:, 4 + h, :], identb.t[:])
                for g in range(2):
                    TR([zr.b, identb.b], [PB[5].b], pk[0:64, g * 128:(g + 1) * 128], zr.t[:, g, :], identb.t[:])
                q = QT[t % 3]
                ACT([PB[4].b], [q.b], q.t[:].rearrange("p h t -> p (h t)"), pq[0:64, :], AF.Copy)
                ACT([PB[5].b], [kt.b], kt.t[:].rearrange("p h t -> p (h t)"), pk[0:64, 0:256], AF.Copy)
                for hh, bank in ((0, 2), (1, 3)):
                    ACT([PB[bank].b], [gu.b], gu.t[:, hh, :], PB[bank].t[:, :], AF.Copy)
                guf = gu.t[:].rearrange("p a f -> p (a f)")
                g1f = g1.t[:].rearrange("p a f -> p (a f)")
                TT("pool", [gu.b], [g1.b], g1f, guf, guf, ALU.mult)
                TS("dve", [g1.b], [g1.b], g1f, g1f, 0.044715, 1.0, ALU.mult, ALU.add)
                TT("pool", [g1.b, gu.b], [g1.b], g1f, g1f, guf, ALU.mult)
                ACT([g1.b], [g1.b], g1f, g1f, AF.Tanh, scale=0.7978845608)
                TS("dve", [g1.b], [g1.b], g1f, g1f, 0.5, 0.5, ALU.mult, ALU.add)
                TT("dve", [g1.b, gu.b], [gu.b], guf, guf, g1f, ALU.mult)
                I("dve", "bn_stats", [gu.b], [bst.b], out=bst.t[:], in_=gu.t[:, 1, :])
                I("dve", "bn_aggr", [bst.b], [mv.b], out=mv.t[:], in_=bst.t[:])
                RSQ(lrs, lrs.t[:], mv, mv.t[:, 1:2])
                TS("dve", [gu.b, mv.b, lrs.b], [vn.b], vn.t[:], gu.t[:, 1, :], mv.t[:, 0:1], lrs.t[:, 0:1], ALU.subtract, ALU.mult)
                TT("pool", [vn.b, sgug.b], [vnb.b], vnb.t[:], vn.t[:], sgug.t[:], ALU.mult)
                for g in range(4):
                    MM([WsT.b, vnb.b], [PB[3].b], PB[3].t[:, g * 128:(g + 1) * 128], WsT.t[:, g, :], vnb.t[:, g * 128:(g + 1) * 128], True, True)
                for g in range(4):
                    STT("dve", [PB[3].b, bsT.b, gu.b], [sb_.b], sb_.t[:, g * 128:(g + 1) * 128], PB[3].t[:, g * 128:(g + 1) * 128],
                        bsT.t[:, g:g + 1], gu.t[:, 0, g * 128:(g + 1) * 128], ALU.add, ALU.mult)
                pv2 = pbf(4)
                for k in range(4):
                    TR([sb_.b, identb.b], [PB[4].b], pv2[:, k * 128:(k + 1) * 128], sb_.t[:, k * 128:(k + 1) * 128], identb.t[:])
                st_ = sT[t % 3]
                ACT([PB[4].b], [st_.b], st_.t[:].rearrange("p k t -> p (k t)"), pv2[:, 0:512], AF.Copy)

            def phaseB(t):
                if VERBOSE:
                    print("even phaseB", t, "op", S.nops)
                r = 1 if t >= NTL else 0
                A, Sh, G = mods[r]
                x = xt[t % 4]
                q = QT[t % 3]
                i2 = t % 2
                tmp, ON, xo = tmp_r[i2], ON_r[i2], xo_r[i2]
                if t >= NTL:
                    keys = [(NTL, None), (NTL + 1, None)]
                else:
                    keys = []
                    if t > 0:
                        keys.append((t - 1, 0))
                    keys.append((t, None))
                    if t < NTL - 1:
                        keys.append((t + 1, 1))
                    keys += [(NTL, None), (NTL + 1, None)]
                npt = 0
                for g in range(2):
                    for j, (kt_i, mk) in enumerate(keys):
                        kt, va = kv_of(kt_i)
                        bank = 5 + (npt % 2)
                        pt_ = PT[npt % 3]
                        npt += 1
                        MM([kt.b, q.b], [PB[bank].b], PB[bank].t[:, :], kt.t[:, g, :],
                           q.t[:, 4 * g:4 * g + 4, :].rearrange("p h t -> p (h t)"), True, mk is None)
                        if mk is not None:
                            MM([identb.b, maskneg.b], [PB[bank].b], PB[bank].t[:, :], identb.t[:], maskneg.t[:, mk, :], False, True)
                        ACT([PB[bank].b], [pt_.b], pt_.t[:], PB[bank].t[:, :], AF.Exp, scale=0.125)
                        MM([va.b, pt_.b], [PB[7].b], PB[7].t[0:65, :], va.t[:, g, :], pt_.t[:], j == 0, False)
                    MM([e65.b, esink.b], [PB[7].b], PB[7].t[0:65, :], e65.t[0:1, :],
                       esink.t[0:1, 4 * g:4 * g + 4, :].rearrange("p h t -> p (h t)"), False, True)
                    attn_norm(7, 2, rs_r[g], Osb_r[g], ON.t[:, 4 * g:4 * g + 4, :].rearrange("p h t -> p (h t)"), 512, ON.b)
                wout_residual(t, ON, sT[t % 3], Wo_o, Wo_s, G, x, tmp, xo)

            phaseA(NTL)
            phaseA(NTL + 1)
            if need_ctx:
                phaseB(NTL)
                phaseB(NTL + 1)
            phaseA(0)
            for t in range(NTL):
                if t + 1 < NTL:
                    phaseA(t + 1)
                phaseB(t)
            S.barrier()

    def odd_mixer(l, need_ctx):
        i = l // 2
        SCALE = 96.0 ** -0.5
        with ExitStack() as ks:
            KT = kb.sb(ks, [96, 8, NTOK], BF16, "KTm")
            VA = kb.sb(ks, [128, NT, 8, 65], BF16, "VAm")
            I("pool", "memset", [], [VA.b], VA.t[:].rearrange("p t h d -> p (t h d)"), 1.0)
            with ExitStack() as ms:
                Wa = kb.sb(ms, [128, 8, 672], BF16, "Wa")
                Wukv = kb.sb(ms, [128, 2, 1024], BF16, "Wukv")
                Wuq = kb.sb(ms, [128, 3, 768], BF16, "Wuq")
                gkv = kb.sb(ms, [128, 256], F32, "gkv")
                gq = kb.sb(ms, [128, 384], F32, "gq")
                DMA("pool", [], [Wa.b], Wa.t[:], od_w_in[i].rearrange("(k p) c -> p k c", p=128)[:, :, 0:672])
                DMA("pool", [], [Wukv.b], Wukv.t[:], od_w_ukv[i].rearrange("(k p) c -> p k c", p=128))
                DMA("pool", [], [Wuq.b], Wuq.t[:], od_w_uq[i].rearrange("(k p) c -> p k c", p=128))
                bcast_row(gkv.t[:], od_kv_norm_g[i], [gkv.b])
                bcast_row(gq.t[:], od_q_norm_g[i], [gq.b])
                mods = load_mod(ms, l, 0, norm1_g[l], want_g=False)
                xt = kb.ring(ms, 2, [128, D], F32, "oxt")
                junk = kb.sb(ms, [128, D], BF16, "ojunk")
                tmp = kb.sb(ms, [128, D], F32, "otmp")
                hb = kb.sb(ms, [128, D], BF16, "ohb")
                hT = kb.sb(ms, [128, 8, 128], BF16, "ohT")
                msr = kb.sb(ms, [128, 1], F32, "omsr")
                rstd = kb.sb(ms, [128, 1], F32, "orstd")
                zc = kb.sb(ms, [128, 672], F32, "zc")
                ms2 = kb.sb(ms, [128, 2], F32, "ms2")
                rs2 = kb.sb(ms, [128, 2], F32, "rs2")
                cn = kb.sb(ms, [128, 640], BF16, "cn")
                cT = kb.sb(ms, [128, 5, 128], BF16, "cT")
                qs = kb.sb(ms, [128, 9, 96], F32, "qs")
                qr = kb.sb(ms, [128, 9, 96], BF16, "qr")
                kf = kb.sb(ms, [128, 8, 96], BF16, "kf")
                p1 = kb.sb(ms, [128, 9, 16], F32, "p1")
                p2 = kb.sb(ms, [128, 9, 16], F32, "p2")
                p3 = kb.sb(ms, [128, 9, 16], F32, "p3")
                p4 = kb.sb(ms, [128, 9, 16], F32, "p4")
                qT = kb.ring(ms, 2, [96, 8, 128], BF16, "qT")
                I("pool", "memset", [], [qs.b], qs.t[:].rearrange("p h d -> p (h d)"), 0.0)
                for t in range(NT):
                    r = 1 if t >= NTL else 0
                    A, Sh, _ = mods[r]
                    x = xt[t % 2]
                    DMA("sp", [Xb[t]], [x.b], x.t[:], X[t * 128:(t + 1) * 128, :])
                    norm_mod(x, A, Sh, hb, msr, rstd, junk, tmp)
                    transpose8(hb, hT, 4)
                    for (bank, c0, c1) in ((0, 0, 512), (1, 512, 672)):
                        for k in range(8):
                            MM([hT.b, Wa.b], [PB[bank].b], PB[bank].t[:, 0:c1 - c0], hT.t[:, k, :], Wa.t[:, k, c0:c1], k == 0, k == 7)
                    ACT([PB[0].b], [zc.b], zc.t[:, 0:512], PB[0].t[:, :], AF.Copy)
                    ACT([PB[1].b], [zc.b], zc.t[:, 512:672], PB[1].t[:, 0:160], AF.Copy)
                    ACT([zc.b], [junk.b, ms2.b], junk.t[:, 0:256], zc.t[:, 0:256], AF.Square, scale=1.0 / 16.0, accum_out=ms2.t[:, 0:1])
                    ACT([zc.b], [junk.b, ms2.b], junk.t[:, 256:640], zc.t[:, 288:672], AF.Square, scale=384.0 ** -0.5, accum_out=ms2.t[:, 1:2])
                    RSQ(rs2, rs2.t[:], ms2, ms2.t[:])
                    STT("dve", [zc.b, rs2.b, gkv.b], [cn.b], cn.t[:, 0:256], zc.t[:, 0:256], rs2.t[:, 0:1], gkv.t[:], ALU.mult, ALU.mult)
                    STT("dve", [zc.b, rs2.b, gq.b], [cn.b], cn.t[:, 256:640], zc.t[:, 288:672], rs2.t[:, 1:2], gq.t[:], ALU.mult, ALU.mult)
                    pv = pbf(4)
                    for k in range(5):
                        TR([cn.b, identb.b], [PB[4].b], pv[:, k * 128:(k + 1) * 128], cn.t[:, k * 128:(k + 1) * 128], identb.t[:])
                    ACT([PB[4].b], [cT.b], cT.t[:].rearrange("p k t -> p (k t)"), pv[:, 0:640], AF.Copy)
                    for half in range(2):
                        for kc in range(2):
                            MM([cT.b, Wukv.b], [PB[2 + half].b], PB[2 + half].t[:, :], cT.t[:, kc, :],
                               Wukv.t[:, kc, half * 512:(half + 1) * 512], kc == 0, kc == 1)
                    for (bank, c0, c1) in ((5, 0, 512), (6, 512, 768)):
                        for kc in range(3):
                            MM([cT.b, Wuq.b], [PB[bank].b], PB[bank].t[:, 0:c1 - c0], cT.t[:, 2 + kc, :], Wuq.t[:, kc, c0:c1], kc == 0, kc == 2)
                    for half in range(2):
                        pvv = PB[2 + half].t[:, :].rearrange("p (h d) -> p h d", d=128)
                        ACT([PB[2 + half].b], [kf.b], kf.t[:, 4 * half:4 * half + 4, 0:64], pvv[:, :, 0:64], AF.Copy)
                        ACT([PB[2 + half].b], [VA.b], VA.t[:, t, 4 * half:4 * half + 4, 0:64], pvv[:, :, 64:128], AF.Copy)
                    qsf = qs.t[:].rearrange("p h d -> p (h d)")
                    ACT([PB[5].b], [qs.b], qsf[:, 0:512], PB[5].t[:, :], AF.Copy)
                    ACT([PB[6].b], [qs.b], qsf[:, 512:768], PB[6].t[:, 0:256], AF.Copy)
                    I("dve", "tensor_copy", [zc.b], [qs.b], out=qs.t[:, 8, 64:96], in_=zc.t[:, 256:288])
                    cos = ropeC_sb.t[:, t, 0:16].rearrange("p (o f) -> p o f", o=1).to_broadcast([128, 9, 16])
                    sin = ropeC_sb.t[:, t, 16:32].rearrange("p (o f) -> p o f", o=1).to_broadcast([128, 9, 16])
                    x1 = qs.t[:, :, 64:80]
                    x2 = qs.t[:, :, 80:96]
                    TT("dve", [qs.b, ropeC_sb.b], [p1.b], p1.t[:], x1, cos, ALU.mult)
                    TT("pool", [qs.b, ropeC_sb.b], [p2.b], p2.t[:], x2, sin, ALU.mult)
                    TT("dve", [qs.b, ropeC_sb.b], [p3.b], p3.t[:], x2, cos, ALU.mult)
                    TT("pool", [qs.b, ropeC_sb.b], [p4.b], p4.t[:], x1, sin, ALU.mult)
                    TT("dve", [p1.b, p2.b], [qr.b], qr.t[:, :, 64:80], p1.t[:], p2.t[:], ALU.subtract)
                    TT("pool", [p3.b, p4.b], [qr.b], qr.t[:, :, 80:96], p3.t[:], p4.t[:], ALU.add)
                    ACT([qs.b], [qr.b], qr.t[:, 0:8, 0:64], qs.t[:, 0:8, 0:64], AF.Copy)
                    I("dve", "tensor_copy", [qr.b], [kf.b], out=kf.t[:, :, 64:96],
                      in_=qr.t[:, 8:9, 64:96].to_broadcast([128, 8, 32]))
                    pk = pbf(4)
                    pq = pbf(7)
                    for h in range(8):
                        TR([kf.b, identb.b], [PB[4].b], pk[0:96, h * 128:(h + 1) * 128], kf.t[:, h, :], identb.t[:])
                    for h in range(8):
                        TR([qr.b, identb.b], [PB[7].b], pq[0:96, h * 128:(h + 1) * 128], qr.t[:, h, :], identb.t[:])
                    ACT([PB[4].b], [KT.b], KT.t[:, :, t * 128:(t + 1) * 128], pk[0:96, :].rearrange("p (h t) -> p h t", t=128), AF.Copy)
                    q_ = qT[t % 2]
                    ACT([PB[7].b], [q_.b], q_.t[:].rearrange("p h t -> p (h t)"), pq[0:96, :], AF.Copy)
                    DMA("sp", [q_.b], [], QTD[:, :, t * 128:(t + 1) * 128].rearrange("h d t -> d h t"), q_.t[:])
                S.barrier()
            with ExitStack() as ms:
                qt = kb.ring(ms, 2, [96, 8, 512], BF16, "qt")
                PT = kb.ring(ms, 6, [128, 512], BF16, "PTm")
                rs = kb.sb(ms, [65, 512], F32, "rsm")
                Osb = kb.sb(ms, [64, 512], F32, "Osbm")
                on = kb.ring(ms, 2, [64, 512], BF16, "onm")
                blocks = [(b * 512, 512, list(range(NT))) for b in range(NLAT // 512)]
                if need_ctx:
                    blocks.append((NLAT, NCTX, [NTL, NTL + 1]))
                npt = 0
                nh = 0
                for bi, (q0, nq, keys) in enumerate(blocks):
                    q = qt[bi % 2]
                    DMA("sp", [], [q.b], q.t[:, :, 0:nq], QTD[:, :, q0:q0 + nq].rearrange("h d t -> d h t"))
                    for h in range(8):
                        pbank = 7 if nh % 2 == 0 else 3
                        bbank = 2 if nh % 2 == 0 else 1
                        o_ = on[nh % 2]
                        nh += 1
                        for j, kt_i in enumerate(keys):
                            bank = (5, 6, 0, 4)[npt % 4]
                            pt_ = PT[npt % 6]
                            npt += 1
                            MM([KT.b, q.b], [PB[bank].b], PB[bank].t[:, 0:nq], KT.t[:, h, kt_i * 128:(kt_i + 1) * 128], q.t[:, h, 0:nq], True, True)
                            ACT([PB[bank].b], [pt_.b], pt_.t[:, 0:nq], PB[bank].t[:, 0:nq], AF.Exp, scale=SCALE)
                            MM([VA.b, pt_.b], [PB[pbank].b], PB[pbank].t[0:65, 0:nq], VA.t[:, kt_i, h, :], pt_.t[:, 0:nq], j == 0, j == len(keys) - 1)
                        attn_norm(pbank, bbank, rs, Osb, o_.t[:, 0:nq], nq, o_.b)
                        DMA("sp", [o_.b], [], OTD[h, :, q0:q0 + nq], o_.t[:, 0:nq])
                S.barrier()
        with ExitStack() as ms:
            Wb = kb.sb(ms, [128, 8, 1536], BF16, "Wb")
            Wo_o = kb.sb(ms, [64, 8, D], BF16, "Wo_o2")
            Wo_c = kb.sb(ms, [128, 4, D], BF16, "Wo_c")
            cw = kb.sb(ms, [128, 3, 4], F32, "cw")
            cwb = kb.sb(ms, [128, 3, 4, 128], F32, "cwb")
            DMA("pool", [], [Wb.b], Wb.t[:], od_w_in[i].rearrange("(k p) c -> p k c", p=128)[:, :, 672:2208])
            DMA("pool", [], [Wo_o.b], Wo_o.t[:], od_w_out[i, 0:512, :].rearrange("(h d) c -> d h c", d=64))
            DMA("pool", [], [Wo_c.b], Wo_c.t[:], od_w_out[i, 512:1024, :].rearrange("(k p) c -> p k c", p=128))
            DMA("sp", [], [cw.b], cw.t[:].rearrange("p j (c o) -> p j c o", o=1), od_conv_w[i].rearrange("j (c p o) -> p j c o", p=128, o=1))
            I("dve", "tensor_copy", [cw.b], [cwb.b], out=cwb.t[:].rearrange("p j c t -> p (j c) t"),
              in_=cw.t[:].rearrange("p j (c o) -> p (j c) o", o=1).to_broadcast([128, 12, 128]))
            mods = load_mod(ms, l, 0, norm1_g[l])
            xt = kb.ring(ms, 3, [128, D], F32, "cxt")
            junk = kb.sb(ms, [128, D], BF16, "cjunk")
            tmp_r = kb.ring(ms, 2, [128, D], F32, "ctmp")
            hb_r = kb.ring(ms, 2, [128, D], BF16, "chb")
            hT_r = kb.ring(ms, 2, [128, 8, 128], BF16, "chT")
            msr_r = kb.ring(ms, 2, [128, 1], F32, "cmsr")
            rstd_r = kb.ring(ms, 2, [128, 1], F32, "crstd")
            UT = kb.ring(ms, 3, [128, 4, 130], F32, "UT")
            BT = kb.ring(ms, 3, [128, 4, 128], F32, "BT")
            Cs_r = kb.ring(ms, 2, [128, 4, 128], F32, "Cs")
            c1_r = kb.ring(ms, 2, [128, 4, 128], F32, "c1")
            c2_r = kb.ring(ms, 2, [128, 4, 128], F32, "c2")
            cTt_r = kb.ring(ms, 2, [128, 4, 128], BF16, "cTt")
            ot = kb.ring(ms, 3, [64, 8, 128], BF16, "ot")
            xo_r = kb.ring(ms, 2, [128, D], F32, "cxo")
            last = NT if need_ctx else NTL

            def seg_first(t):
                return t == 0 or t == NTL

            def seg_last(t):
                return t == NTL - 1 or t == NT - 1

            def phaseA(t):
                r = 1 if t >= NTL else 0
                A, Sh, G = mods[r]
                x = xt[t % 3]
                u = UT[t % 3]
                i2 = t % 2
                tmp, hb, hT, msr, rstd, Cs = tmp_r[i2], hb_r[i2], hT_r[i2], msr_r[i2], rstd_r[i2], Cs_r[i2]
                DMA("sp", [Xb[t]], [x.b], x.t[:], X[t * 128:(t + 1) * 128, :])
                norm_mod(x, A, Sh, hb, msr, rstd, junk, tmp)
                transpose8(hb, hT, 4)
                zb = 0
                for part in range(3):
                    for c in range(4):
                        c0 = part * 512 + c * 128
                        for k in range(8):
                            MM([hT.b, Wb.b], [PB[zb + part].b], PB[zb + part].t[:, c * 128:(c + 1) * 128], Wb.t[:, k, c0:c0 + 128], hT.t[:, k, :], k == 0, k == 7)
                b_ = BT[t % 3]
                ACT([PB[zb].b], [b_.b], b_.t[:].rearrange("p c t -> p (c t)"), PB[zb].t[:, :], AF.Copy)
                ACT([PB[zb + 1].b], [Cs.b], Cs.t[:].rearrange("p c t -> p (c t)"), PB[zb + 1].t[:, :], AF.Copy)
                TT("dve", [Cs.b, PB[zb + 2].b], [u.b], u.t[:, :, 1:129], Cs.t[:], PB[zb + 2].t[:, :].rearrange("p (c t) -> p c t", t=128), ALU.mult)
                if seg_first(t):
                    I("pool", "memset", [u.b], [u.b], u.t[:, :, 0:1], 0.0)
                else:
                    up = UT[(t - 1) % 3]
                    I("pool", "tensor_copy", [u.b, up.b], [u.b], out=u.t[:, :, 0:1], in_=up.t[:, :, 128:129])
                    I("pool", "tensor_copy", [u.b, up.b], [up.b], out=up.t[:, :, 129:130], in_=u.t[:, :, 1:2])
                if seg_last(t):
                    I("pool", "memset", [u.b], [u.b], u.t[:, :, 129:130], 0.0)

            def phaseB(t):
                r = 1 if t >= NTL else 0
                A, Sh, G = mods[r]
                x = xt[t % 3]
                u = UT[t % 3]
                b_ = BT[t % 3]
                i2 = t % 2
                tmp, c1, c2, cTt, xo = tmp_r[i2], c1_r[i2], c2_r[i2], cTt_r[i2], xo_r[i2]
                TT("dve", [u.b, cwb.b], [c1.b], c1.t[:], u.t[:, :, 0:128], cwb.t[:, 0], ALU.mult)
                TT("pool", [u.b, cwb.b], [c2.b], c2.t[:], u.t[:, :, 1:129], cwb.t[:, 1], ALU.mult)
                TT("dve", [c1.b, c2.b], [c1.b], c1.t[:], c1.t[:], c2.t[:], ALU.add)
                TT("pool", [u.b, cwb.b], [c2.b], c2.t[:], u.t[:, :, 2:130], cwb.t[:, 2], ALU.mult)
                TT("dve", [c1.b, c2.b], [c1.b], c1.t[:], c1.t[:], c2.t[:], ALU.add)
                TT("dve", [c1.b, b_.b], [cTt.b], cTt.t[:], c1.t[:], b_.t[:], ALU.mult)
                o_ = ot[t % 3]
                DMA("sp", [], [o_.b], o_.t[:], OTD[:, :, t * 128:(t + 1) * 128].rearrange("h d t -> d h t"))
                wout_residual(t, o_, cTt, Wo_o, Wo_c, G, x, tmp, xo)

            order = list(range(NTL)) + ([NTL, NTL + 1] if need_ctx else [])
            for idx, t in enumerate(order):
                if idx == 0 or seg_first(t):
                    phaseA(t)
                if not seg_last(t):
                    phaseA(t + 1)
                phaseB(t)
            S.barrier()

    def ffn(l, need_ctx):
        with ExitStack() as ms:
            mods = load_mod(ms, l, 3, norm2_g[l], want_as=False)
            G2 = [mods[0][2], mods[1][2]]
            affT = kb.sb(ms, [128, NT, NE], F32, "affT")
            affE = kb.sb(ms, [NE, NTOK], F32, "affE")
            tok_i = kb.sb(ms, [128, 2, 4, NE], I32, "tok_i")
            ntiles = NT if need_ctx else NTL
            with ExitStack() as f1:
                modsA = load_mod(f1, l, 3, norm2_g[l], want_g=False)
                Wr = kb.sb(f1, [128, 8, NE], BF16, "Wr")
                DMA("pool", [], [Wr.b], Wr.t[:], router_w[l].rearrange("(k p) c -> p k c", p=128))
                xt = kb.ring(f1, 3, [128, D], F32, "fxt")
                junk = kb.sb(f1, [128, D], BF16, "fjunk")
                tmp_r = kb.ring(f1, 2, [128, D], F32, "ftmp")
                hb = kb.ring(f1, 3, [128, D], BF16, "fhb")
                hT_r = kb.ring(f1, 2, [128, 8, 128], BF16, "fhT")
                msr_r = kb.ring(f1, 2, [128, 1], F32, "fmsr")
                rstd_r = kb.ring(f1, 2, [128, 1], F32, "frstd")
                mx_r = kb.ring(f1, 2, [128, 1], F32, "mx")
                ssum_r = kb.ring(f1, 2, [128, 1], F32, "ssum")
                ee_r = kb.ring(f1, 2, [128, NE], F32, "ee")
                for t in range(ntiles):
                    r = 1 if t >= NTL else 0
                    A, Sh, _ = modsA[r]
                    x = xt[t % 3]
                    h = hb[t % 3]
                    i2 = t % 2
                    tmp, hT, msr, rstd, mx, ssum, ee = tmp_r[i2], hT_r[i2], msr_r[i2], rstd_r[i2], mx_r[i2], ssum_r[i2], ee_r[i2]
                    DMA("sp", [Xb[t]], [x.b], x.t[:], X[t * 128:(t + 1) * 128, :])
                    norm_mod(x, A, Sh, h, msr, rstd, junk, tmp)
                    DMA("sp", [h.b], [], H2[t * 128:(t + 1) * 128, :], h.t[:])
                    transpose8(h, hT, 4 + i2)
                    for k in range(8):
                        MM([hT.b, Wr.b], [PB[i2].b], PB[i2].t[:, 0:NE], hT.t[:, k, :], Wr.t[:, k, :], k == 0, k == 7)
                    I("dve", "tensor_reduce", [PB[i2].b], [mx.b], out=mx.t[:], in_=PB[i2].t[:, 0:NE], axis=AX.X, op=ALU.max)
                    TS("dve", [mx.b], [mx.b], mx.t[:], mx.t[:], -1.0, None, ALU.mult)
                    ACT([PB[i2].b, mx.b], [ee.b, ssum.b], ee.t[:], PB[i2].t[:, 0:NE], AF.Exp, bias=mx.t[:, 0:1], accum_out=ssum.t[:, 0:1])
                    I("dve", "reciprocal", [ssum.b], [ssum.b], out=ssum.t[:], in_=ssum.t[:])
                    TS("dve", [ee.b, ssum.b], [affT.b], affT.t[:, t, :], ee.t[:], ssum.t[:, 0:1], None, ALU.mult)
                    TR([affT.b, identf.b], [PB[2 + i2].b], PB[2 + i2].t[0:NE, 0:128], affT.t[:, t, :], identf.t[:])
                    ACT([PB[2 + i2].b], [affE.b], affE.t[:, t * 128:(t + 1) * 128], PB[2 + i2].t[0:NE, 0:128], AF.Copy)
                for t0 in range(0, ntiles, 8):
                    t1 = min(t0 + 8, ntiles)
                    DMA("sp", [affT.b], [], AFF[t0 * 128:t1 * 128, :].rearrange("(t p) e -> p t e", p=128), affT.t[:, t0:t1, :])
                S.barrier()
            segs = [(0, NLAT, 512)]
            if need_ctx:
                segs.append((NLAT, NCTX, 32))
            with ExitStack() as ws:
                lo = kb.sb(ws, [NE, 1], F32, "lo")
                mid = kb.sb(ws, [NE, 1], F32, "mid")
                cnt = kb.sb(ws, [NE, 1], F32, "cnt")
                stp = kb.sb(ws, [NE, 1], F32, "stp")
                mask = kb.sb(ws, [NE, NLAT], F32, "mask")
                csum = kb.sb(ws, [NE, NLAT], F32, "csum")
                ctl = kb.sb(ws, [NE, NTL], F32, "ctl")
                ctb = kb.sb(ws, [128, NE * NTL], F32, "ctb")
                le = kb.sb(ws, [128, NE, 128], F32, "le")
                Tf = kb.sb(ws, [128, NE], F32, "Tf")
                ridx = kb.sb(ws, [128, NE], F32, "ridx")
                ridx_i = kb.sb(ws, [128, NE], I32, "ridx_i")
                Gall = kb.sb(ws, [128, NE, 128], F32, "Gall")
                loc = kb.sb(ws, [128, NE], F32, "loc")
                cslot = kb.sb(ws, [128, 4], F32, "cslot")
                e32 = kb.sb(ws, [128, NE], F32, "e32")
                I("pool", "iota", [], [cslot.b], cslot.t[:], pattern=[[128, 4]], base=0, channel_multiplier=1,
                  allow_small_or_imprecise_dtypes=True)
                csb = Buf("CS")
                ctbuf = Buf("CT")
                Gb = [Buf("G%d" % ex) for ex in range(NE)]
                for si, (n0, n, cap) in enumerate(segs):
                    ntl = n // 128
                    nst = (cap + 127) // 128
                    I("pool", "iota", [], [e32.b], e32.t[:], pattern=[[ntl, NE]], base=0, channel_multiplier=0,
                      allow_small_or_imprecise_dtypes=True)
                    av = affE.t[:, n0:n0 + n]
                    I("dve", "memset", [], [lo.b], lo.t[:], 0.0)
                    for it in range(23):
                        w = 2.0 ** (-(it + 1))
                        TS("dve", [lo.b], [mid.b], mid.t[:], lo.t[:], w, None, ALU.add)
                        TS("dve", [affE.b, mid.b], [mask.b, cnt.b], mask.t[:, 0:n], av, mid.t[:, 0:1], 0.0, ALU.is_ge, ALU.add,
                           accum_out=cnt.t[:, 0:1])
                        TS("dve", [cnt.b], [stp.b], stp.t[:], cnt.t[:], float(cap) - 0.5, w, ALU.is_ge, ALU.mult)
                        TT("dve", [lo.b, stp.b], [lo.b], lo.t[:], lo.t[:], stp.t[:], ALU.add)
                    TS("dve", [affE.b, lo.b], [mask.b], mask.t[:, 0:n], av, lo.t[:, 0:1], None, ALU.is_ge)
                    I("dve", "tensor_tensor_scan", [mask.b], [csum.b], out=csum.t[:, 0:n], data0=mask.t[:, 0:n], data1=mask.t[:, 0:n],
                      initial=0.0, op0=ALU.add, op1=ALU.max)
                    DMA("sp", [csum.b], [csb], CS[0:NE * ntl, :].rearrange("(e t) p -> e t p", t=ntl),
                        csum.t[:, 0:n].rearrange("e (t p) -> e t p", p=128))
                    I("dve", "tensor_copy", [csum.b], [ctl.b], out=ctl.t[:, 0:ntl],
                      in_=csum.t[:, 0:n].rearrange("e (t p) -> e t p", p=128)[:, :, 127])
                    DMA("sp", [ctl.b], [ctbuf], CT[0:NE * ntl].rearrange("(e t) -> e t", t=ntl), ctl.t[:, 0:ntl])
                    DMA("sp", [ctbuf], [ctb.b], ctb.t[:, 0:NE * ntl], CT[0:NE * ntl].partition_broadcast(128))
                    ctbv = ctb.t[:, 0:NE * ntl].rearrange("p (e t) -> p e t", t=ntl)
                    for j in range(nst):
                        TS("dve", [ctb.b, cslot.b], [le.b], le.t[:, :, 0:ntl], ctbv, cslot.t[:, j:j + 1], None, ALU.is_le)
                        I("dve", "tensor_reduce", [le.b], [Tf.b], out=Tf.t[:], in_=le.t[:, :, 0:ntl], axis=AX.X, op=ALU.add)
                        TT("dve", [Tf.b, e32.b], [ridx.b], ridx.t[:], Tf.t[:], e32.t[:], ALU.add)
                        I("dve", "tensor_copy", [ridx.b], [ridx_i.b], out=ridx_i.t[:], in_=ridx.t[:])
                        for ex in range(NE):
                            IDMA([ridx_i.b, csb], [Gb[ex]], Gall.t[:, ex, :], None, CS[:, :],
                                 bass.IndirectOffsetOnAxis(ap=ridx_i.t[:, ex:ex + 1], axis=0), NE * ntl - 1)
                        TS("dve", Gb + [cslot.b], [le.b], le.t[:], Gall.t[:], cslot.t[:, j:j + 1], None, ALU.is_le)
                        I("dve", "tensor_reduce", [le.b], [loc.b], out=loc.t[:], in_=le.t[:], axis=AX.X, op=ALU.add)
                        STT("dve", [Tf.b, loc.b], [loc.b], loc.t[:], Tf.t[:], 128.0, loc.t[:], ALU.mult, ALU.add)
                        if n0:
                            TS("dve", [loc.b], [loc.b], loc.t[:], loc.t[:], float(n0), None, ALU.add)
                        I("dve", "tensor_copy", [loc.b], [tok_i.b], out=tok_i.t[:, si, j, :], in_=loc.t[:])
                S.barrier()
            with ExitStack() as ws:
                W1 = kb.ring(ws, 2, [128, 8, D], BF16, "W1")
                W3 = kb.ring(ws, 2, [128, 8, D], BF16, "W3")
                W2 = kb.ring(ws, 2, [128, 8, D], BF16, "W2")
                xg = kb.ring(ws, 4, [128, D], BF16, "xg")
                ag = kb.ring(ws, 5, [128, NE], F32, "ag")
                xsT_r = kb.ring(ws, 2, [128, 8, 512], BF16, "xsT")
                gT_r = kb.ring(ws, 2, [128, 8, 512], BF16, "gT")
                sa = kb.ring(ws, 3, [128, 512], F32, "sa")
                yo = kb.ring(ws, 3, [128, D], F32, "yo")
                prev_marks = []
                nxg = 0
                nyo = 0
                nsa = 0
                nb = 0
                for ex in range(NE):
                    w1, w3, w2 = W1[ex % 2], W3[ex % 2], W2[ex % 2]
                    for (wt, src) in ((w1, exp_w1), (w3, exp_w3), (w2, exp_w2)):
                        DMA("pool", [], [wt.b], wt.t[:], src[l, ex].rearrange("(k p) c -> p k c", p=128))
                    for si, (n0, n, cap) in enumerate(segs):
                        nst = (cap + 127) // 128
                        sp_ = min(cap, 128)
                        ns = cap
                        xsT = xsT_r[nb % 2]
                        gT = gT_r[nb % 2]
                        nb += 1
                        ags = []
                        for j in range(nst):
                            g_ = xg[nxg % 4]
                            a_ = ag[nxg % 5]
                            nxg += 1
                            ags.append(a_)
                            off = bass.IndirectOffsetOnAxis(ap=tok_i.t[0:sp_, si, j, ex:ex + 1], axis=0)
                            IDMA([tok_i.b], [g_.b], g_.t[0:sp_, :], None, H2[:, :], off, NTOK - 1)
                            IDMA([tok_i.b], [a_.b], a_.t[0:sp_, :], None, AFF[:, :], off, NTOK - 1)
                            pv = pbf(4)
                            for k in range(8):
                                TR([g_.b, identb.b], [PB[4].b], pv[:, k * 128:k * 128 + sp_], g_.t[0:sp_, k * 128:(k + 1) * 128],
                                   identb.t[0:sp_, 0:sp_])
                            ACT([PB[4].b], [xsT.b], xsT.t[:, :, j * 128:j * 128 + sp_],
                                pv[:, :].rearrange("p (k t) -> p k t", t=128)[:, :, 0:sp_], AF.Copy)
                        for f in range(8):
                            fs = slice(f * 128, (f + 1) * 128)
                            ba, bb = (0, 1) if f % 2 == 0 else (2, 3)
                            for k in range(8):
                                MM([w1.b, xsT.b], [PB[ba].b], PB[ba].t[:, 0:ns], w1.t[:, k, fs], xsT.t[:, k, 0:ns], k == 0, k == 7)
                            for k in range(8):
                                MM([w3.b, xsT.b], [PB[bb].b], PB[bb].t[:, 0:ns], w3.t[:, k, fs], xsT.t[:, k, 0:ns], k == 0, k == 7)
                            s_ = sa[nsa % 3]
                            nsa += 1
                            ACT([PB[ba].b], [s_.b], s_.t[:, 0:ns], PB[ba].t[:, 0:ns], AF.Silu)
                            TT("dve", [s_.b, PB[bb].b], [gT.b], gT.t[:, f, 0:ns], s_.t[:, 0:ns], PB[bb].t[:, 0:ns], ALU.mult)
                        marks = []
                        for j in range(nst):
                            y_ = yo[nyo % 3]
                            nyo += 1
                            a_ = ags[j]
                            for half in range(2):
                                cs_ = slice(half * 512, (half + 1) * 512)
                                bank = 5 + half
                                for f in range(8):
                                    MM([gT.b, w2.b], [PB[bank].b], PB[bank].t[0:sp_, :], gT.t[:, f, j * 128:j * 128 + sp_], w2.t[:, f, cs_],
                                       f == 0, f == 7)
                                STT("dve", [PB[bank].b, a_.b, G2[si].b], [y_.b], y_.t[0:sp_, cs_], PB[bank].t[0:sp_, :],
                                    a_.t[0:sp_, ex:ex + 1], G2[si].t[0:sp_, cs_], ALU.mult, ALU.mult)
                            mk = Buf("mk")
                            marks.append(mk)
                            off = bass.IndirectOffsetOnAxis(ap=tok_i.t[0:sp_, si, j, ex:ex + 1], axis=0)
                            IDMA([y_.b, tok_i.b] + prev_marks, [mk], X[:, :], off, y_.t[0:sp_, :], None, NTOK - 1, add=True)
                        prev_marks = marks
                S.barrier()

    def final_norm():
        with ExitStack() as ms:
            fg = kb.sb(ms, [128, D], F32, "fg")
            bcast_row(fg.t[:], final_g, [fg.b])
            xt = kb.ring(ms, 2, [128, D], F32, "nxt")
            junk = kb.sb(ms, [128, D], BF16, "njunk")
            ot = kb.ring(ms, 2, [128, D], F32, "not")
            msr = kb.sb(ms, [128, 1], F32, "nmsr")
            rstd = kb.sb(ms, [128, 1], F32, "nrstd")
            for t in range(NTL):
                x = xt[t % 2]
                o = ot[t % 2]
                DMA("sp", [], [x.b], x.t[:], X[t * 128:(t + 1) * 128, :])
                ACT([x.b], [junk.b, msr.b], junk.t[:], x.t[:], AF.Square, scale=1.0 / 32.0, accum_out=msr.t[:, 0:1])
                RSQ(rstd, rstd.t[:, 0:1], msr, msr.t[:, 0:1])
                STT("dve", [x.b, rstd.b, fg.b], [o.b], o.t[:], x.t[:], rstd.t[:, 0:1], fg.t[:], ALU.mult, ALU.mult)
                DMA("sp", [o.b], [], out[t * 128:(t + 1) * 128, :], o.t[:])
            S.barrier()

    S.barrier()
    for l in range(depth_run):
        need_ctx = l < DEPTH - 1
        if l % 2 == 0:
            even_mixer(l, need_ctx)
        else:
            odd_mixer(l, need_ctx)
        if stop_after == (l, "mixer"):
            break
        ffn(l, need_ctx)
    print("[kernel] ops before final:", S.nops)
    S.force = True
    if dbg:
        xd = nc.dram_tensor("xdbg", [NTOK, D], F32, kind="ExternalOutput").ap()
        DMA("sp", [], [], xd[:, :], X[:, :])
    final_norm()
    S.barrier()
    with nc.allow_non_contiguous_dma(reason="tiny strided parameter vectors"):
        with nc.Block() as block:
            S.emit(block)


def _rope_tables():
    def tab(rot_dim):
        rows = NLAT // 64
        row = np.repeat(np.arange(rows, dtype=np.float32), 64)
        col = np.tile(np.arange(64, dtype=np.float32), rows)
        n_freq = rot_dim // 4
        inv = (np.float32(10000.0) ** (-np.arange(n_freq, dtype=np.float32) / np.float32(n_freq))).astype(np.float32)
        ang = np.concatenate([row[:, None] * inv[None, :], col[:, None] * inv[None, :]], axis=-1).astype(np.float32)
        t = np.concatenate([np.cos(ang), np.sin(ang)], axis=-1).astype(np.float32)
        c = np.concatenate([np.ones((NCTX, rot_dim // 2), np.float32), np.zeros((NCTX, rot_dim // 2), np.float32)], axis=-1)
        full = np.concatenate([t, c], axis=0)
        return np.ascontiguousarray(full.reshape(NT, 128, rot_dim).transpose(1, 0, 2).reshape(128, NT * rot_dim))
    return tab(64), tab(32)


_SHARED = ["c_ctx", "mod_w", "mod_b", "norm1_g", "norm2_g", "ev_w_in", "ev_sink", "ev_sgu_norm_g", "ev_sgu_w", "ev_sgu_b",
           "ev_w_out", "od_w_in", "od_q_norm_g", "od_w_uq", "od_kv_norm_g", "od_w_ukv", "od_conv_w", "od_w_out", "router_w",
           "exp_w1", "exp_w3", "exp_w2", "final_g"]


def make_in_maps(inputs, cores):
    ropeA, ropeC = _rope_tables()
    shared = {k: np.ascontiguousarray(np.asarray(inputs[k], dtype=np.float32)) for k in _SHARED}
    maps = []
    for b in cores:
        m = dict(shared)
        m["x"] = np.ascontiguousarray(np.asarray(inputs["x"][b], dtype=np.float32))
        m["ctx"] = np.ascontiguousarray(np.asarray(inputs["ctx"][b], dtype=np.float32))
        m["c"] = np.ascontiguousarray(np.asarray(inputs["c"][b], dtype=np.float32))
        m["ropeA"] = ropeA
        m["ropeC"] = ropeC
        maps.append(m)
    return maps


def kernel(**inputs):
    nc = build_program()
    maps = make_in_maps(inputs, list(range(8)))
    res = run_bass_kernel_spmd(nc, maps, core_ids=list(range(8)))
    return np.stack([np.asarray(r["out"], dtype=np.float32) for r in res.results], axis=0)
```
